# Optimizing a Trainium2 kernel written in Bass

```python
import jax, jax.numpy as jnp
from jax import lax
import numpy as np

D_MODEL = 2048
BATCH = 2
SEQ = 8192
DEPTH = 4

N_MIXERS = 2
N_SB_LAYERS = (DEPTH + 1) // 2
N_SG_LAYERS = DEPTH // 2
SB_HEADS = 16
SB_HEAD_DIM = D_MODEL // SB_HEADS
Q_BLOCK = 128
SG_GROUPS = 16
SG_CHUNK = 128
SG_HALF = D_MODEL
SG_GROUP_DIM = SG_HALF // SG_GROUPS
N_GROUPS = 4
EXPERTS_PER_GROUP = 8
N_EXPERTS = N_GROUPS * EXPERTS_PER_GROUP
TOP_K_IN_GROUP = 2
D_EXPERT = 256
PLE_DIM = 256
EPS = 1e-6

kernel_name = "hybrid_stickbreak_spatialgate_hmoe"


def rmsnorm(x, g):
    xf = x.astype(jnp.float32)
    y = xf * lax.rsqrt(jnp.mean(xf * xf, axis=-1, keepdims=True) + EPS)
    return (y * g.astype(jnp.float32)).astype(x.dtype)


def stick_breaking_attention(h, w_in, q_norm, k_norm, w_out):
    B, S, _ = h.shape
    qkv = h @ w_in
    q, k, v = jnp.split(qkv, 3, axis=-1)
    to_heads = lambda t: t.reshape(B, S, SB_HEADS, SB_HEAD_DIM).transpose(0, 2, 1, 3)
    q = rmsnorm(to_heads(q), q_norm)
    k = rmsnorm(to_heads(k), k_norm)
    v = to_heads(v)
    scale = SB_HEAD_DIM ** -0.5
    n_blk = S // Q_BLOCK
    q_blocks = q.reshape(B, SB_HEADS, n_blk, Q_BLOCK, SB_HEAD_DIM).transpose(2, 0, 1, 3, 4)
    starts = jnp.arange(n_blk, dtype=jnp.int32) * Q_BLOCK
    kf = k.astype(jnp.float32)
    vf = v.astype(jnp.float32)
    key_pos = jnp.arange(S, dtype=jnp.int32)

    def one_block(args):
        q_blk, start = args
        z = jnp.einsum('bhqd,bhsd->bhqs', q_blk.astype(jnp.float32), kf) * scale
        t_pos = start + jnp.arange(Q_BLOCK, dtype=jnp.int32)
        causal = key_pos[None, :] < t_pos[:, None]
        log_keep = jnp.where(causal, jax.nn.log_sigmoid(-z), 0.0)
        log_stick = lax.cumsum(log_keep, axis=3, reverse=True) - log_keep
        weight = jnp.where(causal, jnp.exp(jax.nn.log_sigmoid(z) + log_stick), 0.0)
        return jnp.einsum('bhqs,bhsd->bhqd', weight, vf)

    o = lax.map(one_block, (q_blocks, starts))
    o = o.transpose(1, 0, 3, 2, 4).reshape(B, S, D_MODEL).astype(h.dtype)
    return o @ w_out


def spatial_gating_mlp(h, w_in, v_norm, w_s, b_s, w_out):
    B, S, _ = h.shape
    z = jax.nn.gelu(h @ w_in)
    u, v = jnp.split(z, 2, axis=-1)
    v = rmsnorm(v, v_norm)
    n_chunk = S // SG_CHUNK
    vg = v.reshape(B, n_chunk, SG_CHUNK, SG_GROUPS, SG_GROUP_DIM)
    causal = jnp.tril(jnp.ones((SG_CHUNK, SG_CHUNK), dtype=bool))
    w_c = jnp.where(causal[None], w_s, 0.0)
    mixed = jnp.einsum('gts,bcsgd->bctgd', w_c, vg) + b_s.T[None, None, :, :, None]
    gated = u * mixed.reshape(B, S, SG_HALF)
    return gated @ w_out


def hierarchical_moe(h, w_group, b_group, w_expert, b_expert, w_gate, w_up, w_down):
    B, S, D = h.shape
    hf = h.reshape(B * S, D)
    T = hf.shape[0]
    group_logits = (hf @ w_group + b_group).astype(jnp.float32)
    group_prob = jax.nn.softmax(group_logits, axis=-1)
    g_idx = jnp.argmax(group_logits, axis=-1)
    g_w = jnp.take_along_axis(group_prob, g_idx[:, None], axis=-1)
    exp_logits = (hf @ w_expert + b_expert).astype(jnp.float32).reshape(T, N_GROUPS, EXPERTS_PER_GROUP)
    in_group = jnp.take_along_axis(exp_logits, g_idx[:, None, None], axis=1)[:, 0]
    top_val, top_idx = lax.top_k(in_group, TOP_K_IN_GROUP)
    weights = g_w * jax.nn.softmax(top_val, axis=-1)
    expert_id = g_idx[:, None] * EXPERTS_PER_GROUP + top_idx
    gates = jnp.sum(jax.nn.one_hot(expert_id, N_EXPERTS, dtype=jnp.float32) * weights[..., None], axis=1)
    hidden = jax.nn.silu(jnp.einsum('td,edf->tef', hf, w_gate)) * jnp.einsum('td,edf->tef', hf, w_up)
    y = jnp.einsum('tef,efd->td', hidden * gates[:, :, None].astype(hidden.dtype), w_down)
    return y.reshape(B, S, D)


def per_layer_embedding(x, p_i, norm_in, w_gate, w_proj, norm_out):
    gate = jax.nn.sigmoid(rmsnorm(x, norm_in) @ w_gate)
    e = (p_i @ w_proj) * gate
    return x + rmsnorm(e, norm_out)


def setup_inputs(seed: int = 0) -> dict:
    key = jax.random.key(seed)
    ks = jax.random.split(key, 32)
    f32 = jnp.float32

    def w(k, shape, fan_in):
        return jax.random.normal(k, shape, f32) * fan_in ** -0.5

    def gain(k, shape):
        return 1.0 + 0.02 * jax.random.normal(k, shape, f32)

    D = D_MODEL
    return {
        "x": jax.random.normal(ks[0], (BATCH, SEQ, D), f32),
        "p": jax.random.normal(ks[1], (DEPTH, BATCH, SEQ, PLE_DIM), f32),
        "norm_mix": gain(ks[2], (DEPTH, D)),
        "norm_ffn": gain(ks[3], (DEPTH, D)),
        "sb_w_in": w(ks[4], (N_SB_LAYERS, D, 3 * D), D),
        "sb_q_norm": gain(ks[5], (N_SB_LAYERS, SB_HEAD_DIM)),
        "sb_k_norm": gain(ks[6], (N_SB_LAYERS, SB_HEAD_DIM)),
        "sb_w_out": w(ks[7], (N_SB_LAYERS, D, D), D),
        "sg_w_in": w(ks[8], (N_SG_LAYERS, D, 2 * SG_HALF), D),
        "sg_v_norm": gain(ks[9], (N_SG_LAYERS, SG_HALF)),
        "sg_w_s": 0.5 * w(ks[10], (N_SG_LAYERS, SG_GROUPS, SG_CHUNK, SG_CHUNK), SG_CHUNK),
        "sg_b_s": gain(ks[11], (N_SG_LAYERS, SG_GROUPS, SG_CHUNK)),
        "sg_w_out": w(ks[12], (N_SG_LAYERS, SG_HALF, D), SG_HALF),
        "moe_w_group": w(ks[13], (DEPTH, D, N_GROUPS), D),
        "moe_b_group": 0.01 * jax.random.normal(ks[14], (DEPTH, N_GROUPS), f32),
        "moe_w_expert": w(ks[15], (DEPTH, D, N_EXPERTS), D),
        "moe_b_expert": 0.01 * jax.random.normal(ks[16], (DEPTH, N_EXPERTS), f32),
        "moe_w_gate": w(ks[17], (DEPTH, N_EXPERTS, D, D_EXPERT), D),
        "moe_w_up": w(ks[18], (DEPTH, N_EXPERTS, D, D_EXPERT), D),
        "moe_w_down": w(ks[19], (DEPTH, N_EXPERTS, D_EXPERT, D), D_EXPERT),
        "ple_norm_in": gain(ks[20], (DEPTH, D)),
        "ple_w_gate": w(ks[21], (DEPTH, D, D), D),
        "ple_w_proj": w(ks[22], (DEPTH, PLE_DIM, D), PLE_DIM),
        "ple_norm_out": gain(ks[23], (DEPTH, D)),
    }


def reference(x, p, norm_mix, norm_ffn,
              sb_w_in, sb_q_norm, sb_k_norm, sb_w_out,
              sg_w_in, sg_v_norm, sg_w_s, sg_b_s, sg_w_out,
              moe_w_group, moe_b_group, moe_w_expert, moe_b_expert,
              moe_w_gate, moe_w_up, moe_w_down,
              ple_norm_in, ple_w_gate, ple_w_proj, ple_norm_out):
    for i in range(DEPTH):
        h = rmsnorm(x, norm_mix[i])
        j = i // N_MIXERS
        if i % N_MIXERS == 0:
            x = x + stick_breaking_attention(h, sb_w_in[j], sb_q_norm[j], sb_k_norm[j], sb_w_out[j])
        else:
            x = x + spatial_gating_mlp(h, sg_w_in[j], sg_v_norm[j], sg_w_s[j], sg_b_s[j], sg_w_out[j])
        x = x + hierarchical_moe(rmsnorm(x, norm_ffn[i]), moe_w_group[i], moe_b_group[i],
                                 moe_w_expert[i], moe_b_expert[i],
                                 moe_w_gate[i], moe_w_up[i], moe_w_down[i])
        x = per_layer_embedding(x, p[i], ple_norm_in[i], ple_w_gate[i], ple_w_proj[i], ple_norm_out[i])
    return x
```

```python
import numpy as np
import concourse.bass as bass
import concourse.mybir as mybir
from concourse.bass_utils import run_bass_kernel_spmd
from contextlib import ExitStack

F32 = mybir.dt.float32
BF16 = mybir.dt.bfloat16
AF = mybir.ActivationFunctionType
ALU = mybir.AluOpType
AX = mybir.AxisListType

ENGS = ['pe', 'act', 'dve', 'pool', 'sp']
DMAQ = ('sp', 'act', 'pool')


class Sched:
    def __init__(self, nc, es, R=4):
        self.nc = nc
        self.sem = {e: es.enter_context(nc.semaphore('s_' + e)) for e in ENGS}
        self.cnt = {e: 0 for e in ENGS}
        self.ops = {e: [] for e in ENGS}
        self.waited = {e: {} for e in ENGS}
        self.lastw = {}
        self.readers = {}
        self.R = R
        self.dsem = {q: [es.enter_context(nc.semaphore('d_%s%d' % (q, i))) for i in range(R)] for q in DMAQ}
        self.dcnt = {q: 0 for q in DMAQ}
        self.semobj = {}
        for e in ENGS:
            self.semobj[('c', e)] = self.sem[e]
        for q in DMAQ:
            for i in range(R):
                self.semobj[('d', q, i)] = self.dsem[q][i]

    def _deps(self, eng, reads, writes, skip_sem=None):
        deps = {}

        def add(tok):
            if tok is None:
                return
            s, v = tok
            if s == skip_sem:
                return
            if deps.get(s, 0) < v:
                deps[s] = v
        for k in reads:
            add(self.lastw.get(k))
        for k in writes:
            add(self.lastw.get(k))
            for s, v in self.readers.get(k, {}).items():
                add((s, v))
        out = []
        w = self.waited[eng]
        for s, v in deps.items():
            if w.get(s, 0) < v:
                w[s] = v
                out.append((s, v))
        return out

    def _commit(self, tok, reads, writes):
        s, v = tok
        for k in reads:
            r = self.readers.setdefault(k, {})
            if r.get(s, 0) < v:
                r[s] = v
        for k in writes:
            self.lastw[k] = tok
            self.readers[k] = {}

    def op(self, eng, fn, reads=(), writes=()):
        skip = ('c', 'pe') if eng == 'pe' else None
        waits = self._deps(eng, reads, writes, skip)
        self.cnt[eng] += 1
        tok = (('c', eng), self.cnt[eng])
        self.ops[eng].append((fn, waits, (('c', eng), 1)))
        self._commit(tok, reads, writes)
        return tok

    def dma(self, q, fn, reads=(), writes=()):
        i = self.dcnt[q]
        self.dcnt[q] += 1
        slot = i % self.R
        semk = ('d', q, slot)
        waits = self._deps(q, reads, writes)
        prev = 16 * (i // self.R)
        if prev > 0 and self.waited[q].get(semk, 0) < prev:
            self.waited[q][semk] = prev
            waits.append((semk, prev))
        tok = (semk, prev + 16)
        self.ops[q].append((fn, waits, (semk, 16)))
        self._commit(tok, reads, writes)
        return tok

    def wait_all(self, eng, toks):
        waits = []
        for s, v in toks:
            if self.waited[eng].get(s, 0) < v:
                self.waited[eng][s] = v
                waits.append((s, v))
        self.ops[eng].append((None, waits, None))

    def emit(self):
        nc = self.nc
        S = self
        with nc.Block() as block:
            def run(e, name):
                for fn, waits, inc in S.ops[name]:
                    for s, v in waits:
                        e.wait_ge(S.semobj[s], v)
                    if fn is not None:
                        ins = fn(e)
                        ins.then_inc(S.semobj[inc[0]], inc[1])

            @block.sync
            def _(e):
                run(e, 'sp')

            @block.tensor
            def _(e):
                run(e, 'pe')

            @block.scalar
            def _(e):
                run(e, 'act')

            @block.vector
            def _(e):
                run(e, 'dve')

            @block.gpsimd
            def _(e):
                run(e, 'pool')


D = 2048
NT = 2048
EPS = 1e-6
MUL = ALU.mult
ADD = ALU.add


class Ctx:
    def __init__(self, nc, es):
        self.nc = nc
        self.es = es
        self.S = Sched(nc, es)

    def sb(self, name, shape, dt):
        return self.es.enter_context(self.nc.sbuf_tensor(name, shape, dt))

    def MM(self, out, lhsT, rhs, start, stop, r, w):
        self.S.op('pe', lambda e: e.matmul(out, lhsT, rhs, start=start, stop=stop), r, w)

    def ACT(self, out, in_, func, r, w, **kw):
        self.S.op('act', lambda e: e.activation(out=out, in_=in_, func=func, **kw), r, w)

    def TT(self, eng, out, a, b, op, r, w):
        self.S.op(eng, lambda e: e.tensor_tensor(out=out, in0=a, in1=b, op=op), r, w)

    def TS(self, eng, out, a, s1, s2, op0, op1, r, w):
        if s2 is None:
            self.S.op(eng, lambda e: e.tensor_scalar(out=out, in0=a, scalar1=s1, scalar2=None, op0=op0), r, w)
        else:
            self.S.op(eng, lambda e: e.tensor_scalar(out=out, in0=a, scalar1=s1, scalar2=s2, op0=op0, op1=op1), r, w)

    def STT(self, out, a, s, b, op0, op1, r, w):
        self.S.op('dve', lambda e: e.scalar_tensor_tensor(out=out, in0=a, scalar=s, in1=b, op0=op0, op1=op1), r, w)

    def DMA(self, q, out, in_, r, w):
        return self.S.dma(q, lambda e: e.dma_start(out=out, in_=in_), r, w)

    def OP(self, eng, fn, r, w):
        self.S.op(eng, fn, r, w)


def build_tok(mode):
    nc = bass.Bass("TRN2", target_bir_lowering=False)

    def din(n, s):
        return nc.dram_tensor(n, s, F32, kind="ExternalInput").ap()
    xT = din("xT", [D, NT])
    w_mo = din("w_mo", [D, D])
    if mode == 'attn':
        oT = din("oT", [D, NT])
    else:
        w_in = din("sg_w_in", [D, 4096])
        g_mix_d = din("g_mix", [128, 16])
        vg_d = din("vg", [128, 16])
        wsT_d = din("wsT", [128, 16, 128])
        tri_d = din("trimask", [128, 128])
        bs_d = din("bs", [128, 16, 128])
    g_ffn_d = din("g_ffn", [128, 16])
    wr_d = din("wr", [D, 36])
    br_d = din("br", [128, 36])
    wg_d = din("wg", [32, D, 256])
    wu_d = din("wu", [32, D, 256])
    wd_d = din("wd", [32, 256, D])
    g_pin_d = din("g_pin", [128, 16])
    w_pg = din("w_pg", [D, D])
    w_pp = din("w_pp", [256, D])
    g_pout_d = din("g_pout", [128, 16])
    pT = din("pT", [256, NT])
    ident_d = din("ident", [128, 128])
    ones_d = din("ones", [128, 128])
    xo = nc.dram_tensor("xo", [D, NT], F32, kind="ExternalOutput").ap()

    xT_v = xT.rearrange("(c q) t -> q c t", q=128)
    xo_v = xo.rearrange("(c q) t -> q c t", q=128)
    wmo_v = w_mo.rearrange("(c q) n -> q c n", q=128)
    wpg_v = w_pg.rearrange("(c q) n -> q c n", q=128)
    wpp_v = w_pp.rearrange("(c q) n -> q c n", q=128)
    pT_v = pT.rearrange("(c q) t -> q c t", q=128)
    wr_v = wr_d.rearrange("(c q) n -> q c n", q=128)

    with ExitStack() as es:
        C = Ctx(nc, es)
        S = C.S
        sb = C.sb
        X1 = sb("X1", [128, 16, 1024], F32)
        H = sb("H", [128, 16, 1024], BF16)
        WA = sb("WA", [128, 12288], BF16)
        WB = sb("WB", [128, 12288], BF16)
        AR = [WA, WB]
        HID = sb("HID", [128, 2, 2, 1024], BF16)
        ETF = sb("ETF", [128, 4096], F32)
        ET = ETF[:, :].rearrange("p (m t) -> p m t", m=16)
        SQ = sb("SQ", [128, 2, 512], F32)
        RS = sb("RS", [128, 512], F32)
        T1 = sb("T1", [128, 2, 512], F32)
        SL = sb("SL", [128, 2, 512], F32)
        ONESF = sb("ONESF", [128, 128], F32)
        IDF = sb("IDF", [128, 128], F32)
        GEXP = sb("GEXP", [128, 2, 128], F32)
        G_FFN = sb("G_FFN", [128, 16], F32)
        G_PIN = sb("G_PIN", [128, 16], F32)
        G_POUT = sb("G_POUT", [128, 16], F32)
        WR = sb("WR", [128, 16, 36], BF16)
        BR = sb("BR", [128, 36], F32)
        GATES = sb("GATES", [128, 8, 32], F32)
        LG = sb("LG", [128, 36], F32)
        ME = sb("ME", [128, 32], F32)
        EX = sb("EX", [128, 32], F32)
        SEL = sb("SEL", [128, 32], F32)
        SM = sb("SM", [128, 16], F32)
        M8 = sb("M8", [128, 8], F32)
        PT = sb("PT", [128, 2, 256], BF16)
        if mode == 'sg':
            G_MIX = sb("G_MIX", [128, 16], F32)
            VG = sb("VG", [128, 16], F32)
            WCT = sb("WCT", [128, 16, 128], BF16)
            TRIB = sb("TRIB", [128, 128], BF16)
            BS = sb("BS", [128, 16, 128], F32)
            UT = sb("UT", [128, 512], F32)
            MX = sb("MX", [128, 512], F32)
            SS = sb("SS", [128, 16], F32)
            RV = sb("RV", [128, 4], F32)
            GTD = ETF[:, :].bitcast(BF16).rearrange("p (m t) -> p m t", m=16)
        PB = [es.enter_context(nc.psum_tensor("pb%d" % i, [128, 512], F32)) for i in range(8)]
        pk = ['p%d' % i for i in range(8)]

        C.DMA('sp', ONESF[:], ones_d, [], ['ONESF'])
        C.DMA('sp', IDF[:], ident_d, [], ['IDF'])
        C.DMA('sp', G_FFN[:], g_ffn_d, [], ['G_FFN'])
        C.DMA('sp', G_PIN[:], g_pin_d, [], ['G_PIN'])
        C.DMA('sp', G_POUT[:], g_pout_d, [], ['G_POUT'])
        C.DMA('sp', BR[:], br_d, [], ['BR'])
        C.DMA('pool', WR[:], wr_v, [], ['WR'])
        if mode == 'sg':
            C.DMA('sp', G_MIX[:], g_mix_d, [], ['G_MIX'])
            C.DMA('sp', VG[:], vg_d, [], ['VG'])
            C.DMA('sp', BS[:], bs_d, [], ['BS'])
            C.DMA('pool', WCT[:], wsT_d, [], ['WCT'])
            C.DMA('pool', TRIB[:], tri_d, [], ['TRIB'])
            for g in range(16):
                C.TT('pool', WCT[:, g, :], WCT[:, g, :], TRIB[:], MUL, ['WCT', 'TRIB'], ['WCT'])
            C.OP('dve', lambda e: e.memset(SS[:], 0.0), [], ['SS'])
        C.OP('dve', lambda e: e.memset(SM[:], 0.0), [], ['SM'])

        arena_i = [0]

        def next_arena():
            a = arena_i[0] % 2
            arena_i[0] += 1
            return AR[a], 'AR%d' % a

        def rms_fm(src, skey, g_tile, gkey, dst, dkey, W, bank, bkey):
            for c in range(16):
                sqb = SQ[:, c % 2, 0:W]
                C.ACT(sqb, src(c), AF.Square, [skey(c)], [('SQ', c % 2)])
                C.MM(bank[:, 0:W], ONESF[:], sqb, c == 0, c == 15, [('SQ', c % 2), 'ONESF'], [bkey])
            C.ACT(RS[:, 0:W], bank[:, 0:W], AF.Sqrt, [bkey], ['RS'], scale=1.0 / D, bias=EPS)
            C.OP('dve', lambda e: e.reciprocal(RS[:, 0:W], RS[:, 0:W]), ['RS'], ['RS'])
            for c in range(16):
                C.STT(dst(c), src(c), g_tile[:, c:c + 1], RS[:, 0:W], MUL, MUL, [skey(c), 'RS', gkey], [dkey(c)])

        gelu_i = [0]

        def gelu(bank, bkey, W, out, okeys, sscol=None):
            k = gelu_i[0] % 2
            gelu_i[0] += 1
            t = T1[:, k, 0:W]
            tk = ('T1', k)
            C.ACT(t, bank, AF.Square, [bkey], [tk])
            C.TS('dve', t, t, 0.044715, 1.0, MUL, ADD, [tk], [tk])
            C.TT('dve', t, t, bank, MUL, [tk, bkey], [tk])
            C.ACT(t, t, AF.Sigmoid, [tk], [tk], scale=1.5957691216057308)
            C.TT('dve', out, t, bank, MUL, [tk, bkey], okeys)
            if sscol is not None:
                C.ACT(t, out, AF.Square, okeys, [tk, 'SS'], accum_out=sscol)

        def wst_view(ar, n):
            return ar[:, 0:16 * n].rearrange("p (c f) -> p c f", c=16)

        for p in range(2):
            for b in range(2):
                g0 = p * 1024 + b * 512
                bl = slice(b * 512, (b + 1) * 512)
                ol = slice((1 - b) * 512, (2 - b) * 512)
                xk = [('X1', b, c) for c in range(16)]
                hk = [('H', b, c) for c in range(16)]
                ok = [('H', 1 - b, c) for c in range(16)]
                C.DMA('sp', X1[:, :, bl], xT_v[:, :, g0:g0 + 512], [], xk)
                if mode == 'attn':
                    C.DMA('pool', H[:, :, ol], oT.rearrange("(c q) t -> q c t", q=128)[:, :, g0:g0 + 512], [], ok)
                    src_keys = ok
                    src = lambda c: H[:, c, ol]
                else:
                    rms_fm(lambda c: X1[:, c, bl], lambda c: ('X1', b, c), G_MIX, 'G_MIX',
                           lambda c: H[:, c, bl], lambda c: ('H', b, c), 512, PB[7], pk[7])
                    C.OP('dve', lambda e: e.memset(SS[:], 0.0), [], ['SS'])
                    for j in range(4):
                        ar, ak = next_arena()
                        wst = wst_view(ar, 512)
                        C.DMA('pool', wst, w_in.rearrange("(c q) n -> q c n", q=128)[:, :, 2048 + j * 512:2048 + (j + 1) * 512], [], [ak, ak + 'u'])
                        for s in range(4):
                            bi = (j * 4 + s) % 2
                            for c in range(16):
                                C.MM(PB[bi][:], H[:, c, b * 512 + s * 128:b * 512 + (s + 1) * 128], wst[:, c, :],
                                     c == 0, c == 15, [('H', b, c), ak, ak + 'u'], [pk[bi]])
                            gelu(PB[bi][:], pk[bi], 512, H[:, s * 4 + j, ol], [('H', 1 - b, s * 4 + j)], sscol=SS[:, s * 4 + j:s * 4 + j + 1])
                    C.OP('dve', lambda e: e.tensor_reduce(out=RV[:], in_=SS[:].rearrange("p (s j) -> p s j", j=4), axis=AX.X, op=ADD), ['SS'], ['RV'])
                    C.ACT(RV[:], RV[:], AF.Sqrt, ['RV'], ['RV'], scale=1.0 / 2048, bias=EPS)
                    C.OP('dve', lambda e: e.reciprocal(RV[:], RV[:]), ['RV'], ['RV'])
                    for s in range(4):
                        for j in range(4):
                            vk = ('H', 1 - b, s * 4 + j)
                            C.TS('dve', H[:, s * 4 + j, ol], H[:, s * 4 + j, ol], RV[:, s:s + 1], None, MUL, None, [vk, 'RV'], [vk])
                    for mg in range(4):
                        ar, ak = next_arena()
                        wst = wst_view(ar, 512)
                        C.DMA('pool', wst, w_in.rearrange("(c q) n -> q c n", q=128)[:, :, mg * 512:(mg + 1) * 512], [], [ak, ak + 'u'])
                        for mi in range(4):
                            m = mg * 4 + mi
                            bu = 2 + (m % 2) * 2
                            bm = 3 + (m % 2) * 2
                            for c in range(16):
                                C.MM(PB[bu][:], wst[:, c, mi * 128:(mi + 1) * 128], H[:, c, bl], c == 0, c == 15,
                                     [ak, ak + 'u', ('H', b, c)], [pk[bu]])
                            for s in range(4):
                                vk = ('H', 1 - b, s * 4 + m // 4)
                                o0 = (1 - b) * 512 + (m % 4) * 128
                                C.MM(PB[bm][:, s * 128:(s + 1) * 128], H[:, s * 4 + m // 4, o0:o0 + 128], WCT[:, m, :], True, True,
                                     [vk, 'WCT'], [pk[bm]])
                            gelu(PB[bu][:], pk[bu], 512, UT[:], ['UT'])
                            for s in range(4):
                                C.STT(MX[:, s * 128:(s + 1) * 128], PB[bm][:, s * 128:(s + 1) * 128], VG[:, m:m + 1], BS[:, m, :], MUL, ADD,
                                      [pk[bm], 'VG', 'BS'], ['MX'])
                            C.TT('dve', GTD[:, m, :], UT[:], MX[:], MUL, ['UT', 'MX'], [('GTD', m)])
                    src_keys = [('GTD', c) for c in range(16)]
                    src = lambda c: GTD[:, c, :]
                for mg in range(4):
                    ar, ak = next_arena()
                    wst = wst_view(ar, 512)
                    C.DMA('pool', wst, wmo_v[:, :, mg * 512:(mg + 1) * 512], [], [ak, ak + 'u'])
                    for mi in range(4):
                        m = mg * 4 + mi
                        bi = m % 2
                        for c in range(16):
                            C.MM(PB[bi][:], wst[:, c, mi * 128:(mi + 1) * 128], src(c), c == 0, c == 15, [ak, ak + 'u', src_keys[c]], [pk[bi]])
                        C.TT('dve', X1[:, m, bl], X1[:, m, bl], PB[bi][:], ADD, [pk[bi], ('X1', b, m)], [('X1', b, m)])
            for b in range(2):
                bl = slice(b * 512, (b + 1) * 512)
                rms_fm(lambda c: X1[:, c, bl], lambda c: ('X1', b, c), G_FFN, 'G_FFN',
                       lambda c: H[:, c, bl], lambda c: ('H', b, c), 512, PB[7], pk[7])
            for sub in range(8):
                b = sub // 4
                for c in range(16):
                    C.MM(PB[6][:, 0:36], H[:, c, sub * 128:(sub + 1) * 128], WR[:, c, :], c == 0, c == 15, [('H', b, c), 'WR'], [pk[6]])
                C.TT('dve', LG[:], PB[6][:, 0:36], BR[:], ADD, [pk[6], 'BR'], ['LG'])
                C.OP('dve', lambda e: e.tensor_reduce(out=SM[:, 0:1], in_=LG[:, 0:4], axis=AX.X, op=ALU.max), ['LG'], ['SM'])
                C.TS('dve', SM[:, 1:2], SM[:, 0:1], -1.0, None, MUL, None, ['SM'], ['SM'])
                C.OP('dve', lambda e: e.memset(SM[:, 2:3], 0.0), [], ['SM'])
                C.ACT(EX[:, 0:4], LG[:, 0:4], AF.Exp, ['LG', 'SM'], ['EX', 'SM'], bias=SM[:, 1:2], accum_out=SM[:, 2:3])
                C.TS('dve', SEL[:, 0:4], LG[:, 0:4], SM[:, 0:1], None, ALU.is_equal, None, ['LG', 'SM'], ['SEL'])
                C.TS('dve', SEL[:, 0:4], SEL[:, 0:4], -1.0, 1.0e4, ADD, MUL, ['SEL'], ['SEL'])
                for g in range(4):
                    C.TS('dve', ME[:, g * 8:(g + 1) * 8], LG[:, 4 + g * 8:4 + (g + 1) * 8], SEL[:, g:g + 1], None, ADD, None, ['LG', 'SEL'], ['ME'])
                C.OP('dve', lambda e: e.max(out=M8[:], in_=ME[:]), ['ME'], ['M8'])
                C.TS('dve', SM[:, 3:4], M8[:, 0:1], -1.0, None, MUL, None, ['M8'], ['SM'])
                C.ACT(EX[:], ME[:], AF.Exp, ['ME', 'SM'], ['EX'], bias=SM[:, 3:4])
                C.TS('dve', SEL[:], ME[:], M8[:, 1:2], None, ALU.is_ge, None, ['ME', 'M8'], ['SEL'])
                C.TT('dve', EX[:], EX[:], SEL[:], MUL, ['EX', 'SEL'], ['EX'])
                C.OP('dve', lambda e: e.tensor_reduce(out=SM[:, 4:5], in_=EX[:], axis=AX.X, op=ADD), ['EX'], ['SM'])
                C.TT('dve', SM[:, 5:6], SM[:, 4:5], SM[:, 2:3], MUL, ['SM'], ['SM'])
                C.OP('dve', lambda e: e.reciprocal(SM[:, 6:7], SM[:, 5:6]), ['SM'], ['SM'])
                C.TS('dve', GATES[:, sub, :], EX[:], SM[:, 6:7], None, MUL, None, ['EX', 'SM'], [('GATES', sub)])
            for ex in range(32):
                ar, ak = next_arena()
                Wg = ar[:, 0:4096].rearrange("p (c f) -> p c f", c=16)
                Wu = ar[:, 4096:8192].rearrange("p (c f) -> p c f", c=16)
                Wd = ar[:, 8192:12288].rearrange("p (c f) -> p c f", c=2)
                C.DMA('pool', Wg, wg_d[ex].rearrange("(c q) f -> q c f", q=128), [], [ak])
                C.DMA('pool', Wu, wu_d[ex].rearrange("(c q) f -> q c f", q=128), [], [ak + 'u'])
                C.DMA('pool', Wd, wd_d[ex].rearrange("(c q) f -> q c f", q=128), [], [ak + 'd'])
                hp = ex % 2
                for sub in range(8):
                    gk = ('GEXP', sub % 2)
                    C.TS('dve', GEXP[:, sub % 2, :], ONESF[:], GATES[:, sub, ex:ex + 1], None, MUL, None, ['ONESF', ('GATES', sub)], [gk])
                    C.MM(PB[6 + sub // 4][:, (sub % 4) * 128:(sub % 4 + 1) * 128], GEXP[:, sub % 2, :], IDF[:], True, True,
                         [gk, 'IDF'], [pk[6 + sub // 4]])
                for b in range(2):
                    bl = slice(b * 512, (b + 1) * 512)
                    for j in range(2):
                        k = (b * 2 + j) % 2
                        for c in range(16):
                            C.MM(PB[2 * k][:], Wg[:, c, j * 128:(j + 1) * 128], H[:, c, bl], c == 0, c == 15, [ak, ('H', b, c)], [pk[2 * k]])
                        for c in range(16):
                            C.MM(PB[2 * k + 1][:], Wu[:, c, j * 128:(j + 1) * 128], H[:, c, bl], c == 0, c == 15, [ak + 'u', ('H', b, c)], [pk[2 * k + 1]])
                        slk = ('SL', k)
                        C.ACT(SL[:, k, :], PB[2 * k][:], AF.Silu, [pk[2 * k]], [slk])
                        C.TT('dve', SL[:, k, :], SL[:, k, :], PB[2 * k + 1][:], MUL, [slk, pk[2 * k + 1]], [slk])
                        C.TT('dve', HID[:, hp, j, bl], SL[:, k, :], PB[6 + b][:], MUL, [slk, pk[6 + b]], [('HID', hp, j, b)])
                for b in range(2):
                    bl = slice(b * 512, (b + 1) * 512)
                    for m in range(16):
                        bi = 4 + (m % 2)
                        for j in range(2):
                            C.MM(PB[bi][:], Wd[:, j, m * 128:(m + 1) * 128], HID[:, hp, j, bl], j == 0, j == 1,
                                 [ak + 'd', ('HID', hp, j, b)], [pk[bi]])
                        C.TT('dve', X1[:, m, bl], X1[:, m, bl], PB[bi][:], ADD, [pk[bi], ('X1', b, m)], [('X1', b, m)])
            WPP = WB[:, 8192:12288].rearrange("p (c f) -> p c f", c=2)
            C.DMA('pool', WPP, wpp_v, [], ['AR1d'])
            for sbk in range(4):
                b = sbk // 2
                g0 = p * 1024 + sbk * 256
                cl = slice(sbk * 256, (sbk + 1) * 256)
                C.DMA('pool', PT[:], pT_v[:, :, g0:g0 + 256], [], ['PT'])
                hkey = lambda c: ('H', b, c)
                rms_fm(lambda c: X1[:, c, cl], lambda c: ('X1', b, c), G_PIN, 'G_PIN',
                       lambda c: H[:, c, cl], hkey, 256, PB[7], pk[7])
                for mg in range(8):
                    wk = 'AR0' if mg % 2 == 0 else 'AR0u'
                    wst = WA[:, (mg % 2) * 4096:(mg % 2 + 1) * 4096].rearrange("p (c f) -> p c f", c=16)
                    C.DMA('pool', wst, wpg_v[:, :, mg * 256:(mg + 1) * 256], [], [wk])
                    for mi in range(2):
                        m = mg * 2 + mi
                        bg = (m % 2) * 2
                        bp = bg + 1
                        for c in range(16):
                            C.MM(PB[bg][:, 0:256], wst[:, c, mi * 128:(mi + 1) * 128], H[:, c, cl], c == 0, c == 15, [wk, hkey(c)], [pk[bg]])
                        for c in range(2):
                            C.MM(PB[bp][:, 0:256], WPP[:, c, m * 128:(m + 1) * 128], PT[:, c, :], c == 0, c == 1, ['AR1d', 'PT'], [pk[bp]])
                        tk = ('T1', m % 2)
                        C.ACT(T1[:, m % 2, 0:256], PB[bg][:, 0:256], AF.Sigmoid, [pk[bg]], [tk])
                        C.TT('dve', ET[:, m, :], T1[:, m % 2, 0:256], PB[bp][:, 0:256], MUL, [tk, pk[bp]], [('ET', m)])
                for m in range(16):
                    sqb = SQ[:, m % 2, 0:256]
                    C.ACT(sqb, ET[:, m, :], AF.Square, [('ET', m)], [('SQ', m % 2)])
                    C.MM(PB[4][:, 0:256], ONESF[:], sqb, m == 0, m == 15, [('SQ', m % 2), 'ONESF'], [pk[4]])
                C.ACT(RS[:, 0:256], PB[4][:, 0:256], AF.Sqrt, [pk[4]], ['RS'], scale=1.0 / D, bias=EPS)
                C.OP('dve', lambda e: e.reciprocal(RS[:, 0:256], RS[:, 0:256]), ['RS'], ['RS'])
                for m in range(16):
                    C.STT(ET[:, m, :], ET[:, m, :], G_POUT[:, m:m + 1], RS[:, 0:256], MUL, MUL, [('ET', m), 'RS', 'G_POUT'], [('ET', m)])
                    C.TT('dve', X1[:, m, cl], X1[:, m, cl], ET[:, m, :], ADD, [('ET', m), ('X1', b, m)], [('X1', b, m)])
                tok = C.DMA('sp', xo_v[:, :, g0:g0 + 256], X1[:, :, cl], [('X1', b, m) for m in range(16)], [])
                S.wait_all('sp', [tok])
        S.emit()
    return nc


def build_p1():
    nc = bass.Bass("TRN2", target_bir_lowering=False)

    def din(n, s):
        return nc.dram_tensor(n, s, F32, kind="ExternalInput").ap()
    xT = din("xT", [D, NT])
    w_in = din("w_in", [D, 3 * D])
    g_mix_d = din("g_mix", [128, 16])
    gq_d = din("gq", [128, 1])
    gk_d = din("gk", [128, 1])
    ones_d = din("ones", [128, 128])
    qT = nc.dram_tensor("qT", [D, NT], F32, kind="ExternalOutput").ap()
    kT = nc.dram_tensor("kT", [D, NT], F32, kind="ExternalOutput").ap()
    vo = nc.dram_tensor("v", [NT, D], F32, kind="ExternalOutput").ap()
    xT_v = xT.rearrange("(c q) t -> q c t", q=128)
    win_v = w_in.rearrange("(c q) n -> q c n", q=128)
    with ExitStack() as es:
        C = Ctx(nc, es)
        S = C.S
        sb = C.sb
        XB = sb("XB", [128, 16, 512], F32)
        HB = sb("HB", [128, 16, 512], BF16)
        WS = [sb("WS%d" % i, [128, 16, 512], BF16) for i in range(2)]
        SQ = sb("SQ", [128, 2, 512], F32)
        RS = sb("RS", [128, 2, 512], F32)
        OB = sb("OB", [128, 4, 512], F32)
        ONESF = sb("ONESF", [128, 128], F32)
        G_MIX = sb("G_MIX", [128, 16], F32)
        GQK = sb("GQK", [128, 2], F32)
        PB = [es.enter_context(nc.psum_tensor("pb%d" % i, [128, 512], F32)) for i in range(8)]
        pk = ['p%d' % i for i in range(8)]
        C.DMA('sp', ONESF[:], ones_d, [], ['ONESF'])
        C.DMA('sp', G_MIX[:], g_mix_d, [], ['G_MIX'])
        C.DMA('sp', GQK[:, 0:1], gq_d, [], ['GQK'])
        C.DMA('sp', GQK[:, 1:2], gk_d, [], ['GQK'])
        toks = []
        wi = 0
        oi = 0
        for blk in range(4):
            g0 = blk * 512
            C.DMA('sp', XB[:], xT_v[:, :, g0:g0 + 512], [], [('XB', c) for c in range(16)])
            for c in range(16):
                sqb = SQ[:, c % 2, :]
                C.ACT(sqb, XB[:, c, :], AF.Square, [('XB', c)], [('SQ', c % 2)])
                C.MM(PB[7][:], ONESF[:], sqb, c == 0, c == 15, [('SQ', c % 2), 'ONESF'], [pk[7]])
            C.ACT(RS[:, 0, :], PB[7][:], AF.Sqrt, [pk[7]], [('RS', 0)], scale=1.0 / D, bias=EPS)
            C.OP('dve', lambda e: e.reciprocal(RS[:, 0, :], RS[:, 0, :]), [('RS', 0)], [('RS', 0)])
            for c in range(16):
                C.STT(HB[:, c, :], XB[:, c, :], G_MIX[:, c:c + 1], RS[:, 0, :], MUL, MUL, [('XB', c), ('RS', 0), 'G_MIX'], [('HB', c)])
            for which in range(2):
                dst = qT if which == 0 else kT
                for mg in range(4):
                    ws = WS[wi % 2]
                    wk = 'WS%d' % (wi % 2)
                    wi += 1
                    C.DMA('pool', ws[:], win_v[:, :, which * 2048 + mg * 512:which * 2048 + (mg + 1) * 512], [], [wk])
                    for mi in range(4):
                        hd = mg * 4 + mi
                        bi = hd % 2
                        for c in range(16):
                            C.MM(PB[bi][:], ws[:, c, mi * 128:(mi + 1) * 128], HB[:, c, :], c == 0, c == 15, [wk, ('HB', c)], [pk[bi]])
                        sqb = SQ[:, bi, :]
                        C.ACT(sqb, PB[bi][:], AF.Square, [pk[bi]], [('SQ', bi)])
                        C.MM(PB[2 + bi][:], ONESF[:], sqb, True, True, [('SQ', bi), 'ONESF'], [pk[2 + bi]])
                        rk = ('RS', 1)
                        if which == 0:
                            C.ACT(RS[:, 1, :], PB[2 + bi][:], AF.Sqrt, [pk[2 + bi]], [rk], scale=1.0, bias=128.0 * EPS)
                        else:
                            C.ACT(RS[:, 1, :], PB[2 + bi][:], AF.Sqrt, [pk[2 + bi]], [rk], scale=1.0 / 128, bias=EPS)
                        C.OP('dve', lambda e: e.reciprocal(RS[:, 1, :], RS[:, 1, :]), [rk], [rk])
                        ok = ('OB', oi % 4)
                        ob = OB[:, oi % 4, :]
                        oi += 1
                        C.STT(ob, PB[bi][:], GQK[:, which:which + 1], RS[:, 1, :], MUL, MUL, [pk[bi], rk, 'GQK'], [ok])
                        toks.append(C.DMA('sp', dst[hd * 128:(hd + 1) * 128, g0:g0 + 512], ob, [ok], []))
            for j in range(4):
                ws = WS[wi % 2]
                wk = 'WS%d' % (wi % 2)
                wi += 1
                C.DMA('pool', ws[:], win_v[:, :, 4096 + j * 512:4096 + (j + 1) * 512], [], [wk])
                for s in range(4):
                    bi = 4 + (s % 2)
                    for c in range(16):
                        C.MM(PB[bi][:], HB[:, c, s * 128:(s + 1) * 128], ws[:, c, :], c == 0, c == 15, [wk, ('HB', c)], [pk[bi]])
                    ok = ('OB', oi % 4)
                    ob = OB[:, oi % 4, :]
                    oi += 1
                    C.ACT(ob, PB[bi][:], AF.Copy, [pk[bi]], [ok])
                    toks.append(C.DMA('sp', vo[g0 + s * 128:g0 + (s + 1) * 128, j * 512:(j + 1) * 512], ob, [ok], []))
        S.wait_all('sp', toks[-8:])
        S.emit()
    return nc


def build_p2(SEQ=8192, NP=4):
    nc = bass.Bass("TRN2", target_bir_lowering=False)

    def din(n, s):
        return nc.dram_tensor(n, s, F32, kind="ExternalInput").ap()
    q4 = din("qT4", [NP, 128, SEQ])
    k4 = din("kT4", [NP, 128, SEQ])
    v4 = din("v4", [NP, SEQ, 128])
    ntri_d = din("ntri", [128, 128])
    ones_d = din("ones", [128, 128])
    masks_d = din("masks", [128, 4, 512])
    o4 = nc.dram_tensor("oT4", [NP, 128, SEQ], F32, kind="ExternalOutput").ap()
    NKB = SEQ // 128
    NQB = SEQ // 512
    with ExitStack() as es:
        C = Ctx(nc, es)
        S = C.S
        sb = C.sb
        QT = [sb("QT%d" % i, [128, SEQ], BF16) for i in range(2)]
        KT = [sb("KT%d" % i, [128, SEQ], BF16) for i in range(2)]
        V = [sb("V%d" % i, [128, NKB, 128], BF16) for i in range(2)]
        NTRI = sb("NTRI", [128, 128], BF16)
        ONESB = sb("ONESB", [128, 128], BF16)
        MASK = sb("MASK", [128, 4, 512], BF16)
        EB = sb("EB", [128, 2, 512], F32)
        SPB = sb("SPB", [128, 2, 512], BF16)
        ARG = sb("ARG", [128, 2, 512], F32)
        WB = sb("WB", [128, 2, 512], BF16)
        R = sb("R", [128, 512], F32)
        OB = sb("OB", [128, 2, 512], F32)
        PB = [es.enter_context(nc.psum_tensor("pb%d" % i, [128, 512], F32)) for i in range(8)]
        pk = ['p%d' % i for i in range(8)]
        C.DMA('pool', NTRI[:], ntri_d, [], ['NTRI'])
        C.DMA('pool', ONESB[:], ones_d, [], ['ONESB'])
        C.DMA('pool', MASK[:], masks_d, [], ['MASK'])
        toks = []
        it = 0
        for pr in range(NP):
            d = pr % 2
            C.DMA('pool', QT[d][:], q4[pr], [], [('QT', d)])
            C.DMA('pool', KT[d][:], k4[pr], [], [('KT', d)])
            C.DMA('pool', V[d][:], v4[pr].rearrange("(kb q) e -> q kb e", q=128), [], [('V', d)])
            for qb in range(NQB):
                q0 = qb * 512
                ob_i = (pr * NQB + qb) % 2
                obank = 6 + ob_i
                C.OP('dve', lambda e: e.memset(R[:], 0.0), [], ['R'])
                kbs = list(range(4 * qb + 3, -1, -1))
                for n, kb in enumerate(kbs):
                    i2 = it % 2
                    it += 1
                    zb, ab, tb = i2, 2 + i2, 4 + i2
                    kslice = KT[d][:, kb * 128:(kb + 1) * 128]
                    qslice = QT[d][:, q0:q0 + 512]
                    diag = kb >= 4 * qb
                    C.MM(PB[zb][:], kslice, qslice, True, True, [('KT', d), ('QT', d)], [pk[zb]])
                    C.ACT(EB[:, i2, :], PB[zb][:], AF.Exp, [pk[zb]], [('EB', i2)])
                    C.ACT(SPB[:, i2, :], EB[:, i2, :], AF.Ln, [('EB', i2)], [('SPB', i2)], bias=1.0)
                    if diag:
                        C.TT('pool', SPB[:, i2, :], SPB[:, i2, :], MASK[:, kb - 4 * qb, :], MUL, [('SPB', i2), 'MASK'], [('SPB', i2)])
                    C.MM(PB[ab][:], kslice, qslice, True, False, [('KT', d), ('QT', d)], [pk[ab]])
                    C.MM(PB[ab][:], NTRI[:], SPB[:, i2, :], False, True, ['NTRI', ('SPB', i2)], [pk[ab]])
                    C.MM(PB[tb][:], ONESB[:], SPB[:, i2, :], True, True, ['ONESB', ('SPB', i2)], [pk[tb]])
                    C.TT('dve', ARG[:, i2, :], PB[ab][:], R[:], ALU.subtract, [pk[ab], 'R'], [('ARG', i2)])
                    C.ACT(WB[:, i2, :], ARG[:, i2, :], AF.Exp, [('ARG', i2)], [('WB', i2)])
                    if diag:
                        C.TT('pool', WB[:, i2, :], WB[:, i2, :], MASK[:, kb - 4 * qb, :], MUL, [('WB', i2), 'MASK'], [('WB', i2)])
                    C.MM(PB[obank][:], V[d][:, kb, :], WB[:, i2, :], n == 0, n == len(kbs) - 1, [('V', d), ('WB', i2)], [pk[obank]])
                    if n != len(kbs) - 1:
                        C.TT('dve', R[:], R[:], PB[tb][:], ADD, [pk[tb], 'R'], ['R'])
                C.ACT(OB[:, ob_i, :], PB[obank][:], AF.Copy, [pk[obank]], [('OB', ob_i)])
                toks.append(C.DMA('sp', o4[pr, :, q0:q0 + 512], OB[:, ob_i, :], [('OB', ob_i)], []))
        S.wait_all('sp', toks[-8:])
        S.emit()
    return nc


def fm(g):
    return np.ascontiguousarray(g.reshape(16, 128).T.astype(np.float32))

def consts():
    ident = np.eye(128, dtype=np.float32)
    ones = np.ones((128, 128), dtype=np.float32)
    return ident, ones

def tok_common(inp, i, xT_core, pT_core):
    ident, ones = consts()
    m = {
        "xT": xT_core,
        "g_ffn": fm(inp["norm_ffn"][i]),
        "wr": np.ascontiguousarray(np.concatenate([inp["moe_w_group"][i], inp["moe_w_expert"][i]], axis=1)),
        "br": np.ascontiguousarray(np.broadcast_to(np.concatenate([inp["moe_b_group"][i], inp["moe_b_expert"][i]])[None, :], (128, 36))),
        "wg": inp["moe_w_gate"][i], "wu": inp["moe_w_up"][i], "wd": inp["moe_w_down"][i],
        "g_pin": fm(inp["ple_norm_in"][i]), "w_pg": inp["ple_w_gate"][i], "w_pp": inp["ple_w_proj"][i],
        "g_pout": fm(inp["ple_norm_out"][i]), "pT": pT_core, "ident": ident, "ones": ones,
    }
    return m

def tok_sg(inp, i, xT_core, pT_core):
    j = i // 2
    m = tok_common(inp, i, xT_core, pT_core)
    s_idx = np.arange(128)
    m.update({
        "w_mo": inp["sg_w_out"][j], "sg_w_in": inp["sg_w_in"][j], "g_mix": fm(inp["norm_mix"][i]),
        "vg": fm(inp["sg_v_norm"][j]),
        "wsT": np.ascontiguousarray(np.transpose(inp["sg_w_s"][j], (2, 0, 1))),
        "trimask": (s_idx[:, None] <= s_idx[None, :]).astype(np.float32),
        "bs": np.ascontiguousarray(np.broadcast_to(inp["sg_b_s"][j][None], (128, 16, 128))),
    })
    return m

def tok_attn(inp, i, xT_core, pT_core, oT_core):
    j = i // 2
    m = tok_common(inp, i, xT_core, pT_core)
    m.update({"w_mo": inp["sb_w_out"][j], "oT": oT_core})
    return m

def p1_inputs(inp, i, xT_core):
    j = i // 2
    ident, ones = consts()
    return {"xT": xT_core, "w_in": inp["sb_w_in"][j], "g_mix": fm(inp["norm_mix"][i]),
            "gq": np.ascontiguousarray(inp["sb_q_norm"][j].reshape(128, 1)), "gk": np.ascontiguousarray(inp["sb_k_norm"][j].reshape(128, 1)),
            "ones": ones}

def p2_consts():
    k = np.arange(128)
    ntri = -(k[:, None] >= k[None, :]).astype(np.float32)
    q = np.arange(512)
    masks = np.stack([((k[:, None] + o * 128) < q[None, :]).astype(np.float32) for o in range(4)], axis=1)
    return {"ntri": ntri, "ones": np.ones((128, 128), np.float32), "masks": np.ascontiguousarray(masks)}


N_CORES = 8


def _run(nc, in_maps):
    res = run_bass_kernel_spmd(nc, in_maps, core_ids=list(range(N_CORES)))
    return res.results


def kernel(**inp):
    inp = {k: np.asarray(v) for k, v in inp.items()}
    x = np.ascontiguousarray(inp["x"], dtype=np.float32).reshape(16384, 2048)
    p = inp["p"].reshape(4, 16384, 256)
    xT = [np.ascontiguousarray(x[c * NT:(c + 1) * NT].T) for c in range(N_CORES)]
    for i in range(4):
        pT = [np.ascontiguousarray(p[i, c * NT:(c + 1) * NT].T) for c in range(N_CORES)]
        if i % 2 == 0:
            r1 = _run(build_p1(), [p1_inputs(inp, i, xT[c]) for c in range(N_CORES)])
            pc = p2_consts()
            maps = [None] * N_CORES
            for b in range(2):
                qb = np.concatenate([r1[b * 4 + k]["qT"].reshape(16, 128, NT) for k in range(4)], axis=2)
                kb = np.concatenate([r1[b * 4 + k]["kT"].reshape(16, 128, NT) for k in range(4)], axis=2)
                vb = np.concatenate([r1[b * 4 + k]["v"] for k in range(4)], axis=0).reshape(8192, 16, 128).transpose(1, 0, 2)
                for k in range(4):
                    hs = slice(k * 4, (k + 1) * 4)
                    m = {"qT4": np.ascontiguousarray(qb[hs]), "kT4": np.ascontiguousarray(kb[hs]), "v4": np.ascontiguousarray(vb[hs])}
                    m.update(pc)
                    maps[b * 4 + k] = m
            del r1
            r2 = _run(build_p2(), maps)
            oT = [None] * N_CORES
            for b in range(2):
                ob = np.concatenate([r2[b * 4 + k]["oT4"] for k in range(4)], axis=0).reshape(2048, 8192)
                for k in range(4):
                    oT[b * 4 + k] = np.ascontiguousarray(ob[:, k * NT:(k + 1) * NT])
            del r2, maps
            r3 = _run(build_tok('attn'), [tok_attn(inp, i, xT[c], pT[c], oT[c]) for c in range(N_CORES)])
        else:
            r3 = _run(build_tok('sg'), [tok_sg(inp, i, xT[c], pT[c]) for c in range(N_CORES)])
        xT = [np.ascontiguousarray(r3[c]["xo"]) for c in range(N_CORES)]
        del r3
    out = np.concatenate([xT[c].T for c in range(N_CORES)], axis=0).reshape(2, 8192, 2048)
    return np.ascontiguousarray(out, dtype=np.float32)
```

```python
import numpy as np
import concourse.bass as bass
import concourse.mybir as mybir
from concourse.bass_utils import run_bass_kernel_spmd
from contextlib import ExitStack

F32 = mybir.dt.float32
BF16 = mybir.dt.bfloat16
AF = mybir.ActivationFunctionType
ALU = mybir.AluOpType
AX = mybir.AxisListType

ENGS = ['pe', 'act', 'dve', 'pool', 'sp']
DMAQ = ('sp', 'act', 'pool')


class Sched:
    def __init__(self, nc, es, R=4):
        self.nc = nc
        self.sem = {e: es.enter_context(nc.semaphore('s_' + e)) for e in ENGS}
        self.cnt = {e: 0 for e in ENGS}
        self.ops = {e: [] for e in ENGS}
        self.waited = {e: {} for e in ENGS}
        self.lastw = {}
        self.readers = {}
        self.R = R
        self.dsem = {q: [es.enter_context(nc.semaphore('d_%s%d' % (q, i))) for i in range(R)] for q in DMAQ}
        self.dcnt = {q: 0 for q in DMAQ}
        self.semobj = {}
        self.ccn = 0
        self.semobj[('cc',)] = es.enter_context(nc.semaphore('s_cc'))
        for e in ENGS:
            self.semobj[('c', e)] = self.sem[e]
        for q in DMAQ:
            for i in range(R):
                self.semobj[('d', q, i)] = self.dsem[q][i]

    def _deps(self, eng, reads, writes, skip_sem=None):
        deps = {}

        def add(tok):
            if tok is None:
                return
            s, v = tok
            if s == skip_sem:
                return
            if deps.get(s, 0) < v:
                deps[s] = v
        for k in reads:
            add(self.lastw.get(k))
        for k in writes:
            add(self.lastw.get(k))
            for s, v in self.readers.get(k, {}).items():
                add((s, v))
        out = []
        w = self.waited[eng]
        for s, v in deps.items():
            if w.get(s, 0) < v:
                w[s] = v
                out.append((s, v))
        return out

    def _commit(self, tok, reads, writes):
        s, v = tok
        for k in reads:
            r = self.readers.setdefault(k, {})
            if r.get(s, 0) < v:
                r[s] = v
        for k in writes:
            self.lastw[k] = tok
            self.readers[k] = {}

    def op(self, eng, fn, reads=(), writes=()):
        skip = ('c', 'pe') if eng == 'pe' else None
        waits = self._deps(eng, reads, writes, skip)
        self.cnt[eng] += 1
        tok = (('c', eng), self.cnt[eng])
        self.ops[eng].append((fn, waits, (('c', eng), 1)))
        self._commit(tok, reads, writes)
        return tok

    def dma(self, q, fn, reads=(), writes=()):
        i = self.dcnt[q]
        self.dcnt[q] += 1
        slot = i % self.R
        semk = ('d', q, slot)
        waits = self._deps(q, reads, writes)
        prev = 16 * (i // self.R)
        if prev > 0 and self.waited[q].get(semk, 0) < prev:
            self.waited[q][semk] = prev
            waits.append((semk, prev))
        tok = (semk, prev + 16)
        self.ops[q].append((fn, waits, (semk, 16)))
        self._commit(tok, reads, writes)
        return tok


    def cc(self, fn, reads=(), writes=()):
        waits = self._deps('pool', reads, writes)
        n = self.ccn
        if n > 0 and self.waited['pool'].get(('cc',), 0) < n:
            self.waited['pool'][('cc',)] = n
            waits.append((('cc',), n))
        self.ccn += 1
        tok = (('cc',), self.ccn)
        self.ops['pool'].append((fn, waits, (('cc',), 1)))
        self._commit(tok, reads, writes)
        return tok

    def barrier(self):
        latest = {}
        for e in ENGS:
            if self.cnt[e] > 0:
                latest[('c', e)] = self.cnt[e]
        for q in DMAQ:
            n = self.dcnt[q]
            for slot in range(self.R):
                k = (n - slot + self.R - 1) // self.R if n > slot else 0
                if k > 0:
                    latest[('d', q, slot)] = 16 * k
        if self.ccn > 0:
            latest[('cc',)] = self.ccn
        for e in ENGS:
            waits = []
            for s, v in latest.items():
                if self.waited[e].get(s, 0) < v:
                    self.waited[e][s] = v
                    waits.append((s, v))
            self.ops[e].append((None, waits, None))
        self.lastw.clear()
        self.readers.clear()

    def wait_all(self, eng, toks):
        waits = []
        for s, v in toks:
            if self.waited[eng].get(s, 0) < v:
                self.waited[eng][s] = v
                waits.append((s, v))
        self.ops[eng].append((None, waits, None))

    def emit(self):
        nc = self.nc
        S = self
        with nc.Block() as block:
            def run(e, name):
                for fn, waits, inc in S.ops[name]:
                    for s, v in waits:
                        e.wait_ge(S.semobj[s], v)
                    if fn is not None:
                        ins = fn(e)
                        if inc is not None:
                            ins.then_inc(S.semobj[inc[0]], inc[1])

            @block.sync
            def _(e):
                run(e, 'sp')

            @block.tensor
            def _(e):
                run(e, 'pe')

            @block.scalar
            def _(e):
                run(e, 'act')

            @block.vector
            def _(e):
                run(e, 'dve')

            @block.gpsimd
            def _(e):
                run(e, 'pool')


D = 2048
NT = 2048
EPS = 1e-6
MUL = ALU.mult
ADD = ALU.add
N_CORES = 8
GROUPS = [[0, 1, 2, 3], [4, 5, 6, 7]]


class Ctx:
    def __init__(self, nc, es):
        self.nc = nc
        self.es = es
        self.S = Sched(nc, es)

    def MM(self, out, lhsT, rhs, start, stop, r, w):
        self.S.op('pe', lambda e: e.matmul(out, lhsT, rhs, start=start, stop=stop), r, w)

    def ACT(self, out, in_, func, r, w, **kw):
        self.S.op('act', lambda e: e.activation(out=out, in_=in_, func=func, **kw), r, w)

    def TT(self, eng, out, a, b, op, r, w):
        self.S.op(eng, lambda e: e.tensor_tensor(out=out, in0=a, in1=b, op=op), r, w)

    def TS(self, eng, out, a, s1, s2, op0, op1, r, w):
        if s2 is None:
            self.S.op(eng, lambda e: e.tensor_scalar(out=out, in0=a, scalar1=s1, scalar2=None, op0=op0), r, w)
        else:
            self.S.op(eng, lambda e: e.tensor_scalar(out=out, in0=a, scalar1=s1, scalar2=s2, op0=op0, op1=op1), r, w)

    def STT(self, out, a, s, b, op0, op1, r, w):
        self.S.op('dve', lambda e: e.scalar_tensor_tensor(out=out, in0=a, scalar=s, in1=b, op0=op0, op1=op1), r, w)

    def DMA(self, q, out, in_, r, w):
        return self.S.dma(q, lambda e: e.dma_start(out=out, in_=in_), r, w)

    def DMAF(self, q, fn, r, w):
        return self.S.dma(q, fn, r, w)

    def OP(self, eng, fn, r, w):
        self.S.op(eng, fn, r, w)


class Arena:
    def __init__(self, big, words):
        self.big = big
        self.words = words
        self.off = 0

    def reset(self):
        self.off = 0

    def __call__(self, name, shape, dt):
        n = 1
        for s in shape[1:]:
            n *= s
        if dt == BF16:
            w = (n + 1) // 2
            ap = self.big[:, self.off:self.off + w].bitcast(BF16)
        else:
            w = n
            ap = self.big[:, self.off:self.off + w]
        self.off += (w + 15) // 16 * 16
        assert self.off <= self.words, (name, self.off, self.words)
        if len(shape) == 3:
            ap = ap.rearrange("p (a b) -> p a b", a=shape[1])
        elif len(shape) == 4:
            ap = ap.rearrange("p (a b c) -> p a b c", a=shape[1], b=shape[2])
        return ap


_RANK = {}


def get_rank(e):
    if 'r' not in _RANK:
        _RANK['r'] = e.snap(e.partition_id() % 4, min_val=0, max_val=3)
    return _RANK['r']


_VIEWS = {}


def dyn_view(e, key, mk):
    if key not in _VIEWS:
        _VIEWS[key] = mk(get_rank(e))
    return _VIEWS[key]


def fm_view(ap2d):
    return ap2d.rearrange("(c q) t -> q c t", q=128)


def emit_p1(C, sb, PB, pk, T, j, i, src):
    S = C.S
    xT_v = fm_view(src)
    win_v = T["sb_w_in"][j].rearrange("(c q) n -> q c n", q=128)
    QS, KS, VS = T["QS"][j], T["KS"][j], T["VS"][j]
    VS3 = VS.rearrange("(h a) (b e) -> h (a b) e", h=16, b=16, e=128)
    XB = sb("XB", [128, 16, 512], F32)
    HB = sb("HB", [128, 16, 512], BF16)
    WS = [sb("WS%d" % k, [128, 16, 512], BF16) for k in range(2)]
    SQ = sb("SQ", [128, 2, 512], F32)
    RS = sb("RS", [128, 2, 512], F32)
    OB = sb("OB", [128, 4, 512], BF16)
    ONESF = sb("ONESF", [128, 128], F32)
    G_MIX = sb("G_MIX", [128, 16], F32)
    GQK = sb("GQK", [128, 2], F32)
    C.DMA('sp', ONESF, T["ones"], [], ['ONESF'])
    C.DMA('sp', G_MIX, T["g_mix"][i], [], ['G_MIX'])
    C.DMA('sp', GQK[:, 0:1], T["gq"][j], [], ['GQK'])
    C.DMA('sp', GQK[:, 1:2], T["gk"][j], [], ['GQK'])
    wi = 0
    oi = 0
    for blk in range(4):
        g0 = blk * 512
        C.DMA('sp', XB, xT_v[:, :, g0:g0 + 512], [], [('XB', c) for c in range(16)])
        for c in range(16):
            sqb = SQ[:, c % 2, :]
            C.ACT(sqb, XB[:, c, :], AF.Square, [('XB', c)], [('SQ', c % 2)])
            C.MM(PB[7][:], ONESF, sqb, c == 0, c == 15, [('SQ', c % 2), 'ONESF'], [pk[7]])
        C.ACT(RS[:, 0, :], PB[7][:], AF.Sqrt, [pk[7]], [('RS', 0)], scale=1.0 / D, bias=EPS)
        C.OP('dve', lambda e: e.reciprocal(RS[:, 0, :], RS[:, 0, :]), [('RS', 0)], [('RS', 0)])
        for c in range(16):
            C.STT(HB[:, c, :], XB[:, c, :], G_MIX[:, c:c + 1], RS[:, 0, :], MUL, MUL, [('XB', c), ('RS', 0), 'G_MIX'], [('HB', c)])
        for which in range(2):
            dst = QS if which == 0 else KS
            dk = 'QS' if which == 0 else 'KS'
            for mg in range(4):
                ws = WS[wi % 2]
                wk = 'WS%d' % (wi % 2)
                wi += 1
                C.DMA('pool', ws, win_v[:, :, which * 2048 + mg * 512:which * 2048 + (mg + 1) * 512], [], [wk])
                for mi in range(4):
                    hd = mg * 4 + mi
                    bi = hd % 2
                    for c in range(16):
                        C.MM(PB[bi][:], ws[:, c, mi * 128:(mi + 1) * 128], HB[:, c, :], c == 0, c == 15, [wk, ('HB', c)], [pk[bi]])
                    sqb = SQ[:, bi, :]
                    C.ACT(sqb, PB[bi][:], AF.Square, [pk[bi]], [('SQ', bi)])
                    C.MM(PB[2 + bi][:], ONESF, sqb, True, True, [('SQ', bi), 'ONESF'], [pk[2 + bi]])
                    rk = ('RS', 1)
                    if which == 0:
                        C.ACT(RS[:, 1, :], PB[2 + bi][:], AF.Sqrt, [pk[2 + bi]], [rk], scale=1.0, bias=128.0 * EPS)
                    else:
                        C.ACT(RS[:, 1, :], PB[2 + bi][:], AF.Sqrt, [pk[2 + bi]], [rk], scale=1.0 / 128, bias=EPS)
                    C.OP('dve', lambda e: e.reciprocal(RS[:, 1, :], RS[:, 1, :]), [rk], [rk])
                    ok = ('OB', oi % 4)
                    ob = OB[:, oi % 4, :]
                    oi += 1
                    C.STT(ob, PB[bi][:], GQK[:, which:which + 1], RS[:, 1, :], MUL, MUL, [pk[bi], rk, 'GQK'], [ok])
                    C.DMA('sp', dst[hd * 128:(hd + 1) * 128, g0:g0 + 512], ob, [ok], [dk])
        for jv in range(4):
            ws = WS[wi % 2]
            wk = 'WS%d' % (wi % 2)
            wi += 1
            C.DMA('pool', ws, win_v[:, :, 4096 + jv * 512:4096 + (jv + 1) * 512], [], [wk])
            for s in range(4):
                bi = 4 + (s % 2)
                for c in range(16):
                    C.MM(PB[bi][:], HB[:, c, s * 128:(s + 1) * 128], ws[:, c, :], c == 0, c == 15, [wk, ('HB', c)], [pk[bi]])
                ok = ('OB', oi % 4)
                ob = OB[:, oi % 4, :]
                oi += 1
                C.ACT(ob, PB[bi][:], AF.Copy, [pk[bi]], [ok])
                t0 = g0 + s * 128
                C.DMA('sp', VS3[jv * 4:(jv + 1) * 4, t0:t0 + 128, :].rearrange("h t e -> t h e"),
                      ob.rearrange("p (h e) -> p h e", h=4), [ok], ['VS'])


def emit_p2(C, sb, PB, pk, T, j):
    S = C.S
    SEQ = 8192
    NKB = SEQ // 128
    NQB = SEQ // 512
    OS = T["OS"][j]
    QL, KL, VL = T["QL"], T["KL"], T["VL"]
    VL4 = VL.rearrange("(r pr a) (b e) -> r pr (a b) e", r=4, pr=4, b=16, e=128)
    QT = [sb("QT%d" % k, [128, SEQ], BF16) for k in range(2)]
    KT = [sb("KT%d" % k, [128, SEQ], BF16) for k in range(2)]
    V = [sb("V%d" % k, [128, NKB, 128], BF16) for k in range(2)]
    NTRI = sb("NTRI", [128, 128], BF16)
    ONESB = sb("ONESB", [128, 128], BF16)
    MASK = sb("MASK", [128, 4, 512], BF16)
    EB = sb("EB", [128, 2, 512], F32)
    SPB = sb("SPB", [128, 2, 512], BF16)
    ARG = sb("ARG", [128, 2, 512], F32)
    WB = sb("WB", [128, 2, 512], BF16)
    R = sb("R", [128, 512], F32)
    OB = sb("OB", [128, 2, 512], BF16)
    C.DMA('pool', NTRI, T["ntri"], [], ['NTRI'])
    C.DMA('pool', ONESB, T["ones"], [], ['ONESB'])
    C.DMA('pool', MASK, T["masks"], [], ['MASK'])
    it = 0
    for pr in range(4):
        d = pr % 2
        for r in range(4):
            C.DMA('pool', QT[d][:, r * 2048:(r + 1) * 2048], QL[r * 512 + pr * 128:r * 512 + (pr + 1) * 128, :], ['QL'], [('QT', d)])
            C.DMA('pool', KT[d][:, r * 2048:(r + 1) * 2048], KL[r * 512 + pr * 128:r * 512 + (pr + 1) * 128, :], ['KL'], [('KT', d)])
            C.DMA('pool', V[d][:, r * 16:(r + 1) * 16, :], VL4[r, pr].rearrange("(kb q) e -> q kb e", q=128), ['VL'], [('V', d)])
        for qb in range(NQB):
            q0 = qb * 512
            ob_i = (pr * NQB + qb) % 2
            obank = 6 + ob_i
            C.OP('dve', lambda e: e.memset(R, 0.0), [], ['R'])
            kbs = list(range(4 * qb + 3, -1, -1))
            for n, kb in enumerate(kbs):
                i2 = it % 2
                it += 1
                zb, ab, tb = i2, 2 + i2, 4 + i2
                kslice = KT[d][:, kb * 128:(kb + 1) * 128]
                qslice = QT[d][:, q0:q0 + 512]
                diag = kb >= 4 * qb
                C.MM(PB[zb][:], kslice, qslice, True, True, [('KT', d), ('QT', d)], [pk[zb]])
                C.ACT(EB[:, i2, :], PB[zb][:], AF.Exp, [pk[zb]], [('EB', i2)])
                C.ACT(SPB[:, i2, :], EB[:, i2, :], AF.Ln, [('EB', i2)], [('SPB', i2)], bias=1.0)
                if diag:
                    C.TT('pool', SPB[:, i2, :], SPB[:, i2, :], MASK[:, kb - 4 * qb, :], MUL, [('SPB', i2), 'MASK'], [('SPB', i2)])
                C.MM(PB[ab][:], kslice, qslice, True, False, [('KT', d), ('QT', d)], [pk[ab]])
                C.MM(PB[ab][:], NTRI, SPB[:, i2, :], False, True, ['NTRI', ('SPB', i2)], [pk[ab]])
                C.MM(PB[tb][:], ONESB, SPB[:, i2, :], True, True, ['ONESB', ('SPB', i2)], [pk[tb]])
                C.TT('dve', ARG[:, i2, :], PB[ab][:], R, ALU.subtract, [pk[ab], 'R'], [('ARG', i2)])
                C.ACT(WB[:, i2, :], ARG[:, i2, :], AF.Exp, [('ARG', i2)], [('WB', i2)])
                if diag:
                    C.TT('pool', WB[:, i2, :], WB[:, i2, :], MASK[:, kb - 4 * qb, :], MUL, [('WB', i2), 'MASK'], [('WB', i2)])
                C.MM(PB[obank][:], V[d][:, kb, :], WB[:, i2, :], n == 0, n == len(kbs) - 1, [('V', d), ('WB', i2)], [pk[obank]])
                if n != len(kbs) - 1:
                    C.TT('dve', R, R, PB[tb][:], ADD, [pk[tb], 'R'], ['R'])
            C.ACT(OB[:, ob_i, :], PB[obank][:], AF.Copy, [pk[obank]], [('OB', ob_i)])
            tq = q0 // 2048
            row = tq * 512 + pr * 128
            C.DMA('sp', OS[row:row + 128, (q0 % 2048):(q0 % 2048) + 512], OB[:, ob_i, :], [('OB', ob_i)], ['OS'])


def emit_tok(C, sb, PB, pk, T, mode, i, src, dst, final):
    S = C.S
    j = i // 2
    xT_v = fm_view(src)
    xo_v = fm_view(dst)
    w_mo = T["sb_w_out"][j] if mode == 'attn' else T["sg_w_out"][j]
    wmo_v = w_mo.rearrange("(c q) n -> q c n", q=128)
    wpg_v = T["ple_w_gate"][i].rearrange("(c q) n -> q c n", q=128)
    wpp_v = T["ple_w_proj"][i].rearrange("(c q) n -> q c n", q=128)
    pT_v = T["pT"][i].rearrange("(c q) t -> q c t", q=128)
    wr_v = T["wr"][i].rearrange("(c q) n -> q c n", q=128)
    wg_d, wu_d, wd_d = T["moe_w_gate"][i], T["moe_w_up"][i], T["moe_w_down"][i]
    if mode == 'sg':
        w_in_v = T["sg_w_in"][j].rearrange("(c q) n -> q c n", q=128)

    X1 = sb("X1", [128, 16, 1024], F32)
    H = sb("H", [128, 16, 1024], BF16)
    WA = sb("WA", [128, 12288], BF16)
    WB_ = sb("WB", [128, 12288], BF16)
    AR = [WA, WB_]
    HID = sb("HID", [128, 2, 2, 1024], BF16)
    ETF = sb("ETF", [128, 4096], F32)
    ET = ETF.rearrange("p (m t) -> p m t", m=16)
    SQ = sb("SQ", [128, 2, 512], F32)
    RS = sb("RS", [128, 512], F32)
    T1 = sb("T1", [128, 2, 512], F32)
    SL = sb("SL", [128, 2, 512], F32)
    ONESF = sb("ONESF", [128, 128], F32)
    IDF = sb("IDF", [128, 128], F32)
    GEXP = sb("GEXP", [128, 2, 128], F32)
    G_FFN = sb("G_FFN", [128, 16], F32)
    G_PIN = sb("G_PIN", [128, 16], F32)
    G_POUT = sb("G_POUT", [128, 16], F32)
    WR = sb("WR", [128, 16, 36], BF16)
    BR = sb("BR", [128, 36], F32)
    GATES = sb("GATES", [128, 8, 32], F32)
    LG = sb("LG", [128, 36], F32)
    ME = sb("ME", [128, 32], F32)
    EX = sb("EX", [128, 32], F32)
    SEL = sb("SEL", [128, 32], F32)
    SM = sb("SM", [128, 16], F32)
    M8 = sb("M8", [128, 8], F32)
    PT = sb("PT", [128, 2, 256], BF16)
    if mode == 'sg':
        G_MIX = sb("G_MIX", [128, 16], F32)
        VG = sb("VG", [128, 16], F32)
        WCT = sb("WCT", [128, 16, 128], BF16)
        TRIB = sb("TRIB", [128, 128], BF16)
        BS = sb("BS", [128, 16, 128], F32)
        UT = sb("UT", [128, 512], F32)
        MX = sb("MX", [128, 512], F32)
        SS = sb("SS", [128, 16], F32)
        RV = sb("RV", [128, 4], F32)
        GTD = ETF.bitcast(BF16).rearrange("p (m t) -> p m t", m=16)

    C.DMA('sp', ONESF, T["ones"], [], ['ONESF'])
    C.DMA('sp', IDF, T["ident"], [], ['IDF'])
    C.DMA('sp', G_FFN, T["g_ffn"][i], [], ['G_FFN'])
    C.DMA('sp', G_PIN, T["g_pin"][i], [], ['G_PIN'])
    C.DMA('sp', G_POUT, T["g_pout"][i], [], ['G_POUT'])
    C.DMA('sp', BR, T["br"][i], [], ['BR'])
    C.DMA('pool', WR, wr_v, [], ['WR'])
    if mode == 'sg':
        C.DMA('sp', G_MIX, T["g_mix"][i], [], ['G_MIX'])
        C.DMA('sp', VG, T["vg"][j], [], ['VG'])
        C.DMA('sp', BS, T["bs"][j], [], ['BS'])
        C.DMA('pool', WCT, T["wsT"][j], [], ['WCT'])
        C.DMA('pool', TRIB, T["trimask"], [], ['TRIB'])
        for g in range(16):
            C.TT('pool', WCT[:, g, :], WCT[:, g, :], TRIB, MUL, ['WCT', 'TRIB'], ['WCT'])
        C.OP('dve', lambda e: e.memset(SS, 0.0), [], ['SS'])
    C.OP('dve', lambda e: e.memset(SM, 0.0), [], ['SM'])

    arena_i = [0]

    def next_arena():
        a = arena_i[0] % 2
        arena_i[0] += 1
        return AR[a], 'AR%d' % a

    def rms_fm(src_, skey, g_tile, gkey, dst_, dkey, W, bank, bkey):
        for c in range(16):
            sqb = SQ[:, c % 2, 0:W]
            C.ACT(sqb, src_(c), AF.Square, [skey(c)], [('SQ', c % 2)])
            C.MM(bank[:, 0:W], ONESF, sqb, c == 0, c == 15, [('SQ', c % 2), 'ONESF'], [bkey])
        C.ACT(RS[:, 0:W], bank[:, 0:W], AF.Sqrt, [bkey], ['RS'], scale=1.0 / D, bias=EPS)
        C.OP('dve', lambda e: e.reciprocal(RS[:, 0:W], RS[:, 0:W]), ['RS'], ['RS'])
        for c in range(16):
            C.STT(dst_(c), src_(c), g_tile[:, c:c + 1], RS[:, 0:W], MUL, MUL, [skey(c), 'RS', gkey], [dkey(c)])

    gelu_i = [0]

    def gelu(bank, bkey, W, out, okeys, sscol=None):
        k = gelu_i[0] % 2
        gelu_i[0] += 1
        t = T1[:, k, 0:W]
        tk = ('T1', k)
        C.ACT(t, bank, AF.Square, [bkey], [tk])
        C.TS('dve', t, t, 0.044715, 1.0, MUL, ADD, [tk], [tk])
        C.TT('dve', t, t, bank, MUL, [tk, bkey], [tk])
        C.ACT(t, t, AF.Sigmoid, [tk], [tk], scale=1.5957691216057308)
        C.TT('dve', out, t, bank, MUL, [tk, bkey], okeys)
        if sscol is not None:
            C.ACT(t, out, AF.Square, okeys, [tk, 'SS'], accum_out=sscol)

    def wst_view(ar, n):
        return ar[:, 0:16 * n].rearrange("p (c f) -> p c f", c=16)

    out_toks = []
    for p in range(2):
        for b in range(2):
            g0 = p * 1024 + b * 512
            bl = slice(b * 512, (b + 1) * 512)
            ol = slice((1 - b) * 512, (2 - b) * 512)
            xk = [('X1', b, c) for c in range(16)]
            ok = [('H', 1 - b, c) for c in range(16)]
            C.DMA('sp', X1[:, :, bl], xT_v[:, :, g0:g0 + 512], [], xk)
            if mode == 'attn':
                C.DMA('pool', H[:, :, ol], fm_view(T["OL"])[:, :, g0:g0 + 512], ['OL'], ok)
                src_keys = ok
                srcf = lambda c: H[:, c, ol]
            else:
                rms_fm(lambda c: X1[:, c, bl], lambda c: ('X1', b, c), G_MIX, 'G_MIX',
                       lambda c: H[:, c, bl], lambda c: ('H', b, c), 512, PB[7], pk[7])
                C.OP('dve', lambda e: e.memset(SS, 0.0), [], ['SS'])
                for jv in range(4):
                    ar, ak = next_arena()
                    wst = wst_view(ar, 512)
                    C.DMA('pool', wst, w_in_v[:, :, 2048 + jv * 512:2048 + (jv + 1) * 512], [], [ak, ak + 'u'])
                    for s in range(4):
                        bi = (jv * 4 + s) % 2
                        for c in range(16):
                            C.MM(PB[bi][:], H[:, c, b * 512 + s * 128:b * 512 + (s + 1) * 128], wst[:, c, :],
                                 c == 0, c == 15, [('H', b, c), ak, ak + 'u'], [pk[bi]])
                        gelu(PB[bi][:], pk[bi], 512, H[:, s * 4 + jv, ol], [('H', 1 - b, s * 4 + jv)], sscol=SS[:, s * 4 + jv:s * 4 + jv + 1])
                C.OP('dve', lambda e: e.tensor_reduce(out=RV, in_=SS.rearrange("p (s j) -> p s j", j=4), axis=AX.X, op=ADD), ['SS'], ['RV'])
                C.ACT(RV, RV, AF.Sqrt, ['RV'], ['RV'], scale=1.0 / 2048, bias=EPS)
                C.OP('dve', lambda e: e.reciprocal(RV, RV), ['RV'], ['RV'])
                for s in range(4):
                    for jv in range(4):
                        vk = ('H', 1 - b, s * 4 + jv)
                        C.TS('dve', H[:, s * 4 + jv, ol], H[:, s * 4 + jv, ol], RV[:, s:s + 1], None, MUL, None, [vk, 'RV'], [vk])
                for mg in range(4):
                    ar, ak = next_arena()
                    wst = wst_view(ar, 512)
                    C.DMA('pool', wst, w_in_v[:, :, mg * 512:(mg + 1) * 512], [], [ak, ak + 'u'])
                    for mi in range(4):
                        m = mg * 4 + mi
                        bu = 2 + (m % 2) * 2
                        bm = 3 + (m % 2) * 2
                        for c in range(16):
                            C.MM(PB[bu][:], wst[:, c, mi * 128:(mi + 1) * 128], H[:, c, bl], c == 0, c == 15,
                                 [ak, ak + 'u', ('H', b, c)], [pk[bu]])
                        for s in range(4):
                            vk = ('H', 1 - b, s * 4 + m // 4)
                            o0 = (1 - b) * 512 + (m % 4) * 128
                            C.MM(PB[bm][:, s * 128:(s + 1) * 128], H[:, s * 4 + m // 4, o0:o0 + 128], WCT[:, m, :], True, True,
                                 [vk, 'WCT'], [pk[bm]])
                        gelu(PB[bu][:], pk[bu], 512, UT, ['UT'])
                        for s in range(4):
                            C.STT(MX[:, s * 128:(s + 1) * 128], PB[bm][:, s * 128:(s + 1) * 128], VG[:, m:m + 1], BS[:, m, :], MUL, ADD,
                                  [pk[bm], 'VG', 'BS'], ['MX'])
                        C.TT('dve', GTD[:, m, :], UT, MX, MUL, ['UT', 'MX'], [('GTD', m)])
                src_keys = [('GTD', c) for c in range(16)]
                srcf = lambda c: GTD[:, c, :]
            for mg in range(4):
                ar, ak = next_arena()
                wst = wst_view(ar, 512)
                C.DMA('pool', wst, wmo_v[:, :, mg * 512:(mg + 1) * 512], [], [ak, ak + 'u'])
                for mi in range(4):
                    m = mg * 4 + mi
                    bi = m % 2
                    for c in range(16):
                        C.MM(PB[bi][:], wst[:, c, mi * 128:(mi + 1) * 128], srcf(c), c == 0, c == 15, [ak, ak + 'u', src_keys[c]], [pk[bi]])
                    C.TT('dve', X1[:, m, bl], X1[:, m, bl], PB[bi][:], ADD, [pk[bi], ('X1', b, m)], [('X1', b, m)])
        for b in range(2):
            bl = slice(b * 512, (b + 1) * 512)
            rms_fm(lambda c: X1[:, c, bl], lambda c: ('X1', b, c), G_FFN, 'G_FFN',
                   lambda c: H[:, c, bl], lambda c: ('H', b, c), 512, PB[7], pk[7])
        for sub in range(8):
            b = sub // 4
            for c in range(16):
                C.MM(PB[6][:, 0:36], H[:, c, sub * 128:(sub + 1) * 128], WR[:, c, :], c == 0, c == 15, [('H', b, c), 'WR'], [pk[6]])
            C.TT('dve', LG, PB[6][:, 0:36], BR, ADD, [pk[6], 'BR'], ['LG'])
            C.OP('dve', lambda e: e.tensor_reduce(out=SM[:, 0:1], in_=LG[:, 0:4], axis=AX.X, op=ALU.max), ['LG'], ['SM'])
            C.TS('dve', SM[:, 1:2], SM[:, 0:1], -1.0, None, MUL, None, ['SM'], ['SM'])
            C.OP('dve', lambda e: e.memset(SM[:, 2:3], 0.0), [], ['SM'])
            C.ACT(EX[:, 0:4], LG[:, 0:4], AF.Exp, ['LG', 'SM'], ['EX', 'SM'], bias=SM[:, 1:2], accum_out=SM[:, 2:3])
            C.TS('dve', SEL[:, 0:4], LG[:, 0:4], SM[:, 0:1], None, ALU.is_equal, None, ['LG', 'SM'], ['SEL'])
            C.TS('dve', SEL[:, 0:4], SEL[:, 0:4], -1.0, 1.0e4, ADD, MUL, ['SEL'], ['SEL'])
            for g in range(4):
                C.TS('dve', ME[:, g * 8:(g + 1) * 8], LG[:, 4 + g * 8:4 + (g + 1) * 8], SEL[:, g:g + 1], None, ADD, None, ['LG', 'SEL'], ['ME'])
            C.OP('dve', lambda e: e.max(out=M8, in_=ME), ['ME'], ['M8'])
            C.TS('dve', SM[:, 3:4], M8[:, 0:1], -1.0, None, MUL, None, ['M8'], ['SM'])
            C.ACT(EX, ME, AF.Exp, ['ME', 'SM'], ['EX'], bias=SM[:, 3:4])
            C.TS('dve', SEL, ME, M8[:, 1:2], None, ALU.is_ge, None, ['ME', 'M8'], ['SEL'])
            C.TT('dve', EX, EX, SEL, MUL, ['EX', 'SEL'], ['EX'])
            C.OP('dve', lambda e: e.tensor_reduce(out=SM[:, 4:5], in_=EX, axis=AX.X, op=ADD), ['EX'], ['SM'])
            C.TT('dve', SM[:, 5:6], SM[:, 4:5], SM[:, 2:3], MUL, ['SM'], ['SM'])
            C.OP('dve', lambda e: e.reciprocal(SM[:, 6:7], SM[:, 5:6]), ['SM'], ['SM'])
            C.TS('dve', GATES[:, sub, :], EX, SM[:, 6:7], None, MUL, None, ['EX', 'SM'], [('GATES', sub)])
        for ex in range(32):
            ar, ak = next_arena()
            Wg = ar[:, 0:4096].rearrange("p (c f) -> p c f", c=16)
            Wu = ar[:, 4096:8192].rearrange("p (c f) -> p c f", c=16)
            Wd = ar[:, 8192:12288].rearrange("p (c f) -> p c f", c=2)
            C.DMA('pool', Wg, wg_d[ex].rearrange("(c q) f -> q c f", q=128), [], [ak])
            C.DMA('pool', Wu, wu_d[ex].rearrange("(c q) f -> q c f", q=128), [], [ak + 'u'])
            C.DMA('pool', Wd, wd_d[ex].rearrange("(c q) f -> q c f", q=128), [], [ak + 'd'])
            hp = ex % 2
            for sub in range(8):
                gk = ('GEXP', sub % 2)
                C.TS('dve', GEXP[:, sub % 2, :], ONESF, GATES[:, sub, ex:ex + 1], None, MUL, None, ['ONESF', ('GATES', sub)], [gk])
                C.MM(PB[6 + sub // 4][:, (sub % 4) * 128:(sub % 4 + 1) * 128], GEXP[:, sub % 2, :], IDF, True, True,
                     [gk, 'IDF'], [pk[6 + sub // 4]])
            for b in range(2):
                bl = slice(b * 512, (b + 1) * 512)
                for jf in range(2):
                    k = (b * 2 + jf) % 2
                    for c in range(16):
                        C.MM(PB[2 * k][:], Wg[:, c, jf * 128:(jf + 1) * 128], H[:, c, bl], c == 0, c == 15, [ak, ('H', b, c)], [pk[2 * k]])
                    for c in range(16):
                        C.MM(PB[2 * k + 1][:], Wu[:, c, jf * 128:(jf + 1) * 128], H[:, c, bl], c == 0, c == 15, [ak + 'u', ('H', b, c)], [pk[2 * k + 1]])
                    slk = ('SL', k)
                    C.ACT(SL[:, k, :], PB[2 * k][:], AF.Silu, [pk[2 * k]], [slk])
                    C.TT('dve', SL[:, k, :], SL[:, k, :], PB[2 * k + 1][:], MUL, [slk, pk[2 * k + 1]], [slk])
                    C.TT('dve', HID[:, hp, jf, bl], SL[:, k, :], PB[6 + b][:], MUL, [slk, pk[6 + b]], [('HID', hp, jf, b)])
            for b in range(2):
                bl = slice(b * 512, (b + 1) * 512)
                for m in range(16):
                    bi = 4 + (m % 2)
                    for jf in range(2):
                        C.MM(PB[bi][:], Wd[:, jf, m * 128:(m + 1) * 128], HID[:, hp, jf, bl], jf == 0, jf == 1,
                             [ak + 'd', ('HID', hp, jf, b)], [pk[bi]])
                    C.TT('dve', X1[:, m, bl], X1[:, m, bl], PB[bi][:], ADD, [pk[bi], ('X1', b, m)], [('X1', b, m)])
        WPP = WB_[:, 8192:12288].rearrange("p (c f) -> p c f", c=2)
        C.DMA('pool', WPP, wpp_v, [], ['AR1d'])
        for sbk in range(4):
            b = sbk // 2
            g0 = p * 1024 + sbk * 256
            cl = slice(sbk * 256, (sbk + 1) * 256)
            C.DMA('pool', PT, pT_v[:, :, g0:g0 + 256], [], ['PT'])
            hkey = lambda c: ('H', b, c)
            rms_fm(lambda c: X1[:, c, cl], lambda c: ('X1', b, c), G_PIN, 'G_PIN',
                   lambda c: H[:, c, cl], hkey, 256, PB[7], pk[7])
            for mg in range(8):
                wk = 'AR0' if mg % 2 == 0 else 'AR0u'
                wst = WA[:, (mg % 2) * 4096:(mg % 2 + 1) * 4096].rearrange("p (c f) -> p c f", c=16)
                C.DMA('pool', wst, wpg_v[:, :, mg * 256:(mg + 1) * 256], [], [wk])
                for mi in range(2):
                    m = mg * 2 + mi
                    bg = (m % 2) * 2
                    bp = bg + 1
                    for c in range(16):
                        C.MM(PB[bg][:, 0:256], wst[:, c, mi * 128:(mi + 1) * 128], H[:, c, cl], c == 0, c == 15, [wk, hkey(c)], [pk[bg]])
                    for c in range(2):
                        C.MM(PB[bp][:, 0:256], WPP[:, c, m * 128:(m + 1) * 128], PT[:, c, :], c == 0, c == 1, ['AR1d', 'PT'], [pk[bp]])
                    tk = ('T1', m % 2)
                    C.ACT(T1[:, m % 2, 0:256], PB[bg][:, 0:256], AF.Sigmoid, [pk[bg]], [tk])
                    C.TT('dve', ET[:, m, :], T1[:, m % 2, 0:256], PB[bp][:, 0:256], MUL, [tk, pk[bp]], [('ET', m)])
            for m in range(16):
                sqb = SQ[:, m % 2, 0:256]
                C.ACT(sqb, ET[:, m, :], AF.Square, [('ET', m)], [('SQ', m % 2)])
                C.MM(PB[4][:, 0:256], ONESF, sqb, m == 0, m == 15, [('SQ', m % 2), 'ONESF'], [pk[4]])
            C.ACT(RS[:, 0:256], PB[4][:, 0:256], AF.Sqrt, [pk[4]], ['RS'], scale=1.0 / D, bias=EPS)
            C.OP('dve', lambda e: e.reciprocal(RS[:, 0:256], RS[:, 0:256]), ['RS'], ['RS'])
            for m in range(16):
                C.STT(ET[:, m, :], ET[:, m, :], G_POUT[:, m:m + 1], RS[:, 0:256], MUL, MUL, [('ET', m), 'RS', 'G_POUT'], [('ET', m)])
                C.TT('dve', X1[:, m, cl], X1[:, m, cl], ET[:, m, :], ADD, [('ET', m), ('X1', b, m)], [('X1', b, m)])
            out_toks.append(C.DMA('sp', xo_v[:, :, g0:g0 + 256], X1[:, :, cl], [('X1', b, m) for m in range(16)], ['XDST']))
    if final:
        S.wait_all('sp', out_toks)


def build_fused():
    nc = bass.Bass("TRN2", target_bir_lowering=False)

    def din(n, s):
        return nc.dram_tensor(n, s, F32, kind="ExternalInput").ap()
    T = {}
    T["xT"] = din("xT", [D, NT])
    T["pT"] = din("pT", [4, 256, NT])
    T["g_mix"] = din("g_mix", [4, 128, 16])
    T["g_ffn"] = din("g_ffn", [4, 128, 16])
    T["g_pin"] = din("g_pin", [4, 128, 16])
    T["g_pout"] = din("g_pout", [4, 128, 16])
    T["gq"] = din("gq", [2, 128, 1])
    T["gk"] = din("gk", [2, 128, 1])
    T["vg"] = din("vg", [2, 128, 16])
    T["wsT"] = din("wsT", [2, 128, 16, 128])
    T["bs"] = din("bs", [2, 128, 16, 128])
    T["wr"] = din("wr", [4, D, 36])
    T["br"] = din("br", [4, 128, 36])
    T["sb_w_in"] = din("sb_w_in", [2, D, 3 * D])
    T["sb_w_out"] = din("sb_w_out", [2, D, D])
    T["sg_w_in"] = din("sg_w_in", [2, D, 4096])
    T["sg_w_out"] = din("sg_w_out", [2, D, D])
    T["moe_w_gate"] = din("moe_w_gate", [4, 32, D, 256])
    T["moe_w_up"] = din("moe_w_up", [4, 32, D, 256])
    T["moe_w_down"] = din("moe_w_down", [4, 32, 256, D])
    T["ple_w_gate"] = din("ple_w_gate", [4, D, D])
    T["ple_w_proj"] = din("ple_w_proj", [4, 256, D])
    T["ident"] = din("ident", [128, 128])
    T["ones"] = din("ones", [128, 128])
    T["trimask"] = din("trimask", [128, 128])
    T["ntri"] = din("ntri", [128, 128])
    T["masks"] = din("masks", [128, 4, 512])
    xo = nc.dram_tensor("xo", [D, NT], F32, kind="ExternalOutput").ap()
    XS = [nc.dram_tensor("XS%d" % k, [D, NT], F32).ap() for k in range(2)]
    for nm, shp in (("QS", [D, NT]), ("KS", [D, NT]), ("VS", [D, NT]), ("OS", [D, NT]),
                    ("QG", [8, 1024, NT]), ("KG", [8, 1024, NT]), ("VG", [8, 1024, NT]), ("OG", [8, 1024, NT])):
        T[nm] = [nc.dram_tensor("%s%d" % (nm, k), shp, BF16).ap() for k in range(2)]
    for nm in ("QL", "KL", "VL", "OL"):
        T[nm] = nc.dram_tensor(nm, [D, NT], BF16).ap()

    _RANK.clear()
    _VIEWS.clear()
    with ExitStack() as es:
        C = Ctx(nc, es)
        S = C.S
        S.ops['pool'].append((lambda e: (get_rank(e), None)[1], [], None))
        WORDS = 52992
        BIG = es.enter_context(nc.sbuf_tensor("BIG", [128, WORDS], F32))
        sb = Arena(BIG[:, :], WORDS)
        PB = [es.enter_context(nc.psum_tensor("pb%d" % k, [128, 512], F32)) for k in range(8)]
        pk = ['p%d' % k for k in range(8)]

        def gather(a, b, ka, kb_, loc, kl):
            for k in range(8):
                S.cc(lambda e, k=k: e.collective_compute("AllGather", ALU.bypass, replica_groups=GROUPS,
                                                         ins=[a[k * 256:(k + 1) * 256, :].opt()], outs=[b[k].opt()]), [ka], [kb_])

            def pick(e, two):
                mine = b.rearrange("(g two) (r xp) t -> g r two xp t", two=2, r=4)[bass.ds(get_rank(e), 1)].squeeze(0)
                return e.dma_start(out=loc.rearrange("(r two xp) t -> r two xp t", r=4, two=2)[:, two], in_=mine[:, two])
            for two in range(2):
                C.DMAF('pool', lambda e, two=two: pick(e, two), [kb_], [kl])

        for i in range(4):
            src = T["xT"] if i == 0 else XS[(i - 1) % 2]
            dst = xo if i == 3 else XS[i % 2]
            j = i // 2
            if i % 2 == 0:
                sb.reset()
                emit_p1(C, sb, PB, pk, T, j, i, src)
                S.barrier()
                gather(T["QS"][j], T["QG"][j], 'QS', 'QG', T["QL"], 'QL')
                gather(T["KS"][j], T["KG"][j], 'KS', 'KG', T["KL"], 'KL')
                gather(T["VS"][j], T["VG"][j], 'VS', 'VG', T["VL"], 'VL')
                sb.reset()
                emit_p2(C, sb, PB, pk, T, j)
                S.barrier()
                gather(T["OS"][j], T["OG"][j], 'OS', 'OG', T["OL"], 'OL')
                sb.reset()
                emit_tok(C, sb, PB, pk, T, 'attn', i, src, dst, False)
                S.barrier()
            else:
                sb.reset()
                emit_tok(C, sb, PB, pk, T, 'sg', i, src, dst, i == 3)
                S.barrier()
        S.emit()
    return nc


def fm(g):
    return np.ascontiguousarray(g.reshape(16, 128).T.astype(np.float32))


def kernel(**inp):
    inp = {k: np.asarray(v) for k, v in inp.items()}
    x = np.ascontiguousarray(inp["x"], dtype=np.float32).reshape(16384, 2048)
    p = inp["p"].reshape(4, 16384, 256)
    k = np.arange(128)
    q = np.arange(512)
    shared = {
        "g_mix": np.stack([fm(inp["norm_mix"][i]) for i in range(4)]),
        "g_ffn": np.stack([fm(inp["norm_ffn"][i]) for i in range(4)]),
        "g_pin": np.stack([fm(inp["ple_norm_in"][i]) for i in range(4)]),
        "g_pout": np.stack([fm(inp["ple_norm_out"][i]) for i in range(4)]),
        "gq": np.ascontiguousarray(inp["sb_q_norm"].reshape(2, 128, 1)),
        "gk": np.ascontiguousarray(inp["sb_k_norm"].reshape(2, 128, 1)),
        "vg": np.stack([fm(inp["sg_v_norm"][j]) for j in range(2)]),
        "wsT": np.ascontiguousarray(np.transpose(inp["sg_w_s"], (0, 3, 1, 2))),
        "bs": np.ascontiguousarray(np.broadcast_to(inp["sg_b_s"][:, None], (2, 128, 16, 128))),
        "wr": np.ascontiguousarray(np.concatenate([inp["moe_w_group"], inp["moe_w_expert"]], axis=2)),
        "br": np.ascontiguousarray(np.broadcast_to(np.concatenate([inp["moe_b_group"], inp["moe_b_expert"]], axis=1)[:, None, :], (4, 128, 36))),
        "sb_w_in": inp["sb_w_in"], "sb_w_out": inp["sb_w_out"], "sg_w_in": inp["sg_w_in"], "sg_w_out": inp["sg_w_out"],
        "moe_w_gate": inp["moe_w_gate"], "moe_w_up": inp["moe_w_up"], "moe_w_down": inp["moe_w_down"],
        "ple_w_gate": inp["ple_w_gate"], "ple_w_proj": inp["ple_w_proj"],
        "ident": np.eye(128, dtype=np.float32), "ones": np.ones((128, 128), np.float32),
        "trimask": (k[:, None] <= k[None, :]).astype(np.float32),
        "ntri": -(k[:, None] >= k[None, :]).astype(np.float32),
        "masks": np.ascontiguousarray(np.stack([((k[:, None] + o * 128) < q[None, :]).astype(np.float32) for o in range(4)], axis=1)),
    }
    maps = []
    for c in range(N_CORES):
        m = dict(shared)
        m["xT"] = np.ascontiguousarray(x[c * NT:(c + 1) * NT].T)
        m["pT"] = np.ascontiguousarray(np.transpose(p[:, c * NT:(c + 1) * NT, :], (0, 2, 1)))
        maps.append(m)
    nc = build_fused()
    res = run_bass_kernel_spmd(nc, maps, core_ids=list(range(N_CORES)))
    out = np.concatenate([res.results[c]["xo"].T for c in range(N_CORES)], axis=0).reshape(2, 8192, 2048)
    return np.ascontiguousarray(out, dtype=np.float32)
```

```python
import numpy as np
import concourse.bass as bass
import concourse.mybir as mybir
from concourse.bass_utils import run_bass_kernel_spmd
from contextlib import ExitStack

F32 = mybir.dt.float32
BF16 = mybir.dt.bfloat16
AF = mybir.ActivationFunctionType
ALU = mybir.AluOpType
AX = mybir.AxisListType

ENGS = ['pe', 'act', 'dve', 'pool', 'sp']
DMAQ = ('sp', 'act', 'pool')


class Sched:
    def __init__(self, nc, es, R=4):
        self.nc = nc
        self.sem = {e: es.enter_context(nc.semaphore('s_' + e)) for e in ENGS}
        self.cnt = {e: 0 for e in ENGS}
        self.ops = {e: [] for e in ENGS}
        self.waited = {e: {} for e in ENGS}
        self.lastw = {}
        self.readers = {}
        self.R = R
        self.dsem = {q: [es.enter_context(nc.semaphore('d_%s%d' % (q, i))) for i in range(R)] for q in DMAQ}
        self.dcnt = {q: 0 for q in DMAQ}
        self.semobj = {}
        self.ccn = 0
        self.semobj[('cc',)] = es.enter_context(nc.semaphore('s_cc'))
        for e in ENGS:
            self.semobj[('c', e)] = self.sem[e]
        for q in DMAQ:
            for i in range(R):
                self.semobj[('d', q, i)] = self.dsem[q][i]

    def _deps(self, eng, reads, writes, skip_sem=None):
        deps = {}

        def add(tok):
            if tok is None:
                return
            s, v = tok
            if s == skip_sem:
                return
            if deps.get(s, 0) < v:
                deps[s] = v
        for k in reads:
            add(self.lastw.get(k))
        for k in writes:
            add(self.lastw.get(k))
            for s, v in self.readers.get(k, {}).items():
                add((s, v))
        out = []
        w = self.waited[eng]
        for s, v in deps.items():
            if w.get(s, 0) < v:
                w[s] = v
                out.append((s, v))
        return out

    def _commit(self, tok, reads, writes):
        s, v = tok
        for k in reads:
            r = self.readers.setdefault(k, {})
            if r.get(s, 0) < v:
                r[s] = v
        for k in writes:
            self.lastw[k] = tok
            self.readers[k] = {}

    def op(self, eng, fn, reads=(), writes=()):
        skip = ('c', 'pe') if eng == 'pe' else None
        waits = self._deps(eng, reads, writes, skip)
        self.cnt[eng] += 1
        tok = (('c', eng), self.cnt[eng])
        self.ops[eng].append((fn, waits, (('c', eng), 1)))
        self._commit(tok, reads, writes)
        return tok

    def dma(self, q, fn, reads=(), writes=()):
        i = self.dcnt[q]
        self.dcnt[q] += 1
        slot = i % self.R
        semk = ('d', q, slot)
        waits = self._deps(q, reads, writes)
        prev = 16 * (i // self.R)
        if prev > 0 and self.waited[q].get(semk, 0) < prev:
            self.waited[q][semk] = prev
            waits.append((semk, prev))
        tok = (semk, prev + 16)
        self.ops[q].append((fn, waits, (semk, 16)))
        self._commit(tok, reads, writes)
        return tok


    def cc(self, fn, reads=(), writes=()):
        waits = self._deps('pool', reads, writes)
        n = self.ccn
        if n > 0 and self.waited['pool'].get(('cc',), 0) < n:
            self.waited['pool'][('cc',)] = n
            waits.append((('cc',), n))
        self.ccn += 1
        tok = (('cc',), self.ccn)
        self.ops['pool'].append((fn, waits, (('cc',), 1)))
        self._commit(tok, reads, writes)
        return tok

    def barrier(self):
        latest = {}
        for e in ENGS:
            if self.cnt[e] > 0:
                latest[('c', e)] = self.cnt[e]
        for q in DMAQ:
            n = self.dcnt[q]
            for slot in range(self.R):
                k = (n - slot + self.R - 1) // self.R if n > slot else 0
                if k > 0:
                    latest[('d', q, slot)] = 16 * k
        if self.ccn > 0:
            latest[('cc',)] = self.ccn
        for e in ENGS:
            waits = []
            for s, v in latest.items():
                if self.waited[e].get(s, 0) < v:
                    self.waited[e][s] = v
                    waits.append((s, v))
            self.ops[e].append((None, waits, None))
        self.lastw.clear()
        self.readers.clear()

    def wait_all(self, eng, toks):
        waits = []
        for s, v in toks:
            if self.waited[eng].get(s, 0) < v:
                self.waited[eng][s] = v
                waits.append((s, v))
        self.ops[eng].append((None, waits, None))

    def emit(self):
        nc = self.nc
        S = self
        with nc.Block() as block:
            def run(e, name):
                for fn, waits, inc in S.ops[name]:
                    for s, v in waits:
                        e.wait_ge(S.semobj[s], v)
                    if fn is not None:
                        ins = fn(e)
                        if inc is not None:
                            ins.then_inc(S.semobj[inc[0]], inc[1])

            @block.sync
            def _(e):
                run(e, 'sp')

            @block.tensor
            def _(e):
                run(e, 'pe')

            @block.scalar
            def _(e):
                run(e, 'act')

            @block.vector
            def _(e):
                run(e, 'dve')

            @block.gpsimd
            def _(e):
                run(e, 'pool')


D = 2048
NT = 2048
EPS = 1e-6
MUL = ALU.mult
ADD = ALU.add
N_CORES = 8
GROUPS = [[0, 1, 2, 3], [4, 5, 6, 7]]


class Ctx:
    def __init__(self, nc, es):
        self.nc = nc
        self.es = es
        self.S = Sched(nc, es)

    def MM(self, out, lhsT, rhs, start, stop, r, w):
        self.S.op('pe', lambda e: e.matmul(out, lhsT, rhs, start=start, stop=stop), r, w)

    def ACT(self, out, in_, func, r, w, **kw):
        self.S.op('act', lambda e: e.activation(out=out, in_=in_, func=func, **kw), r, w)

    def TT(self, eng, out, a, b, op, r, w):
        self.S.op(eng, lambda e: e.tensor_tensor(out=out, in0=a, in1=b, op=op), r, w)

    def TS(self, eng, out, a, s1, s2, op0, op1, r, w):
        if s2 is None:
            self.S.op(eng, lambda e: e.tensor_scalar(out=out, in0=a, scalar1=s1, scalar2=None, op0=op0), r, w)
        else:
            self.S.op(eng, lambda e: e.tensor_scalar(out=out, in0=a, scalar1=s1, scalar2=s2, op0=op0, op1=op1), r, w)

    def STT(self, out, a, s, b, op0, op1, r, w):
        self.S.op('dve', lambda e: e.scalar_tensor_tensor(out=out, in0=a, scalar=s, in1=b, op0=op0, op1=op1), r, w)

    def DMA(self, q, out, in_, r, w):
        return self.S.dma(q, lambda e: e.dma_start(out=out, in_=in_), r, w)

    def DMAF(self, q, fn, r, w):
        return self.S.dma(q, fn, r, w)

    def OP(self, eng, fn, r, w):
        self.S.op(eng, fn, r, w)


class Arena:
    def __init__(self, big, words):
        self.big = big
        self.words = words
        self.off = 0

    def reset(self):
        self.off = 0

    def __call__(self, name, shape, dt):
        n = 1
        for s in shape[1:]:
            n *= s
        if dt == BF16:
            w = (n + 1) // 2
            ap = self.big[:, self.off:self.off + w].bitcast(BF16)
        else:
            w = n
            ap = self.big[:, self.off:self.off + w]
        self.off += (w + 15) // 16 * 16
        assert self.off <= self.words, (name, self.off, self.words)
        if len(shape) == 3:
            ap = ap.rearrange("p (a b) -> p a b", a=shape[1])
        elif len(shape) == 4:
            ap = ap.rearrange("p (a b c) -> p a b c", a=shape[1], b=shape[2])
        return ap


_RANK = {}


def get_rank(e):
    if 'r' not in _RANK:
        _RANK['r'] = e.snap(e.partition_id() % 4, min_val=0, max_val=3)
    return _RANK['r']


_VIEWS = {}


def dyn_view(e, key, mk):
    if key not in _VIEWS:
        _VIEWS[key] = mk(get_rank(e))
    return _VIEWS[key]


def fm_view(ap2d):
    return ap2d.rearrange("(c q) t -> q c t", q=128)


def emit_p1(C, sb, PB, pk, T, j, i, src):
    S = C.S
    xT_v = fm_view(src)
    win_v = T["sb_w_in"][j].rearrange("(c q) n -> q c n", q=128)
    QS, KS, VS = T["QS"][j], T["KS"][j], T["VS"][j]
    VS3 = VS.rearrange("(h a) (b e) -> h (a b) e", h=16, b=16, e=128)
    XB = sb("XB", [128, 16, 512], F32)
    HB = sb("HB", [128, 16, 512], BF16)
    WS = [sb("WS%d" % k, [128, 16, 512], BF16) for k in range(2)]
    SQ = sb("SQ", [128, 2, 512], F32)
    RS = sb("RS", [128, 2, 512], F32)
    OB = sb("OB", [128, 4, 512], BF16)
    ONESF = sb("ONESF", [128, 128], F32)
    G_MIX = sb("G_MIX", [128, 16], F32)
    GQK = sb("GQK", [128, 2], F32)
    C.DMA('sp', ONESF, T["ones"], [], ['ONESF'])
    C.DMA('sp', G_MIX, T["g_mix"][i], [], ['G_MIX'])
    C.DMA('sp', GQK[:, 0:1], T["gq"][j], [], ['GQK'])
    C.DMA('sp', GQK[:, 1:2], T["gk"][j], [], ['GQK'])
    wi = 0
    oi = 0
    for blk in range(4):
        g0 = blk * 512
        C.DMA('sp', XB, xT_v[:, :, g0:g0 + 512], [], [('XB', c) for c in range(16)])
        for c in range(16):
            sqb = SQ[:, c % 2, :]
            C.ACT(sqb, XB[:, c, :], AF.Square, [('XB', c)], [('SQ', c % 2)])
            C.MM(PB[7][:], ONESF, sqb, c == 0, c == 15, [('SQ', c % 2), 'ONESF'], [pk[7]])
        C.ACT(RS[:, 0, :], PB[7][:], AF.Sqrt, [pk[7]], [('RS', 0)], scale=1.0 / D, bias=EPS)
        C.OP('dve', lambda e: e.reciprocal(RS[:, 0, :], RS[:, 0, :]), [('RS', 0)], [('RS', 0)])
        for c in range(16):
            C.STT(HB[:, c, :], XB[:, c, :], G_MIX[:, c:c + 1], RS[:, 0, :], MUL, MUL, [('XB', c), ('RS', 0), 'G_MIX'], [('HB', c)])
        for which in range(2):
            dst = QS if which == 0 else KS
            dk = 'QS' if which == 0 else 'KS'
            for mg in range(4):
                ws = WS[wi % 2]
                wk = 'WS%d' % (wi % 2)
                wi += 1
                C.DMA('pool', ws, win_v[:, :, which * 2048 + mg * 512:which * 2048 + (mg + 1) * 512], [], [wk])
                for mi in range(4):
                    hd = mg * 4 + mi
                    bi = hd % 2
                    for c in range(16):
                        C.MM(PB[bi][:], ws[:, c, mi * 128:(mi + 1) * 128], HB[:, c, :], c == 0, c == 15, [wk, ('HB', c)], [pk[bi]])
                    sqb = SQ[:, bi, :]
                    C.ACT(sqb, PB[bi][:], AF.Square, [pk[bi]], [('SQ', bi)])
                    C.MM(PB[2 + bi][:], ONESF, sqb, True, True, [('SQ', bi), 'ONESF'], [pk[2 + bi]])
                    rk = ('RS', 1)
                    if which == 0:
                        C.ACT(RS[:, 1, :], PB[2 + bi][:], AF.Sqrt, [pk[2 + bi]], [rk], scale=1.0, bias=128.0 * EPS)
                    else:
                        C.ACT(RS[:, 1, :], PB[2 + bi][:], AF.Sqrt, [pk[2 + bi]], [rk], scale=1.0 / 128, bias=EPS)
                    C.OP('dve', lambda e: e.reciprocal(RS[:, 1, :], RS[:, 1, :]), [rk], [rk])
                    ok = ('OB', oi % 4)
                    ob = OB[:, oi % 4, :]
                    oi += 1
                    C.STT(ob, PB[bi][:], GQK[:, which:which + 1], RS[:, 1, :], MUL, MUL, [pk[bi], rk, 'GQK'], [ok])
                    C.DMA('sp', dst[hd * 128:(hd + 1) * 128, g0:g0 + 512], ob, [ok], [dk])
        for jv in range(4):
            ws = WS[wi % 2]
            wk = 'WS%d' % (wi % 2)
            wi += 1
            C.DMA('pool', ws, win_v[:, :, 4096 + jv * 512:4096 + (jv + 1) * 512], [], [wk])
            for s in range(4):
                bi = 4 + (s % 2)
                for c in range(16):
                    C.MM(PB[bi][:], HB[:, c, s * 128:(s + 1) * 128], ws[:, c, :], c == 0, c == 15, [wk, ('HB', c)], [pk[bi]])
                ok = ('OB', oi % 4)
                ob = OB[:, oi % 4, :]
                oi += 1
                C.ACT(ob, PB[bi][:], AF.Copy, [pk[bi]], [ok])
                t0 = g0 + s * 128
                C.DMA('sp', VS3[jv * 4:(jv + 1) * 4, t0:t0 + 128, :].rearrange("h t e -> t h e"),
                      ob.rearrange("p (h e) -> p h e", h=4), [ok], ['VS'])


def emit_p2(C, sb, PB, pk, T, j):
    S = C.S
    SEQ = 8192
    NKB = SEQ // 128
    NQB = SEQ // 512
    OS = T["OS"][j]
    QL, KL, VL = T["QL"], T["KL"], T["VL"]
    VL4 = VL.rearrange("(r pr a) (b e) -> r pr (a b) e", r=4, pr=4, b=16, e=128)
    QT = [sb("QT%d" % k, [128, SEQ], BF16) for k in range(2)]
    KT = [sb("KT%d" % k, [128, SEQ], BF16) for k in range(2)]
    V = [sb("V%d" % k, [128, NKB, 128], BF16) for k in range(2)]
    NTRI = sb("NTRI", [128, 128], BF16)
    ONESB = sb("ONESB", [128, 128], BF16)
    MASK = sb("MASK", [128, 4, 512], BF16)
    EB = sb("EB", [128, 2, 512], F32)
    SPB = sb("SPB", [128, 2, 512], BF16)
    ARG = sb("ARG", [128, 2, 512], F32)
    WB = sb("WB", [128, 2, 512], BF16)
    R = sb("R", [128, 512], F32)
    OB = sb("OB", [128, 2, 512], BF16)
    C.DMA('pool', NTRI, T["ntri"], [], ['NTRI'])
    C.DMA('pool', ONESB, T["ones"], [], ['ONESB'])
    C.DMA('pool', MASK, T["masks"], [], ['MASK'])
    def loads(pr):
        d = pr % 2
        for r in range(4):
            C.DMA('pool', QT[d][:, r * 2048:(r + 1) * 2048], QL[r * 512 + pr * 128:r * 512 + (pr + 1) * 128, :], ['QL'], [('QT', d)])
            C.DMA('pool', KT[d][:, r * 2048:(r + 1) * 2048], KL[r * 512 + pr * 128:r * 512 + (pr + 1) * 128, :], ['KL'], [('KT', d)])
            C.DMA('pool', V[d][:, r * 16:(r + 1) * 16, :], VL4[r, pr].rearrange("(kb q) e -> q kb e", q=128), ['VL'], [('V', d)])

    its = []
    grp = 0
    for pr in range(4):
        for qb in range(NQB):
            kbs = list(range(4 * qb + 3, -1, -1))
            for n, kb in enumerate(kbs):
                its.append(dict(pr=pr, d=pr % 2, qb=qb, q0=qb * 512, kb=kb, n=n, last=(n == len(kbs) - 1),
                                diag=(kb >= 4 * qb), g=grp, first_of_pair=(qb == 0 and n == 0)))
            grp += 1
    N = len(its)

    def stageA(k):
        t = its[k]
        i2 = k % 2
        d = t['d']
        kslice = KT[d][:, t['kb'] * 128:(t['kb'] + 1) * 128]
        qslice = QT[d][:, t['q0']:t['q0'] + 512]
        C.MM(PB[i2][:], kslice, qslice, True, True, [('KT', d), ('QT', d)], [pk[i2]])
        C.ACT(EB[:, i2, :], PB[i2][:], AF.Exp, [pk[i2]], [('EB', i2)])
        C.ACT(SPB[:, i2, :], EB[:, i2, :], AF.Ln, [('EB', i2)], [('SPB', i2)], bias=1.0)
        if t['diag']:
            C.TT('pool', SPB[:, i2, :], SPB[:, i2, :], MASK[:, t['kb'] - 4 * t['qb'], :], MUL, [('SPB', i2), 'MASK'], [('SPB', i2)])

    def stageB1(k):
        t = its[k]
        i2 = k % 2
        d = t['d']
        ab, tb = 2 + i2, 4 + i2
        kslice = KT[d][:, t['kb'] * 128:(t['kb'] + 1) * 128]
        qslice = QT[d][:, t['q0']:t['q0'] + 512]
        if t['n'] == 0:
            C.OP('dve', lambda e: e.memset(R, 0.0), [], ['R'])
        C.MM(PB[ab][:], kslice, qslice, True, False, [('KT', d), ('QT', d)], [pk[ab]])
        C.MM(PB[ab][:], NTRI, SPB[:, i2, :], False, True, ['NTRI', ('SPB', i2)], [pk[ab]])
        if not t['last']:
            C.MM(PB[tb][:], ONESB, SPB[:, i2, :], True, True, ['ONESB', ('SPB', i2)], [pk[tb]])
        C.TT('dve', ARG[:, i2, :], PB[ab][:], R, ALU.subtract, [pk[ab], 'R'], [('ARG', i2)])
        C.ACT(WB[:, i2, :], ARG[:, i2, :], AF.Exp, [('ARG', i2)], [('WB', i2)])
        if t['diag']:
            C.TT('pool', WB[:, i2, :], WB[:, i2, :], MASK[:, t['kb'] - 4 * t['qb'], :], MUL, [('WB', i2), 'MASK'], [('WB', i2)])
        if not t['last']:
            C.TT('dve', R, R, PB[tb][:], ADD, [pk[tb], 'R'], ['R'])

    def stageB2(k):
        t = its[k]
        i2 = k % 2
        d = t['d']
        ob_i = t['g'] % 2
        obank = 6 + ob_i
        C.MM(PB[obank][:], V[d][:, t['kb'], :], WB[:, i2, :], t['n'] == 0, t['last'], [('V', d), ('WB', i2)], [pk[obank]])
        if t['last']:
            C.ACT(OB[:, ob_i, :], PB[obank][:], AF.Copy, [pk[obank]], [('OB', ob_i)])
            tq = t['q0'] // 2048
            row = tq * 512 + t['pr'] * 128
            C.DMA('sp', OS[row:row + 128, (t['q0'] % 2048):(t['q0'] % 2048) + 512], OB[:, ob_i, :], [('OB', ob_i)], ['OS'])

    loads(0)
    for sidx in range(N + 2):
        if sidx < N:
            stageA(sidx)
        if 0 <= sidx - 1 < N:
            stageB1(sidx - 1)
        if 0 <= sidx - 2 < N:
            stageB2(sidx - 2)
            if its[sidx - 2]['first_of_pair'] and its[sidx - 2]['pr'] + 1 < 4:
                loads(its[sidx - 2]['pr'] + 1)


def emit_tok(C, sb, PB, pk, T, mode, i, src, dst, final):
    S = C.S
    j = i // 2
    xT_v = fm_view(src)
    xo_v = fm_view(dst)
    w_mo = T["sb_w_out"][j] if mode == 'attn' else T["sg_w_out"][j]
    wmo_v = w_mo.rearrange("(c q) n -> q c n", q=128)
    wpg_v = T["ple_w_gate"][i].rearrange("(c q) n -> q c n", q=128)
    wpp_v = T["ple_w_proj"][i].rearrange("(c q) n -> q c n", q=128)
    pT_v = T["pT"][i].rearrange("(c q) t -> q c t", q=128)
    wr_v = T["wr"][i].rearrange("(c q) n -> q c n", q=128)
    wg_d, wu_d, wd_d = T["moe_w_gate"][i], T["moe_w_up"][i], T["moe_w_down"][i]
    if mode == 'sg':
        w_in_v = T["sg_w_in"][j].rearrange("(c q) n -> q c n", q=128)

    X1 = sb("X1", [128, 16, 1024], F32)
    H = sb("H", [128, 16, 1024], BF16)
    WA = sb("WA", [128, 12288], BF16)
    WB_ = sb("WB", [128, 12288], BF16)
    AR = [WA, WB_]
    HID = sb("HID", [128, 2, 2, 1024], BF16)
    ETF = sb("ETF", [128, 4096], F32)
    ET = ETF.rearrange("p (m t) -> p m t", m=16)
    SQ = sb("SQ", [128, 2, 512], F32)
    RS = sb("RS", [128, 512], F32)
    T1 = sb("T1", [128, 2, 512], F32)
    SL = sb("SL", [128, 2, 512], F32)
    ONESF = sb("ONESF", [128, 128], F32)
    IDF = sb("IDF", [128, 128], F32)
    GEXP = sb("GEXP", [128, 2, 128], F32)
    G_FFN = sb("G_FFN", [128, 16], F32)
    G_PIN = sb("G_PIN", [128, 16], F32)
    G_POUT = sb("G_POUT", [128, 16], F32)
    WR = sb("WR", [128, 16, 36], BF16)
    BR = sb("BR", [128, 36], F32)
    GATES = sb("GATES", [128, 8, 32], F32)
    LG = sb("LG", [128, 36], F32)
    ME = sb("ME", [128, 32], F32)
    EX = sb("EX", [128, 32], F32)
    SEL = sb("SEL", [128, 32], F32)
    SM = sb("SM", [128, 16], F32)
    M8 = sb("M8", [128, 8], F32)
    PT = sb("PT", [128, 2, 256], BF16)
    if mode == 'sg':
        G_MIX = sb("G_MIX", [128, 16], F32)
        VG = sb("VG", [128, 16], F32)
        WCT = sb("WCT", [128, 16, 128], BF16)
        TRIB = sb("TRIB", [128, 128], BF16)
        BS = sb("BS", [128, 16, 128], F32)
        UT = sb("UT", [128, 512], F32)
        MX = sb("MX", [128, 512], F32)
        SS = sb("SS", [128, 16], F32)
        RV = sb("RV", [128, 4], F32)
        GTD = ETF.bitcast(BF16).rearrange("p (m t) -> p m t", m=16)

    C.DMA('sp', ONESF, T["ones"], [], ['ONESF'])
    C.DMA('sp', IDF, T["ident"], [], ['IDF'])
    C.DMA('sp', G_FFN, T["g_ffn"][i], [], ['G_FFN'])
    C.DMA('sp', G_PIN, T["g_pin"][i], [], ['G_PIN'])
    C.DMA('sp', G_POUT, T["g_pout"][i], [], ['G_POUT'])
    C.DMA('sp', BR, T["br"][i], [], ['BR'])
    C.DMA('pool', WR, wr_v, [], ['WR'])
    if mode == 'sg':
        C.DMA('sp', G_MIX, T["g_mix"][i], [], ['G_MIX'])
        C.DMA('sp', VG, T["vg"][j], [], ['VG'])
        C.DMA('sp', BS, T["bs"][j], [], ['BS'])
        C.DMA('pool', WCT, T["wsT"][j], [], ['WCT'])
        C.DMA('pool', TRIB, T["trimask"], [], ['TRIB'])
        for g in range(16):
            C.TT('pool', WCT[:, g, :], WCT[:, g, :], TRIB, MUL, ['WCT', 'TRIB'], ['WCT'])
        C.OP('dve', lambda e: e.memset(SS, 0.0), [], ['SS'])
    C.OP('dve', lambda e: e.memset(SM, 0.0), [], ['SM'])

    arena_i = [0]

    def next_arena():
        a = arena_i[0] % 2
        arena_i[0] += 1
        return AR[a], 'AR%d' % a

    def rms_fm(src_, skey, g_tile, gkey, dst_, dkey, W, bank, bkey):
        for c in range(16):
            sqb = SQ[:, c % 2, 0:W]
            C.ACT(sqb, src_(c), AF.Square, [skey(c)], [('SQ', c % 2)])
            C.MM(bank[:, 0:W], ONESF, sqb, c == 0, c == 15, [('SQ', c % 2), 'ONESF'], [bkey])
        C.ACT(RS[:, 0:W], bank[:, 0:W], AF.Sqrt, [bkey], ['RS'], scale=1.0 / D, bias=EPS)
        C.OP('dve', lambda e: e.reciprocal(RS[:, 0:W], RS[:, 0:W]), ['RS'], ['RS'])
        for c in range(16):
            C.STT(dst_(c), src_(c), g_tile[:, c:c + 1], RS[:, 0:W], MUL, MUL, [skey(c), 'RS', gkey], [dkey(c)])

    gelu_i = [0]

    def gelu(bank, bkey, W, out, okeys, sscol=None):
        k = gelu_i[0] % 2
        gelu_i[0] += 1
        t = T1[:, k, 0:W]
        tk = ('T1', k)
        C.ACT(t, bank, AF.Square, [bkey], [tk])
        C.TS('dve', t, t, 0.044715, 1.0, MUL, ADD, [tk], [tk])
        C.TT('dve', t, t, bank, MUL, [tk, bkey], [tk])
        C.ACT(t, t, AF.Sigmoid, [tk], [tk], scale=1.5957691216057308)
        C.TT('dve', out, t, bank, MUL, [tk, bkey], okeys)
        if sscol is not None:
            C.ACT(t, out, AF.Square, okeys, [tk, 'SS'], accum_out=sscol)

    def wst_view(ar, n):
        return ar[:, 0:16 * n].rearrange("p (c f) -> p c f", c=16)

    out_toks = []
    for p in range(2):
        for b in range(2):
            g0 = p * 1024 + b * 512
            bl = slice(b * 512, (b + 1) * 512)
            ol = slice((1 - b) * 512, (2 - b) * 512)
            xk = [('X1', b, c) for c in range(16)]
            ok = [('H', 1 - b, c) for c in range(16)]
            C.DMA('sp', X1[:, :, bl], xT_v[:, :, g0:g0 + 512], [], xk)
            if mode == 'attn':
                C.DMA('pool', H[:, :, ol], fm_view(T["OL"])[:, :, g0:g0 + 512], ['OL'], ok)
                src_keys = ok
                srcf = lambda c: H[:, c, ol]
            else:
                rms_fm(lambda c: X1[:, c, bl], lambda c: ('X1', b, c), G_MIX, 'G_MIX',
                       lambda c: H[:, c, bl], lambda c: ('H', b, c), 512, PB[7], pk[7])
                C.OP('dve', lambda e: e.memset(SS, 0.0), [], ['SS'])
                for jv in range(4):
                    ar, ak = next_arena()
                    wst = wst_view(ar, 512)
                    C.DMA('pool', wst, w_in_v[:, :, 2048 + jv * 512:2048 + (jv + 1) * 512], [], [ak, ak + 'u'])
                    for s in range(4):
                        bi = (jv * 4 + s) % 2
                        for c in range(16):
                            C.MM(PB[bi][:], H[:, c, b * 512 + s * 128:b * 512 + (s + 1) * 128], wst[:, c, :],
                                 c == 0, c == 15, [('H', b, c), ak, ak + 'u'], [pk[bi]])
                        gelu(PB[bi][:], pk[bi], 512, H[:, s * 4 + jv, ol], [('H', 1 - b, s * 4 + jv)], sscol=SS[:, s * 4 + jv:s * 4 + jv + 1])
                C.OP('dve', lambda e: e.tensor_reduce(out=RV, in_=SS.rearrange("p (s j) -> p s j", j=4), axis=AX.X, op=ADD), ['SS'], ['RV'])
                C.ACT(RV, RV, AF.Sqrt, ['RV'], ['RV'], scale=1.0 / 2048, bias=EPS)
                C.OP('dve', lambda e: e.reciprocal(RV, RV), ['RV'], ['RV'])
                for s in range(4):
                    for jv in range(4):
                        vk = ('H', 1 - b, s * 4 + jv)
                        C.TS('dve', H[:, s * 4 + jv, ol], H[:, s * 4 + jv, ol], RV[:, s:s + 1], None, MUL, None, [vk, 'RV'], [vk])
                for mg in range(4):
                    ar, ak = next_arena()
                    wst = wst_view(ar, 512)
                    C.DMA('pool', wst, w_in_v[:, :, mg * 512:(mg + 1) * 512], [], [ak, ak + 'u'])
                    for mi in range(4):
                        m = mg * 4 + mi
                        bu = 2 + (m % 2) * 2
                        bm = 3 + (m % 2) * 2
                        for c in range(16):
                            C.MM(PB[bu][:], wst[:, c, mi * 128:(mi + 1) * 128], H[:, c, bl], c == 0, c == 15,
                                 [ak, ak + 'u', ('H', b, c)], [pk[bu]])
                        for s in range(4):
                            vk = ('H', 1 - b, s * 4 + m // 4)
                            o0 = (1 - b) * 512 + (m % 4) * 128
                            C.MM(PB[bm][:, s * 128:(s + 1) * 128], H[:, s * 4 + m // 4, o0:o0 + 128], WCT[:, m, :], True, True,
                                 [vk, 'WCT'], [pk[bm]])
                        gelu(PB[bu][:], pk[bu], 512, UT, ['UT'])
                        for s in range(4):
                            C.STT(MX[:, s * 128:(s + 1) * 128], PB[bm][:, s * 128:(s + 1) * 128], VG[:, m:m + 1], BS[:, m, :], MUL, ADD,
                                  [pk[bm], 'VG', 'BS'], ['MX'])
                        C.TT('dve', GTD[:, m, :], UT, MX, MUL, ['UT', 'MX'], [('GTD', m)])
                src_keys = [('GTD', c) for c in range(16)]
                srcf = lambda c: GTD[:, c, :]
            for mg in range(4):
                ar, ak = next_arena()
                wst = wst_view(ar, 512)
                C.DMA('pool', wst, wmo_v[:, :, mg * 512:(mg + 1) * 512], [], [ak, ak + 'u'])
                for mi in range(4):
                    m = mg * 4 + mi
                    bi = m % 2
                    for c in range(16):
                        C.MM(PB[bi][:], wst[:, c, mi * 128:(mi + 1) * 128], srcf(c), c == 0, c == 15, [ak, ak + 'u', src_keys[c]], [pk[bi]])
                    C.TT('dve', X1[:, m, bl], X1[:, m, bl], PB[bi][:], ADD, [pk[bi], ('X1', b, m)], [('X1', b, m)])
        for b in range(2):
            bl = slice(b * 512, (b + 1) * 512)
            rms_fm(lambda c: X1[:, c, bl], lambda c: ('X1', b, c), G_FFN, 'G_FFN',
                   lambda c: H[:, c, bl], lambda c: ('H', b, c), 512, PB[7], pk[7])
        for sub in range(8):
            b = sub // 4
            for c in range(16):
                C.MM(PB[6][:, 0:36], H[:, c, sub * 128:(sub + 1) * 128], WR[:, c, :], c == 0, c == 15, [('H', b, c), 'WR'], [pk[6]])
            C.TT('dve', LG, PB[6][:, 0:36], BR, ADD, [pk[6], 'BR'], ['LG'])
            C.OP('dve', lambda e: e.tensor_reduce(out=SM[:, 0:1], in_=LG[:, 0:4], axis=AX.X, op=ALU.max), ['LG'], ['SM'])
            C.TS('dve', SM[:, 1:2], SM[:, 0:1], -1.0, None, MUL, None, ['SM'], ['SM'])
            C.OP('dve', lambda e: e.memset(SM[:, 2:3], 0.0), [], ['SM'])
            C.ACT(EX[:, 0:4], LG[:, 0:4], AF.Exp, ['LG', 'SM'], ['EX', 'SM'], bias=SM[:, 1:2], accum_out=SM[:, 2:3])
            C.TS('dve', SEL[:, 0:4], LG[:, 0:4], SM[:, 0:1], None, ALU.is_equal, None, ['LG', 'SM'], ['SEL'])
            C.TS('dve', SEL[:, 0:4], SEL[:, 0:4], -1.0, 1.0e4, ADD, MUL, ['SEL'], ['SEL'])
            for g in range(4):
                C.TS('dve', ME[:, g * 8:(g + 1) * 8], LG[:, 4 + g * 8:4 + (g + 1) * 8], SEL[:, g:g + 1], None, ADD, None, ['LG', 'SEL'], ['ME'])
            C.OP('dve', lambda e: e.max(out=M8, in_=ME), ['ME'], ['M8'])
            C.TS('dve', SM[:, 3:4], M8[:, 0:1], -1.0, None, MUL, None, ['M8'], ['SM'])
            C.ACT(EX, ME, AF.Exp, ['ME', 'SM'], ['EX'], bias=SM[:, 3:4])
            C.TS('dve', SEL, ME, M8[:, 1:2], None, ALU.is_ge, None, ['ME', 'M8'], ['SEL'])
            C.TT('dve', EX, EX, SEL, MUL, ['EX', 'SEL'], ['EX'])
            C.OP('dve', lambda e: e.tensor_reduce(out=SM[:, 4:5], in_=EX, axis=AX.X, op=ADD), ['EX'], ['SM'])
            C.TT('dve', SM[:, 5:6], SM[:, 4:5], SM[:, 2:3], MUL, ['SM'], ['SM'])
            C.OP('dve', lambda e: e.reciprocal(SM[:, 6:7], SM[:, 5:6]), ['SM'], ['SM'])
            C.TS('dve', GATES[:, sub, :], EX, SM[:, 6:7], None, MUL, None, ['EX', 'SM'], [('GATES', sub)])
        for ex in range(32):
            ar, ak = next_arena()
            Wg = ar[:, 0:4096].rearrange("p (c f) -> p c f", c=16)
            Wu = ar[:, 4096:8192].rearrange("p (c f) -> p c f", c=16)
            Wd = ar[:, 8192:12288].rearrange("p (c f) -> p c f", c=2)
            C.DMA('pool', Wg, wg_d[ex].rearrange("(c q) f -> q c f", q=128), [], [ak])
            C.DMA('pool', Wu, wu_d[ex].rearrange("(c q) f -> q c f", q=128), [], [ak + 'u'])
            C.DMA('pool', Wd, wd_d[ex].rearrange("(c q) f -> q c f", q=128), [], [ak + 'd'])
            hp = ex % 2
            for sub in range(8):
                gk = ('GEXP', sub % 2)
                C.TS('dve', GEXP[:, sub % 2, :], ONESF, GATES[:, sub, ex:ex + 1], None, MUL, None, ['ONESF', ('GATES', sub)], [gk])
                C.MM(PB[6 + sub // 4][:, (sub % 4) * 128:(sub % 4 + 1) * 128], GEXP[:, sub % 2, :], IDF, True, True,
                     [gk, 'IDF'], [pk[6 + sub // 4]])
            for b in range(2):
                bl = slice(b * 512, (b + 1) * 512)
                for jf in range(2):
                    k = (b * 2 + jf) % 2
                    for c in range(16):
                        C.MM(PB[2 * k][:], Wg[:, c, jf * 128:(jf + 1) * 128], H[:, c, bl], c == 0, c == 15, [ak, ('H', b, c)], [pk[2 * k]])
                    for c in range(16):
                        C.MM(PB[2 * k + 1][:], Wu[:, c, jf * 128:(jf + 1) * 128], H[:, c, bl], c == 0, c == 15, [ak + 'u', ('H', b, c)], [pk[2 * k + 1]])
                    slk = ('SL', k)
                    C.ACT(SL[:, k, :], PB[2 * k][:], AF.Silu, [pk[2 * k]], [slk])
                    C.TT('dve', SL[:, k, :], SL[:, k, :], PB[2 * k + 1][:], MUL, [slk, pk[2 * k + 1]], [slk])
                    C.TT('dve', HID[:, hp, jf, bl], SL[:, k, :], PB[6 + b][:], MUL, [slk, pk[6 + b]], [('HID', hp, jf, b)])
            for b in range(2):
                bl = slice(b * 512, (b + 1) * 512)
                for m in range(16):
                    bi = 4 + (m % 2)
                    for jf in range(2):
                        C.MM(PB[bi][:], Wd[:, jf, m * 128:(m + 1) * 128], HID[:, hp, jf, bl], jf == 0, jf == 1,
                             [ak + 'd', ('HID', hp, jf, b)], [pk[bi]])
                    C.TT('dve', X1[:, m, bl], X1[:, m, bl], PB[bi][:], ADD, [pk[bi], ('X1', b, m)], [('X1', b, m)])
        WPP = WB_[:, 8192:12288].rearrange("p (c f) -> p c f", c=2)
        C.DMA('pool', WPP, wpp_v, [], ['AR1d'])
        for sbk in range(4):
            b = sbk // 2
            g0 = p * 1024 + sbk * 256
            cl = slice(sbk * 256, (sbk + 1) * 256)
            C.DMA('pool', PT, pT_v[:, :, g0:g0 + 256], [], ['PT'])
            hkey = lambda c: ('H', b, c)
            rms_fm(lambda c: X1[:, c, cl], lambda c: ('X1', b, c), G_PIN, 'G_PIN',
                   lambda c: H[:, c, cl], hkey, 256, PB[7], pk[7])
            for mg in range(8):
                wk = 'AR0' if mg % 2 == 0 else 'AR0u'
                wst = WA[:, (mg % 2) * 4096:(mg % 2 + 1) * 4096].rearrange("p (c f) -> p c f", c=16)
                C.DMA('pool', wst, wpg_v[:, :, mg * 256:(mg + 1) * 256], [], [wk])
                for mi in range(2):
                    m = mg * 2 + mi
                    bg = (m % 2) * 2
                    bp = bg + 1
                    for c in range(16):
                        C.MM(PB[bg][:, 0:256], wst[:, c, mi * 128:(mi + 1) * 128], H[:, c, cl], c == 0, c == 15, [wk, hkey(c)], [pk[bg]])
                    for c in range(2):
                        C.MM(PB[bp][:, 0:256], WPP[:, c, m * 128:(m + 1) * 128], PT[:, c, :], c == 0, c == 1, ['AR1d', 'PT'], [pk[bp]])
                    tk = ('T1', m % 2)
                    C.ACT(T1[:, m % 2, 0:256], PB[bg][:, 0:256], AF.Sigmoid, [pk[bg]], [tk])
                    C.TT('dve', ET[:, m, :], T1[:, m % 2, 0:256], PB[bp][:, 0:256], MUL, [tk, pk[bp]], [('ET', m)])
            for m in range(16):
                sqb = SQ[:, m % 2, 0:256]
                C.ACT(sqb, ET[:, m, :], AF.Square, [('ET', m)], [('SQ', m % 2)])
                C.MM(PB[4][:, 0:256], ONESF, sqb, m == 0, m == 15, [('SQ', m % 2), 'ONESF'], [pk[4]])
            C.ACT(RS[:, 0:256], PB[4][:, 0:256], AF.Sqrt, [pk[4]], ['RS'], scale=1.0 / D, bias=EPS)
            C.OP('dve', lambda e: e.reciprocal(RS[:, 0:256], RS[:, 0:256]), ['RS'], ['RS'])
            for m in range(16):
                C.STT(ET[:, m, :], ET[:, m, :], G_POUT[:, m:m + 1], RS[:, 0:256], MUL, MUL, [('ET', m), 'RS', 'G_POUT'], [('ET', m)])
                C.TT('dve', X1[:, m, cl], X1[:, m, cl], ET[:, m, :], ADD, [('ET', m), ('X1', b, m)], [('X1', b, m)])
            out_toks.append(C.DMA('sp', xo_v[:, :, g0:g0 + 256], X1[:, :, cl], [('X1', b, m) for m in range(16)], ['XDST']))
    if final:
        S.wait_all('sp', out_toks)


def build_fused():
    nc = bass.Bass("TRN2", target_bir_lowering=False)

    def din(n, s):
        return nc.dram_tensor(n, s, F32, kind="ExternalInput").ap()
    T = {}
    T["xT"] = din("xT", [D, NT])
    T["pT"] = din("pT", [4, 256, NT])
    T["g_mix"] = din("g_mix", [4, 128, 16])
    T["g_ffn"] = din("g_ffn", [4, 128, 16])
    T["g_pin"] = din("g_pin", [4, 128, 16])
    T["g_pout"] = din("g_pout", [4, 128, 16])
    T["gq"] = din("gq", [2, 128, 1])
    T["gk"] = din("gk", [2, 128, 1])
    T["vg"] = din("vg", [2, 128, 16])
    T["wsT"] = din("wsT", [2, 128, 16, 128])
    T["bs"] = din("bs", [2, 128, 16, 128])
    T["wr"] = din("wr", [4, D, 36])
    T["br"] = din("br", [4, 128, 36])
    T["sb_w_in"] = din("sb_w_in", [2, D, 3 * D])
    T["sb_w_out"] = din("sb_w_out", [2, D, D])
    T["sg_w_in"] = din("sg_w_in", [2, D, 4096])
    T["sg_w_out"] = din("sg_w_out", [2, D, D])
    T["moe_w_gate"] = din("moe_w_gate", [4, 32, D, 256])
    T["moe_w_up"] = din("moe_w_up", [4, 32, D, 256])
    T["moe_w_down"] = din("moe_w_down", [4, 32, 256, D])
    T["ple_w_gate"] = din("ple_w_gate", [4, D, D])
    T["ple_w_proj"] = din("ple_w_proj", [4, 256, D])
    T["ident"] = din("ident", [128, 128])
    T["ones"] = din("ones", [128, 128])
    T["trimask"] = din("trimask", [128, 128])
    T["ntri"] = din("ntri", [128, 128])
    T["masks"] = din("masks", [128, 4, 512])
    xo = nc.dram_tensor("xo", [D, NT], F32, kind="ExternalOutput").ap()
    XS = [nc.dram_tensor("XS%d" % k, [D, NT], F32).ap() for k in range(2)]
    for nm, shp in (("QS", [D, NT]), ("KS", [D, NT]), ("VS", [D, NT]), ("OS", [D, NT]),
                    ("QG", [8, 1024, NT]), ("KG", [8, 1024, NT]), ("VG", [8, 1024, NT]), ("OG", [8, 1024, NT])):
        T[nm] = [nc.dram_tensor("%s%d" % (nm, k), shp, BF16).ap() for k in range(2)]
    for nm in ("QL", "KL", "VL", "OL"):
        T[nm] = nc.dram_tensor(nm, [D, NT], BF16).ap()

    _RANK.clear()
    _VIEWS.clear()
    with ExitStack() as es:
        C = Ctx(nc, es)
        S = C.S
        S.ops['pool'].append((lambda e: (get_rank(e), None)[1], [], None))
        WORDS = 52992
        BIG = es.enter_context(nc.sbuf_tensor("BIG", [128, WORDS], F32))
        sb = Arena(BIG[:, :], WORDS)
        PB = [es.enter_context(nc.psum_tensor("pb%d" % k, [128, 512], F32)) for k in range(8)]
        pk = ['p%d' % k for k in range(8)]

        def gather(a, b, ka, kb_, loc, kl):
            for k in range(8):
                S.cc(lambda e, k=k: e.collective_compute("AllGather", ALU.bypass, replica_groups=GROUPS,
                                                         ins=[a[k * 256:(k + 1) * 256, :].opt()], outs=[b[k].opt()]), [ka], [kb_])

            def pick(e, two):
                mine = b.rearrange("(g two) (r xp) t -> g r two xp t", two=2, r=4)[bass.ds(get_rank(e), 1)].squeeze(0)
                return e.dma_start(out=loc.rearrange("(r two xp) t -> r two xp t", r=4, two=2)[:, two], in_=mine[:, two])
            for two in range(2):
                C.DMAF('pool', lambda e, two=two: pick(e, two), [kb_], [kl])

        for i in range(4):
            src = T["xT"] if i == 0 else XS[(i - 1) % 2]
            dst = xo if i == 3 else XS[i % 2]
            j = i // 2
            if i % 2 == 0:
                sb.reset()
                emit_p1(C, sb, PB, pk, T, j, i, src)
                S.barrier()
                gather(T["QS"][j], T["QG"][j], 'QS', 'QG', T["QL"], 'QL')
                gather(T["KS"][j], T["KG"][j], 'KS', 'KG', T["KL"], 'KL')
                gather(T["VS"][j], T["VG"][j], 'VS', 'VG', T["VL"], 'VL')
                sb.reset()
                emit_p2(C, sb, PB, pk, T, j)
                S.barrier()
                gather(T["OS"][j], T["OG"][j], 'OS', 'OG', T["OL"], 'OL')
                sb.reset()
                emit_tok(C, sb, PB, pk, T, 'attn', i, src, dst, False)
                S.barrier()
            else:
                sb.reset()
                emit_tok(C, sb, PB, pk, T, 'sg', i, src, dst, i == 3)
                S.barrier()
        S.emit()
    return nc


def fm(g):
    return np.ascontiguousarray(g.reshape(16, 128).T.astype(np.float32))


def kernel(**inp):
    inp = {k: np.asarray(v) for k, v in inp.items()}
    x = np.ascontiguousarray(inp["x"], dtype=np.float32).reshape(16384, 2048)
    p = inp["p"].reshape(4, 16384, 256)
    k = np.arange(128)
    q = np.arange(512)
    shared = {
        "g_mix": np.stack([fm(inp["norm_mix"][i]) for i in range(4)]),
        "g_ffn": np.stack([fm(inp["norm_ffn"][i]) for i in range(4)]),
        "g_pin": np.stack([fm(inp["ple_norm_in"][i]) for i in range(4)]),
        "g_pout": np.stack([fm(inp["ple_norm_out"][i]) for i in range(4)]),
        "gq": np.ascontiguousarray(inp["sb_q_norm"].reshape(2, 128, 1)),
        "gk": np.ascontiguousarray(inp["sb_k_norm"].reshape(2, 128, 1)),
        "vg": np.stack([fm(inp["sg_v_norm"][j]) for j in range(2)]),
        "wsT": np.ascontiguousarray(np.transpose(inp["sg_w_s"], (0, 3, 1, 2))),
        "bs": np.ascontiguousarray(np.broadcast_to(inp["sg_b_s"][:, None], (2, 128, 16, 128))),
        "wr": np.ascontiguousarray(np.concatenate([inp["moe_w_group"], inp["moe_w_expert"]], axis=2)),
        "br": np.ascontiguousarray(np.broadcast_to(np.concatenate([inp["moe_b_group"], inp["moe_b_expert"]], axis=1)[:, None, :], (4, 128, 36))),
        "sb_w_in": inp["sb_w_in"], "sb_w_out": inp["sb_w_out"], "sg_w_in": inp["sg_w_in"], "sg_w_out": inp["sg_w_out"],
        "moe_w_gate": inp["moe_w_gate"], "moe_w_up": inp["moe_w_up"], "moe_w_down": inp["moe_w_down"],
        "ple_w_gate": inp["ple_w_gate"], "ple_w_proj": inp["ple_w_proj"],
        "ident": np.eye(128, dtype=np.float32), "ones": np.ones((128, 128), np.float32),
        "trimask": (k[:, None] <= k[None, :]).astype(np.float32),
        "ntri": -(k[:, None] >= k[None, :]).astype(np.float32),
        "masks": np.ascontiguousarray(np.stack([((k[:, None] + o * 128) < q[None, :]).astype(np.float32) for o in range(4)], axis=1)),
    }
    maps = []
    for c in range(N_CORES):
        m = dict(shared)
        m["xT"] = np.ascontiguousarray(x[c * NT:(c + 1) * NT].T)
        m["pT"] = np.ascontiguousarray(np.transpose(p[:, c * NT:(c + 1) * NT, :], (0, 2, 1)))
        maps.append(m)
    nc = build_fused()
    res = run_bass_kernel_spmd(nc, maps, core_ids=list(range(N_CORES)))
    out = np.concatenate([res.results[c]["xo"].T for c in range(N_CORES)], axis=0).reshape(2, 8192, 2048)
    return np.ascontiguousarray(out, dtype=np.float32)
```

```python
import numpy as np
import concourse.bass as bass
import concourse.mybir as mybir
from concourse.bass_utils import run_bass_kernel_spmd
from contextlib import ExitStack

F32 = mybir.dt.float32
BF16 = mybir.dt.bfloat16
AF = mybir.ActivationFunctionType
ALU = mybir.AluOpType
AX = mybir.AxisListType

ENGS = ['pe', 'act', 'dve', 'pool', 'sp']
DMAQ = ('sp', 'act', 'pool')


class Sched:
    def __init__(self, nc, es, R=4):
        self.nc = nc
        self.sem = {e: es.enter_context(nc.semaphore('s_' + e)) for e in ENGS}
        self.cnt = {e: 0 for e in ENGS}
        self.ops = {e: [] for e in ENGS}
        self.waited = {e: {} for e in ENGS}
        self.lastw = {}
        self.readers = {}
        self.R = R
        self.dsem = {q: [es.enter_context(nc.semaphore('d_%s%d' % (q, i))) for i in range(R)] for q in DMAQ}
        self.dcnt = {q: 0 for q in DMAQ}
        self.semobj = {}
        self.ccn = 0
        self.semobj[('cc',)] = es.enter_context(nc.semaphore('s_cc'))
        for e in ENGS:
            self.semobj[('c', e)] = self.sem[e]
        for q in DMAQ:
            for i in range(R):
                self.semobj[('d', q, i)] = self.dsem[q][i]

    def _deps(self, eng, reads, writes, skip_sem=None):
        deps = {}

        def add(tok):
            if tok is None:
                return
            s, v = tok
            if s == skip_sem:
                return
            if deps.get(s, 0) < v:
                deps[s] = v
        for k in reads:
            for s, v in self.lastw.get(k, {}).items():
                add((s, v))
        for k in writes:
            for s, v in self.lastw.get(k, {}).items():
                add((s, v))
            for s, v in self.readers.get(k, {}).items():
                add((s, v))
        out = []
        w = self.waited[eng]
        for s, v in deps.items():
            if w.get(s, 0) < v:
                w[s] = v
                out.append((s, v))
        return out

    def _commit(self, tok, reads, writes):
        s, v = tok
        for k in reads:
            r = self.readers.setdefault(k, {})
            if r.get(s, 0) < v:
                r[s] = v
        for k in writes:
            d = self.lastw.setdefault(k, {})
            if d.get(s, 0) < v:
                d[s] = v
            self.readers[k] = {}

    def op(self, eng, fn, reads=(), writes=()):
        skip = ('c', 'pe') if eng == 'pe' else None
        waits = self._deps(eng, reads, writes, skip)
        self.cnt[eng] += 1
        tok = (('c', eng), self.cnt[eng])
        self.ops[eng].append((fn, waits, (('c', eng), 1)))
        self._commit(tok, reads, writes)
        return tok

    def dma(self, q, fn, reads=(), writes=()):
        i = self.dcnt[q]
        self.dcnt[q] += 1
        slot = i % self.R
        semk = ('d', q, slot)
        waits = self._deps(q, reads, writes)
        prev = 16 * (i // self.R)
        if prev > 0 and self.waited[q].get(semk, 0) < prev:
            self.waited[q][semk] = prev
            waits.append((semk, prev))
        tok = (semk, prev + 16)
        self.ops[q].append((fn, waits, (semk, 16)))
        self._commit(tok, reads, writes)
        return tok


    def cc(self, fn, reads=(), writes=()):
        waits = self._deps('pool', reads, writes)
        self.ccn += 1
        tok = (('cc',), self.ccn)
        self.ops['pool'].append((fn, waits, (('cc',), 1)))
        self._commit(tok, reads, writes)
        return tok

    def barrier(self):
        latest = {}
        for e in ENGS:
            if self.cnt[e] > 0:
                latest[('c', e)] = self.cnt[e]
        for q in DMAQ:
            n = self.dcnt[q]
            for slot in range(self.R):
                k = (n - slot + self.R - 1) // self.R if n > slot else 0
                if k > 0:
                    latest[('d', q, slot)] = 16 * k
        if self.ccn > 0:
            latest[('cc',)] = self.ccn
        for e in ENGS:
            waits = []
            for s, v in latest.items():
                if self.waited[e].get(s, 0) < v:
                    self.waited[e][s] = v
                    waits.append((s, v))
            self.ops[e].append((None, waits, None))
        self.lastw.clear()
        self.readers.clear()

    def wait_all(self, eng, toks):
        waits = []
        for s, v in toks:
            if self.waited[eng].get(s, 0) < v:
                self.waited[eng][s] = v
                waits.append((s, v))
        self.ops[eng].append((None, waits, None))

    def emit(self):
        nc = self.nc
        S = self
        with nc.Block() as block:
            def run(e, name):
                for fn, waits, inc in S.ops[name]:
                    for s, v in waits:
                        e.wait_ge(S.semobj[s], v)
                    if fn is not None:
                        ins = fn(e)
                        if inc is not None:
                            ins.then_inc(S.semobj[inc[0]], inc[1])

            @block.sync
            def _(e):
                run(e, 'sp')

            @block.tensor
            def _(e):
                run(e, 'pe')

            @block.scalar
            def _(e):
                run(e, 'act')

            @block.vector
            def _(e):
                run(e, 'dve')

            @block.gpsimd
            def _(e):
                run(e, 'pool')


D = 2048
NT = 2048
EPS = 1e-6
MUL = ALU.mult
ADD = ALU.add
N_CORES = 8
GROUPS = [[0, 1, 2, 3], [4, 5, 6, 7]]


class Ctx:
    def __init__(self, nc, es):
        self.nc = nc
        self.es = es
        self.S = Sched(nc, es)

    def MM(self, out, lhsT, rhs, start, stop, r, w):
        self.S.op('pe', lambda e: e.matmul(out, lhsT, rhs, start=start, stop=stop), r, w)

    def ACT(self, out, in_, func, r, w, **kw):
        self.S.op('act', lambda e: e.activation(out=out, in_=in_, func=func, **kw), r, w)

    def TT(self, eng, out, a, b, op, r, w):
        self.S.op(eng, lambda e: e.tensor_tensor(out=out, in0=a, in1=b, op=op), r, w)

    def TS(self, eng, out, a, s1, s2, op0, op1, r, w):
        if s2 is None:
            self.S.op(eng, lambda e: e.tensor_scalar(out=out, in0=a, scalar1=s1, scalar2=None, op0=op0), r, w)
        else:
            self.S.op(eng, lambda e: e.tensor_scalar(out=out, in0=a, scalar1=s1, scalar2=s2, op0=op0, op1=op1), r, w)

    def STT(self, out, a, s, b, op0, op1, r, w):
        self.S.op('dve', lambda e: e.scalar_tensor_tensor(out=out, in0=a, scalar=s, in1=b, op0=op0, op1=op1), r, w)

    def DMA(self, q, out, in_, r, w):
        return self.S.dma(q, lambda e: e.dma_start(out=out, in_=in_), r, w)

    def DMAF(self, q, fn, r, w):
        return self.S.dma(q, fn, r, w)

    def OP(self, eng, fn, r, w):
        self.S.op(eng, fn, r, w)


class Arena:
    def __init__(self, big, words):
        self.big = big
        self.words = words
        self.off = 0

    def reset(self):
        self.off = 0

    def __call__(self, name, shape, dt):
        n = 1
        for s in shape[1:]:
            n *= s
        if dt == BF16:
            w = (n + 1) // 2
            ap = self.big[:, self.off:self.off + w].bitcast(BF16)
        else:
            w = n
            ap = self.big[:, self.off:self.off + w]
        self.off += (w + 15) // 16 * 16
        assert self.off <= self.words, (name, self.off, self.words)
        if len(shape) == 3:
            ap = ap.rearrange("p (a b) -> p a b", a=shape[1])
        elif len(shape) == 4:
            ap = ap.rearrange("p (a b c) -> p a b c", a=shape[1], b=shape[2])
        return ap


_RANK = {}


def get_rank(e):
    if 'r' not in _RANK:
        _RANK['r'] = e.snap(e.partition_id() % 4, min_val=0, max_val=3)
    return _RANK['r']


_VIEWS = {}


def dyn_view(e, key, mk):
    if key not in _VIEWS:
        _VIEWS[key] = mk(get_rank(e))
    return _VIEWS[key]


def fm_view(ap2d):
    return ap2d.rearrange("(c q) t -> q c t", q=128)


def emit_p1(C, sb, PB, pk, T, j, i, src, gather_chunk):
    S = C.S
    xT_v = fm_view(src)
    win_v = T["sb_w_in"][j].rearrange("(c q) n -> q c n", q=128)
    QS, KS, VS = T["QS"][j], T["KS"][j], T["VS"][j]
    VS3 = VS.rearrange("(h a) (b e) -> h (a b) e", h=16, b=16, e=128)
    XB = [sb("XB%d" % k, [128, 16, 512], F32) for k in range(2)]
    HB = sb("HB", [128, 16, 2048], BF16)
    WS = [sb("WS%d" % k, [128, 16, 512], BF16) for k in range(2)]
    SQ = sb("SQ", [128, 2, 512], BF16)
    RS = sb("RS", [128, 2, 512], F32)
    OB = sb("OB", [128, 4, 512], BF16)
    ONESB = sb("ONESB", [128, 128], BF16)
    G_MIX = sb("G_MIX", [128, 16], F32)
    GQK = sb("GQK", [128, 2], F32)
    C.DMA('pool', ONESB, T["ones"], [], ['ONESB'])
    C.DMA('sp', G_MIX, T["g_mix"][i], [], ['G_MIX'])
    C.DMA('sp', GQK[:, 0:1], T["gq"][j], [], ['GQK'])
    C.DMA('sp', GQK[:, 1:2], T["gk"][j], [], ['GQK'])
    groups = [(w, g) for w in range(3) for g in range(4)]

    def wload(n):
        w, g = groups[n]
        C.DMA('pool', WS[n % 2], win_v[:, :, w * 2048 + g * 512:w * 2048 + (g + 1) * 512], [], ['WS%d' % (n % 2)])
    wload(0)
    for blk in range(4):
        g0 = blk * 512
        xb = XB[blk % 2]
        C.DMA('sp', xb, xT_v[:, :, g0:g0 + 512], [], [('XB', blk % 2, c) for c in range(16)])
        for c in range(16):
            sqb = SQ[:, c % 2, :]
            C.ACT(sqb, xb[:, c, :], AF.Square, [('XB', blk % 2, c)], [('SQ', c % 2)])
            C.MM(PB[7][:], ONESB, sqb, c == 0, c == 15, [('SQ', c % 2), 'ONESB'], [pk[7]])
        C.ACT(RS[:, 0, :], PB[7][:], AF.Sqrt, [pk[7]], [('RS', 0)], scale=1.0 / D, bias=EPS)
        C.OP('dve', lambda e: e.reciprocal(RS[:, 0, :], RS[:, 0, :]), [('RS', 0)], [('RS', 0)])
        for c in range(16):
            C.STT(HB[:, c, g0:g0 + 512], xb[:, c, :], G_MIX[:, c:c + 1], RS[:, 0, :], MUL, MUL,
                  [('XB', blk % 2, c), ('RS', 0), 'G_MIX'], [('HB', blk, c)])
    oi = 0
    for n, (w, g) in enumerate(groups):
        ws = WS[n % 2]
        wk = 'WS%d' % (n % 2)
        if n + 1 < len(groups):
            wload(n + 1)
        for blk in range(4):
            g0 = blk * 512
            if w < 2:
                dst = QS if w == 0 else KS
                dk = ('QS', g) if w == 0 else ('KS', g)
                for mi in range(4):
                    hd = g * 4 + mi
                    bi = hd % 2
                    for c in range(16):
                        C.MM(PB[bi][:], ws[:, c, mi * 128:(mi + 1) * 128], HB[:, c, g0:g0 + 512], c == 0, c == 15, [wk, ('HB', blk, c)], [pk[bi]])
                    sqb = SQ[:, bi, :]
                    C.ACT(sqb, PB[bi][:], AF.Square, [pk[bi]], [('SQ', bi)])
                    C.MM(PB[2 + bi][:], ONESB, sqb, True, True, [('SQ', bi), 'ONESB'], [pk[2 + bi]])
                    rk = ('RS', 1)
                    if w == 0:
                        C.ACT(RS[:, 1, :], PB[2 + bi][:], AF.Sqrt, [pk[2 + bi]], [rk], scale=1.0, bias=128.0 * EPS)
                    else:
                        C.ACT(RS[:, 1, :], PB[2 + bi][:], AF.Sqrt, [pk[2 + bi]], [rk], scale=1.0 / 128, bias=EPS)
                    C.OP('dve', lambda e: e.reciprocal(RS[:, 1, :], RS[:, 1, :]), [rk], [rk])
                    ok = ('OB', oi % 4)
                    ob = OB[:, oi % 4, :]
                    oi += 1
                    C.STT(ob, PB[bi][:], GQK[:, w:w + 1], RS[:, 1, :], MUL, MUL, [pk[bi], rk, 'GQK'], [ok])
                    C.DMA('sp', dst[hd * 128:(hd + 1) * 128, g0:g0 + 512], ob, [ok], [dk])
            else:
                for sx in range(4):
                    bi = 4 + (sx % 2)
                    for c in range(16):
                        C.MM(PB[bi][:], HB[:, c, g0 + sx * 128:g0 + (sx + 1) * 128], ws[:, c, :], c == 0, c == 15, [wk, ('HB', blk, c)], [pk[bi]])
                    ok = ('OB', oi % 4)
                    ob = OB[:, oi % 4, :]
                    oi += 1
                    C.ACT(ob, PB[bi][:], AF.Copy, [pk[bi]], [ok])
                    t0 = g0 + sx * 128
                    C.DMA('sp', VS3[g * 4:(g + 1) * 4, t0:t0 + 128, :].rearrange("h t e -> t h e"),
                          ob.rearrange("p (h e) -> p h e", h=4), [ok], [('VS', g)])
        nm = ("QS", "KS", "VS")[w]
        gather_chunk(nm, 2 * g, (nm, g))
        gather_chunk(nm, 2 * g + 1, (nm, g))


def emit_p2(C, sb, PB, pk, T, j, gather_chunk):
    S = C.S
    SEQ = 8192
    NKB = SEQ // 128
    NQB = SEQ // 512
    OS = T["OS"][j]
    QL, KL, VL = T["QL"], T["KL"], T["VL"]
    VL4 = VL.rearrange("(r pr a) (b e) -> r pr (a b) e", r=4, pr=4, b=16, e=128)
    QT = [sb("QT%d" % k, [128, SEQ], BF16) for k in range(2)]
    KT = [sb("KT%d" % k, [128, SEQ], BF16) for k in range(2)]
    V = [sb("V%d" % k, [128, NKB, 128], BF16) for k in range(2)]
    NTRI = sb("NTRI", [128, 128], BF16)
    ONESB = sb("ONESB", [128, 128], BF16)
    MASK = sb("MASK", [128, 4, 512], BF16)
    EB = sb("EB", [128, 2, 512], F32)
    SPB = sb("SPB", [128, 2, 512], BF16)
    ARG = sb("ARG", [128, 2, 512], F32)
    WB = sb("WB", [128, 2, 512], BF16)
    R = sb("R", [128, 512], F32)
    OB = sb("OB", [128, 2, 512], BF16)
    C.DMA('pool', NTRI, T["ntri"], [], ['NTRI'])
    C.DMA('pool', ONESB, T["ones"], [], ['ONESB'])
    C.DMA('pool', MASK, T["masks"], [], ['MASK'])
    def loads(pr):
        d = pr % 2
        for r in range(4):
            C.DMA('pool', QT[d][:, r * 2048:(r + 1) * 2048], QL[r * 512 + pr * 128:r * 512 + (pr + 1) * 128, :], ['QL'], [('QT', d)])
            C.DMA('pool', KT[d][:, r * 2048:(r + 1) * 2048], KL[r * 512 + pr * 128:r * 512 + (pr + 1) * 128, :], ['KL'], [('KT', d)])
            C.DMA('pool', V[d][:, r * 16:(r + 1) * 16, :], VL4[r, pr].rearrange("(kb q) e -> q kb e", q=128), ['VL'], [('V', d)])

    its = []
    grp = 0
    for pr in range(4):
        for qb in range(NQB):
            kbs = list(range(4 * qb + 3, -1, -1))
            for n, kb in enumerate(kbs):
                its.append(dict(pr=pr, d=pr % 2, qb=qb, q0=qb * 512, kb=kb, n=n, last=(n == len(kbs) - 1),
                                diag=(kb >= 4 * qb), g=grp, first_of_pair=(qb == 0 and n == 0)))
            grp += 1
    N = len(its)

    def stageA(k):
        t = its[k]
        i2 = k % 2
        d = t['d']
        kslice = KT[d][:, t['kb'] * 128:(t['kb'] + 1) * 128]
        qslice = QT[d][:, t['q0']:t['q0'] + 512]
        C.MM(PB[i2][:], kslice, qslice, True, True, [('KT', d), ('QT', d)], [pk[i2]])
        C.ACT(EB[:, i2, :], PB[i2][:], AF.Exp, [pk[i2]], [('EB', i2)])
        C.ACT(SPB[:, i2, :], EB[:, i2, :], AF.Ln, [('EB', i2)], [('SPB', i2)], bias=1.0)
        if t['diag']:
            C.TT('pool', SPB[:, i2, :], SPB[:, i2, :], MASK[:, t['kb'] - 4 * t['qb'], :], MUL, [('SPB', i2), 'MASK'], [('SPB', i2)])

    def stageB1(k):
        t = its[k]
        i2 = k % 2
        d = t['d']
        ab, tb = 2 + i2, 4 + i2
        kslice = KT[d][:, t['kb'] * 128:(t['kb'] + 1) * 128]
        qslice = QT[d][:, t['q0']:t['q0'] + 512]
        if t['n'] == 0:
            C.OP('dve', lambda e: e.memset(R, 0.0), [], ['R'])
        C.MM(PB[ab][:], kslice, qslice, True, False, [('KT', d), ('QT', d)], [pk[ab]])
        C.MM(PB[ab][:], NTRI, SPB[:, i2, :], False, True, ['NTRI', ('SPB', i2)], [pk[ab]])
        if not t['last']:
            C.MM(PB[tb][:], ONESB, SPB[:, i2, :], True, True, ['ONESB', ('SPB', i2)], [pk[tb]])
        C.TT('dve', ARG[:, i2, :], PB[ab][:], R, ALU.subtract, [pk[ab], 'R'], [('ARG', i2)])
        C.ACT(WB[:, i2, :], ARG[:, i2, :], AF.Exp, [('ARG', i2)], [('WB', i2)])
        if t['diag']:
            C.TT('pool', WB[:, i2, :], WB[:, i2, :], MASK[:, t['kb'] - 4 * t['qb'], :], MUL, [('WB', i2), 'MASK'], [('WB', i2)])
        if not t['last']:
            C.TT('dve', R, R, PB[tb][:], ADD, [pk[tb], 'R'], ['R'])

    def stageB2(k):
        t = its[k]
        i2 = k % 2
        d = t['d']
        ob_i = t['g'] % 2
        obank = 6 + ob_i
        C.MM(PB[obank][:], V[d][:, t['kb'], :], WB[:, i2, :], t['n'] == 0, t['last'], [('V', d), ('WB', i2)], [pk[obank]])
        if t['last']:
            C.ACT(OB[:, ob_i, :], PB[obank][:], AF.Copy, [pk[obank]], [('OB', ob_i)])
            tq = t['q0'] // 2048
            row = tq * 512 + t['pr'] * 128
            C.DMA('sp', OS[row:row + 128, (t['q0'] % 2048):(t['q0'] % 2048) + 512], OB[:, ob_i, :], [('OB', ob_i)], [('OS', t['pr'] // 2)])
            if t['qb'] == NQB - 1 and t['pr'] % 2 == 1:
                for tq2 in range(4):
                    gather_chunk("OS", 2 * tq2 + t['pr'] // 2, ('OS', t['pr'] // 2))

    loads(0)
    for sidx in range(N + 2):
        if sidx < N:
            stageA(sidx)
        if 0 <= sidx - 1 < N:
            stageB1(sidx - 1)
        if 0 <= sidx - 2 < N:
            stageB2(sidx - 2)
            if its[sidx - 2]['first_of_pair'] and its[sidx - 2]['pr'] + 1 < 4:
                loads(its[sidx - 2]['pr'] + 1)


def emit_tok(C, sb, PB, pk, T, mode, i, src, dst, final):
    S = C.S
    j = i // 2
    xT_v = fm_view(src)
    xo_v = fm_view(dst)
    w_mo = T["sb_w_out"][j] if mode == 'attn' else T["sg_w_out"][j]
    wmo_v = w_mo.rearrange("(c q) n -> q c n", q=128)
    wpg_v = T["ple_w_gate"][i].rearrange("(c q) n -> q c n", q=128)
    wpp_v = T["ple_w_proj"][i].rearrange("(c q) n -> q c n", q=128)
    pT_v = T["pT"][i].rearrange("(c q) t -> q c t", q=128)
    wr_v = T["wr"][i].rearrange("(c q) n -> q c n", q=128)
    wg_d, wu_d, wd_d = T["moe_w_gate"][i], T["moe_w_up"][i], T["moe_w_down"][i]
    if mode == 'sg':
        w_in_v = T["sg_w_in"][j].rearrange("(c q) n -> q c n", q=128)

    X1 = sb("X1", [128, 16, 1024], F32)
    H = sb("H", [128, 16, 1024], BF16)
    WA = sb("WA", [128, 12288], BF16)
    WB_ = sb("WB", [128, 12288], BF16)
    AR = [WA, WB_]
    HID = sb("HID", [128, 2, 2, 1024], BF16)
    ETF = sb("ETF", [128, 4096], F32)
    ET = ETF.rearrange("p (m t) -> p m t", m=16)
    SQ = sb("SQ", [128, 2, 512], BF16)
    ONESB = sb("ONESB", [128, 128], BF16)
    RS = sb("RS", [128, 512], F32)
    T1 = sb("T1", [128, 2, 512], F32)
    SL = sb("SL", [128, 2, 512], F32)
    ONESF = sb("ONESF", [128, 128], F32)
    IDF = sb("IDF", [128, 128], F32)
    GEXP = sb("GEXP", [128, 2, 128], F32)
    G_FFN = sb("G_FFN", [128, 16], F32)
    G_PIN = sb("G_PIN", [128, 16], F32)
    G_POUT = sb("G_POUT", [128, 16], F32)
    WR = sb("WR", [128, 16, 36], BF16)
    BR = sb("BR", [128, 36], F32)
    GATES = sb("GATES", [128, 8, 32], F32)
    LG = sb("LG", [128, 36], F32)
    ME = sb("ME", [128, 32], F32)
    EX = sb("EX", [128, 32], F32)
    SEL = sb("SEL", [128, 32], F32)
    SM = sb("SM", [128, 16], F32)
    M8 = sb("M8", [128, 8], F32)
    PT = sb("PT", [128, 2, 256], BF16)
    if mode == 'sg':
        G_MIX = sb("G_MIX", [128, 16], F32)
        VG = sb("VG", [128, 16], F32)
        WCT = sb("WCT", [128, 16, 128], BF16)
        TRIB = sb("TRIB", [128, 128], BF16)
        BS = sb("BS", [128, 16, 128], F32)
        UT = sb("UT", [128, 512], F32)
        MX = sb("MX", [128, 512], F32)
        SS = sb("SS", [128, 16], F32)
        RV = sb("RV", [128, 4], F32)
        GTD = ETF.bitcast(BF16).rearrange("p (m t) -> p m t", m=16)

    C.DMA('sp', ONESF, T["ones"], [], ['ONESF'])
    C.DMA('pool', ONESB, T["ones"], [], ['ONESB'])
    C.DMA('sp', IDF, T["ident"], [], ['IDF'])
    C.DMA('sp', G_FFN, T["g_ffn"][i], [], ['G_FFN'])
    C.DMA('sp', G_PIN, T["g_pin"][i], [], ['G_PIN'])
    C.DMA('sp', G_POUT, T["g_pout"][i], [], ['G_POUT'])
    C.DMA('sp', BR, T["br"][i], [], ['BR'])
    C.DMA('pool', WR, wr_v, [], ['WR'])
    if mode == 'sg':
        C.DMA('sp', G_MIX, T["g_mix"][i], [], ['G_MIX'])
        C.DMA('sp', VG, T["vg"][j], [], ['VG'])
        C.DMA('sp', BS, T["bs"][j], [], ['BS'])
        C.DMA('pool', WCT, T["wsT"][j], [], ['WCT'])
        C.DMA('pool', TRIB, T["trimask"], [], ['TRIB'])
        for g in range(16):
            C.TT('pool', WCT[:, g, :], WCT[:, g, :], TRIB, MUL, ['WCT', 'TRIB'], ['WCT'])
        C.OP('dve', lambda e: e.memset(SS, 0.0), [], ['SS'])
    C.OP('dve', lambda e: e.memset(SM, 0.0), [], ['SM'])

    arena_i = [0]

    def next_arena():
        a = arena_i[0] % 2
        arena_i[0] += 1
        return AR[a], 'AR%d' % a

    def rms_fm(src_, skey, g_tile, gkey, dst_, dkey, W, bank, bkey):
        for c in range(16):
            sqb = SQ[:, c % 2, 0:W]
            C.ACT(sqb, src_(c), AF.Square, [skey(c)], [('SQ', c % 2)])
            C.MM(bank[:, 0:W], ONESB, sqb, c == 0, c == 15, [('SQ', c % 2), 'ONESB'], [bkey])
        C.ACT(RS[:, 0:W], bank[:, 0:W], AF.Sqrt, [bkey], ['RS'], scale=1.0 / D, bias=EPS)
        C.OP('dve', lambda e: e.reciprocal(RS[:, 0:W], RS[:, 0:W]), ['RS'], ['RS'])
        for c in range(16):
            C.STT(dst_(c), src_(c), g_tile[:, c:c + 1], RS[:, 0:W], MUL, MUL, [skey(c), 'RS', gkey], [dkey(c)])

    gelu_i = [0]

    def gelu(bank, bkey, W, out, okeys, sscol=None):
        k = gelu_i[0] % 2
        gelu_i[0] += 1
        t = T1[:, k, 0:W]
        tk = ('T1', k)
        C.ACT(t, bank, AF.Square, [bkey], [tk])
        C.TS('dve', t, t, 0.044715, 1.0, MUL, ADD, [tk], [tk])
        C.TT('dve', t, t, bank, MUL, [tk, bkey], [tk])
        C.ACT(t, t, AF.Sigmoid, [tk], [tk], scale=1.5957691216057308)
        C.TT('dve', out, t, bank, MUL, [tk, bkey], okeys)
        if sscol is not None:
            C.ACT(t, out, AF.Square, okeys, [tk, 'SS'], accum_out=sscol)

    def wst_view(ar, n):
        return ar[:, 0:16 * n].rearrange("p (c f) -> p c f", c=16)

    out_toks = []
    for p in range(2):
        for b in range(2):
            g0 = p * 1024 + b * 512
            bl = slice(b * 512, (b + 1) * 512)
            ol = slice((1 - b) * 512, (2 - b) * 512)
            xk = [('X1', b, c) for c in range(16)]
            ok = [('H', 1 - b, c) for c in range(16)]
            C.DMA('sp', X1[:, :, bl], xT_v[:, :, g0:g0 + 512], [], xk)
            if mode == 'attn':
                C.DMA('pool', H[:, :, ol], fm_view(T["OL"])[:, :, g0:g0 + 512], ['OL'], ok)
                src_keys = ok
                srcf = lambda c: H[:, c, ol]
            else:
                rms_fm(lambda c: X1[:, c, bl], lambda c: ('X1', b, c), G_MIX, 'G_MIX',
                       lambda c: H[:, c, bl], lambda c: ('H', b, c), 512, PB[7], pk[7])
                C.OP('dve', lambda e: e.memset(SS, 0.0), [], ['SS'])
                for jv in range(4):
                    ar, ak = next_arena()
                    wst = wst_view(ar, 512)
                    C.DMA('pool', wst, w_in_v[:, :, 2048 + jv * 512:2048 + (jv + 1) * 512], [], [ak, ak + 'u'])
                    for s in range(4):
                        bi = (jv * 4 + s) % 2
                        for c in range(16):
                            C.MM(PB[bi][:], H[:, c, b * 512 + s * 128:b * 512 + (s + 1) * 128], wst[:, c, :],
                                 c == 0, c == 15, [('H', b, c), ak, ak + 'u'], [pk[bi]])
                        gelu(PB[bi][:], pk[bi], 512, H[:, s * 4 + jv, ol], [('H', 1 - b, s * 4 + jv)], sscol=SS[:, s * 4 + jv:s * 4 + jv + 1])
                C.OP('dve', lambda e: e.tensor_reduce(out=RV, in_=SS.rearrange("p (s j) -> p s j", j=4), axis=AX.X, op=ADD), ['SS'], ['RV'])
                C.ACT(RV, RV, AF.Sqrt, ['RV'], ['RV'], scale=1.0 / 2048, bias=EPS)
                C.OP('dve', lambda e: e.reciprocal(RV, RV), ['RV'], ['RV'])
                for s in range(4):
                    for jv in range(4):
                        vk = ('H', 1 - b, s * 4 + jv)
                        C.TS('dve', H[:, s * 4 + jv, ol], H[:, s * 4 + jv, ol], RV[:, s:s + 1], None, MUL, None, [vk, 'RV'], [vk])
                for mg in range(4):
                    ar, ak = next_arena()
                    wst = wst_view(ar, 512)
                    C.DMA('pool', wst, w_in_v[:, :, mg * 512:(mg + 1) * 512], [], [ak, ak + 'u'])
                    for mi in range(4):
                        m = mg * 4 + mi
                        bu = 2 + (m % 2) * 2
                        bm = 3 + (m % 2) * 2
                        for c in range(16):
                            C.MM(PB[bu][:], wst[:, c, mi * 128:(mi + 1) * 128], H[:, c, bl], c == 0, c == 15,
                                 [ak, ak + 'u', ('H', b, c)], [pk[bu]])
                        for s in range(4):
                            vk = ('H', 1 - b, s * 4 + m // 4)
                            o0 = (1 - b) * 512 + (m % 4) * 128
                            C.MM(PB[bm][:, s * 128:(s + 1) * 128], H[:, s * 4 + m // 4, o0:o0 + 128], WCT[:, m, :], True, True,
                                 [vk, 'WCT'], [pk[bm]])
                        gelu(PB[bu][:], pk[bu], 512, UT, ['UT'])
                        for s in range(4):
                            C.STT(MX[:, s * 128:(s + 1) * 128], PB[bm][:, s * 128:(s + 1) * 128], VG[:, m:m + 1], BS[:, m, :], MUL, ADD,
                                  [pk[bm], 'VG', 'BS'], ['MX'])
                        C.TT('dve', GTD[:, m, :], UT, MX, MUL, ['UT', 'MX'], [('GTD', m)])
                src_keys = [('GTD', c) for c in range(16)]
                srcf = lambda c: GTD[:, c, :]
            for mg in range(4):
                ar, ak = next_arena()
                wst = wst_view(ar, 512)
                C.DMA('pool', wst, wmo_v[:, :, mg * 512:(mg + 1) * 512], [], [ak, ak + 'u'])
                for mi in range(4):
                    m = mg * 4 + mi
                    bi = m % 2
                    for c in range(16):
                        C.MM(PB[bi][:], wst[:, c, mi * 128:(mi + 1) * 128], srcf(c), c == 0, c == 15, [ak, ak + 'u', src_keys[c]], [pk[bi]])
                    C.TT('dve', X1[:, m, bl], X1[:, m, bl], PB[bi][:], ADD, [pk[bi], ('X1', b, m)], [('X1', b, m)])
        for b in range(2):
            bl = slice(b * 512, (b + 1) * 512)
            rms_fm(lambda c: X1[:, c, bl], lambda c: ('X1', b, c), G_FFN, 'G_FFN',
                   lambda c: H[:, c, bl], lambda c: ('H', b, c), 512, PB[7], pk[7])
        for sub in range(8):
            b = sub // 4
            for c in range(16):
                C.MM(PB[6][:, 0:36], H[:, c, sub * 128:(sub + 1) * 128], WR[:, c, :], c == 0, c == 15, [('H', b, c), 'WR'], [pk[6]])
            C.TT('dve', LG, PB[6][:, 0:36], BR, ADD, [pk[6], 'BR'], ['LG'])
            C.OP('dve', lambda e: e.tensor_reduce(out=SM[:, 0:1], in_=LG[:, 0:4], axis=AX.X, op=ALU.max), ['LG'], ['SM'])
            C.TS('dve', SM[:, 1:2], SM[:, 0:1], -1.0, None, MUL, None, ['SM'], ['SM'])
            C.OP('dve', lambda e: e.memset(SM[:, 2:3], 0.0), [], ['SM'])
            C.ACT(EX[:, 0:4], LG[:, 0:4], AF.Exp, ['LG', 'SM'], ['EX', 'SM'], bias=SM[:, 1:2], accum_out=SM[:, 2:3])
            C.TS('dve', SEL[:, 0:4], LG[:, 0:4], SM[:, 0:1], None, ALU.is_equal, None, ['LG', 'SM'], ['SEL'])
            C.TS('dve', SEL[:, 0:4], SEL[:, 0:4], -1.0, 1.0e4, ADD, MUL, ['SEL'], ['SEL'])
            for g in range(4):
                C.TS('dve', ME[:, g * 8:(g + 1) * 8], LG[:, 4 + g * 8:4 + (g + 1) * 8], SEL[:, g:g + 1], None, ADD, None, ['LG', 'SEL'], ['ME'])
            C.OP('dve', lambda e: e.max(out=M8, in_=ME), ['ME'], ['M8'])
            C.TS('dve', SM[:, 3:4], M8[:, 0:1], -1.0, None, MUL, None, ['M8'], ['SM'])
            C.ACT(EX, ME, AF.Exp, ['ME', 'SM'], ['EX'], bias=SM[:, 3:4])
            C.TS('dve', SEL, ME, M8[:, 1:2], None, ALU.is_ge, None, ['ME', 'M8'], ['SEL'])
            C.TT('dve', EX, EX, SEL, MUL, ['EX', 'SEL'], ['EX'])
            C.OP('dve', lambda e: e.tensor_reduce(out=SM[:, 4:5], in_=EX, axis=AX.X, op=ADD), ['EX'], ['SM'])
            C.TT('dve', SM[:, 5:6], SM[:, 4:5], SM[:, 2:3], MUL, ['SM'], ['SM'])
            C.OP('dve', lambda e: e.reciprocal(SM[:, 6:7], SM[:, 5:6]), ['SM'], ['SM'])
            C.TS('dve', GATES[:, sub, :], EX, SM[:, 6:7], None, MUL, None, ['EX', 'SM'], [('GATES', sub)])
        for ex in range(32):
            ar, ak = next_arena()
            Wg = ar[:, 0:4096].rearrange("p (c f) -> p c f", c=16)
            Wu = ar[:, 4096:8192].rearrange("p (c f) -> p c f", c=16)
            Wd = ar[:, 8192:12288].rearrange("p (c f) -> p c f", c=2)
            C.DMA('pool', Wg, wg_d[ex].rearrange("(c q) f -> q c f", q=128), [], [ak])
            C.DMA('pool', Wu, wu_d[ex].rearrange("(c q) f -> q c f", q=128), [], [ak + 'u'])
            C.DMA('pool', Wd, wd_d[ex].rearrange("(c q) f -> q c f", q=128), [], [ak + 'd'])
            hp = ex % 2
            for sub in range(8):
                gk = ('GEXP', sub % 2)
                C.TS('dve', GEXP[:, sub % 2, :], ONESF, GATES[:, sub, ex:ex + 1], None, MUL, None, ['ONESF', ('GATES', sub)], [gk])
                C.MM(PB[6 + sub // 4][:, (sub % 4) * 128:(sub % 4 + 1) * 128], GEXP[:, sub % 2, :], IDF, True, True,
                     [gk, 'IDF'], [pk[6 + sub // 4]])
            for b in range(2):
                bl = slice(b * 512, (b + 1) * 512)
                for jf in range(2):
                    k = (b * 2 + jf) % 2
                    for c in range(16):
                        C.MM(PB[2 * k][:], Wg[:, c, jf * 128:(jf + 1) * 128], H[:, c, bl], c == 0, c == 15, [ak, ('H', b, c)], [pk[2 * k]])
                    for c in range(16):
                        C.MM(PB[2 * k + 1][:], Wu[:, c, jf * 128:(jf + 1) * 128], H[:, c, bl], c == 0, c == 15, [ak + 'u', ('H', b, c)], [pk[2 * k + 1]])
                    slk = ('SL', k)
                    C.ACT(SL[:, k, :], PB[2 * k][:], AF.Silu, [pk[2 * k]], [slk])
                    C.TT('dve', SL[:, k, :], SL[:, k, :], PB[2 * k + 1][:], MUL, [slk, pk[2 * k + 1]], [slk])
                    C.TT('dve', HID[:, hp, jf, bl], SL[:, k, :], PB[6 + b][:], MUL, [slk, pk[6 + b]], [('HID', hp, jf, b)])
            for b in range(2):
                bl = slice(b * 512, (b + 1) * 512)
                for m in range(16):
                    bi = 4 + (m % 2)
                    for jf in range(2):
                        C.MM(PB[bi][:], Wd[:, jf, m * 128:(m + 1) * 128], HID[:, hp, jf, bl], jf == 0, jf == 1,
                             [ak + 'd', ('HID', hp, jf, b)], [pk[bi]])
                    C.TT('dve', X1[:, m, bl], X1[:, m, bl], PB[bi][:], ADD, [pk[bi], ('X1', b, m)], [('X1', b, m)])
        WPP = WB_[:, 8192:12288].rearrange("p (c f) -> p c f", c=2)
        C.DMA('pool', WPP, wpp_v, [], ['AR1d'])
        for sbk in range(4):
            b = sbk // 2
            g0 = p * 1024 + sbk * 256
            cl = slice(sbk * 256, (sbk + 1) * 256)
            C.DMA('pool', PT, pT_v[:, :, g0:g0 + 256], [], ['PT'])
            hkey = lambda c: ('H', b, c)
            rms_fm(lambda c: X1[:, c, cl], lambda c: ('X1', b, c), G_PIN, 'G_PIN',
                   lambda c: H[:, c, cl], hkey, 256, PB[7], pk[7])
            for mg in range(8):
                wk = 'AR0' if mg % 2 == 0 else 'AR0u'
                wst = WA[:, (mg % 2) * 4096:(mg % 2 + 1) * 4096].rearrange("p (c f) -> p c f", c=16)
                C.DMA('pool', wst, wpg_v[:, :, mg * 256:(mg + 1) * 256], [], [wk])
                for mi in range(2):
                    m = mg * 2 + mi
                    bg = (m % 2) * 2
                    bp = bg + 1
                    for c in range(16):
                        C.MM(PB[bg][:, 0:256], wst[:, c, mi * 128:(mi + 1) * 128], H[:, c, cl], c == 0, c == 15, [wk, hkey(c)], [pk[bg]])
                    for c in range(2):
                        C.MM(PB[bp][:, 0:256], WPP[:, c, m * 128:(m + 1) * 128], PT[:, c, :], c == 0, c == 1, ['AR1d', 'PT'], [pk[bp]])
                    tk = ('T1', m % 2)
                    C.ACT(T1[:, m % 2, 0:256], PB[bg][:, 0:256], AF.Sigmoid, [pk[bg]], [tk])
                    C.TT('dve', ET[:, m, :], T1[:, m % 2, 0:256], PB[bp][:, 0:256], MUL, [tk, pk[bp]], [('ET', m)])
            for m in range(16):
                sqb = SQ[:, m % 2, 0:256]
                C.ACT(sqb, ET[:, m, :], AF.Square, [('ET', m)], [('SQ', m % 2)])
                C.MM(PB[4][:, 0:256], ONESB, sqb, m == 0, m == 15, [('SQ', m % 2), 'ONESB'], [pk[4]])
            C.ACT(RS[:, 0:256], PB[4][:, 0:256], AF.Sqrt, [pk[4]], ['RS'], scale=1.0 / D, bias=EPS)
            C.OP('dve', lambda e: e.reciprocal(RS[:, 0:256], RS[:, 0:256]), ['RS'], ['RS'])
            for m in range(16):
                C.STT(ET[:, m, :], ET[:, m, :], G_POUT[:, m:m + 1], RS[:, 0:256], MUL, MUL, [('ET', m), 'RS', 'G_POUT'], [('ET', m)])
                C.TT('dve', X1[:, m, cl], X1[:, m, cl], ET[:, m, :], ADD, [('ET', m), ('X1', b, m)], [('X1', b, m)])
            out_toks.append(C.DMA('sp', xo_v[:, :, g0:g0 + 256], X1[:, :, cl], [('X1', b, m) for m in range(16)], ['XDST']))
    if final:
        S.wait_all('sp', out_toks)


def build_fused():
    nc = bass.Bass("TRN2", target_bir_lowering=False)

    def din(n, s):
        return nc.dram_tensor(n, s, F32, kind="ExternalInput").ap()
    T = {}
    T["xT"] = din("xT", [D, NT])
    T["pT"] = din("pT", [4, 256, NT])
    T["g_mix"] = din("g_mix", [4, 128, 16])
    T["g_ffn"] = din("g_ffn", [4, 128, 16])
    T["g_pin"] = din("g_pin", [4, 128, 16])
    T["g_pout"] = din("g_pout", [4, 128, 16])
    T["gq"] = din("gq", [2, 128, 1])
    T["gk"] = din("gk", [2, 128, 1])
    T["vg"] = din("vg", [2, 128, 16])
    T["wsT"] = din("wsT", [2, 128, 16, 128])
    T["bs"] = din("bs", [2, 128, 16, 128])
    T["wr"] = din("wr", [4, D, 36])
    T["br"] = din("br", [4, 128, 36])
    T["sb_w_in"] = din("sb_w_in", [2, D, 3 * D])
    T["sb_w_out"] = din("sb_w_out", [2, D, D])
    T["sg_w_in"] = din("sg_w_in", [2, D, 4096])
    T["sg_w_out"] = din("sg_w_out", [2, D, D])
    T["moe_w_gate"] = din("moe_w_gate", [4, 32, D, 256])
    T["moe_w_up"] = din("moe_w_up", [4, 32, D, 256])
    T["moe_w_down"] = din("moe_w_down", [4, 32, 256, D])
    T["ple_w_gate"] = din("ple_w_gate", [4, D, D])
    T["ple_w_proj"] = din("ple_w_proj", [4, 256, D])
    T["ident"] = din("ident", [128, 128])
    T["ones"] = din("ones", [128, 128])
    T["trimask"] = din("trimask", [128, 128])
    T["ntri"] = din("ntri", [128, 128])
    T["masks"] = din("masks", [128, 4, 512])
    xo = nc.dram_tensor("xo", [D, NT], F32, kind="ExternalOutput").ap()
    XS = [nc.dram_tensor("XS%d" % k, [D, NT], F32).ap() for k in range(2)]
    for nm, shp in (("QS", [D, NT]), ("KS", [D, NT]), ("VS", [D, NT]), ("OS", [D, NT]),
                    ("QG", [8, 1024, NT]), ("KG", [8, 1024, NT]), ("VG", [8, 1024, NT]), ("OG", [8, 1024, NT])):
        T[nm] = [nc.dram_tensor("%s%d" % (nm, k), shp, BF16).ap() for k in range(2)]
    for nm in ("QL", "KL", "VL", "OL"):
        T[nm] = nc.dram_tensor(nm, [D, NT], BF16).ap()

    _RANK.clear()
    _VIEWS.clear()
    with ExitStack() as es:
        C = Ctx(nc, es)
        S = C.S
        S.ops['pool'].append((lambda e: (get_rank(e), None)[1], [], None))
        WORDS = 52992
        BIG = es.enter_context(nc.sbuf_tensor("BIG", [128, WORDS], F32))
        sb = Arena(BIG[:, :], WORDS)
        PB = [es.enter_context(nc.psum_tensor("pb%d" % k, [128, 512], F32)) for k in range(8)]
        pk = ['p%d' % k for k in range(8)]

        def make_gather_chunk(j):
            def gather_chunk(nm, k, rkey):
                a = T[nm][j]
                b = T[nm[0] + "G"][j]
                S.cc(lambda e: e.collective_compute("AllGather", ALU.bypass, replica_groups=GROUPS,
                                                    ins=[a[k * 256:(k + 1) * 256, :].opt()], outs=[b[k].opt()]), [rkey], [(nm[0] + "G", k)])
            return gather_chunk

        def pick_all(j, names):
            for nm in names:
                b = T[nm + "G"][j]
                loc = T[nm + "L"]

                def pick(e, two, b=b, loc=loc):
                    mine = b.rearrange("(g two) (r xp) t -> g r two xp t", two=2, r=4)[bass.ds(get_rank(e), 1)].squeeze(0)
                    return e.dma_start(out=loc.rearrange("(r two xp) t -> r two xp t", r=4, two=2)[:, two], in_=mine[:, two])
                for two in range(2):
                    C.DMAF('pool', lambda e, two=two, pick=pick: pick(e, two), [(nm + "G", k) for k in range(8)], [nm + "L"])

        for i in range(4):
            src = T["xT"] if i == 0 else XS[(i - 1) % 2]
            dst = xo if i == 3 else XS[i % 2]
            j = i // 2
            if i % 2 == 0:
                sb.reset()
                gc = make_gather_chunk(j)
                emit_p1(C, sb, PB, pk, T, j, i, src, gc)
                S.barrier()
                pick_all(j, ["Q", "K", "V"])
                sb.reset()
                emit_p2(C, sb, PB, pk, T, j, gc)
                S.barrier()
                pick_all(j, ["O"])
                sb.reset()
                emit_tok(C, sb, PB, pk, T, 'attn', i, src, dst, False)
                S.barrier()
            else:
                sb.reset()
                emit_tok(C, sb, PB, pk, T, 'sg', i, src, dst, i == 3)
                S.barrier()
        S.emit()
    return nc


def fm(g):
    return np.ascontiguousarray(g.reshape(16, 128).T.astype(np.float32))


def kernel(**inp):
    inp = {k: np.asarray(v) for k, v in inp.items()}
    x = np.ascontiguousarray(inp["x"], dtype=np.float32).reshape(16384, 2048)
    p = inp["p"].reshape(4, 16384, 256)
    k = np.arange(128)
    q = np.arange(512)
    shared = {
        "g_mix": np.stack([fm(inp["norm_mix"][i]) for i in range(4)]),
        "g_ffn": np.stack([fm(inp["norm_ffn"][i]) for i in range(4)]),
        "g_pin": np.stack([fm(inp["ple_norm_in"][i]) for i in range(4)]),
        "g_pout": np.stack([fm(inp["ple_norm_out"][i]) for i in range(4)]),
        "gq": np.ascontiguousarray(inp["sb_q_norm"].reshape(2, 128, 1)),
        "gk": np.ascontiguousarray(inp["sb_k_norm"].reshape(2, 128, 1)),
        "vg": np.stack([fm(inp["sg_v_norm"][j]) for j in range(2)]),
        "wsT": np.ascontiguousarray(np.transpose(inp["sg_w_s"], (0, 3, 1, 2))),
        "bs": np.ascontiguousarray(np.broadcast_to(inp["sg_b_s"][:, None], (2, 128, 16, 128))),
        "wr": np.ascontiguousarray(np.concatenate([inp["moe_w_group"], inp["moe_w_expert"]], axis=2)),
        "br": np.ascontiguousarray(np.broadcast_to(np.concatenate([inp["moe_b_group"], inp["moe_b_expert"]], axis=1)[:, None, :], (4, 128, 36))),
        "sb_w_in": inp["sb_w_in"], "sb_w_out": inp["sb_w_out"], "sg_w_in": inp["sg_w_in"], "sg_w_out": inp["sg_w_out"],
        "moe_w_gate": inp["moe_w_gate"], "moe_w_up": inp["moe_w_up"], "moe_w_down": inp["moe_w_down"],
        "ple_w_gate": inp["ple_w_gate"], "ple_w_proj": inp["ple_w_proj"],
        "ident": np.eye(128, dtype=np.float32), "ones": np.ones((128, 128), np.float32),
        "trimask": (k[:, None] <= k[None, :]).astype(np.float32),
        "ntri": -(k[:, None] >= k[None, :]).astype(np.float32),
        "masks": np.ascontiguousarray(np.stack([((k[:, None] + o * 128) < q[None, :]).astype(np.float32) for o in range(4)], axis=1)),
    }
    maps = []
    for c in range(N_CORES):
        m = dict(shared)
        m["xT"] = np.ascontiguousarray(x[c * NT:(c + 1) * NT].T)
        m["pT"] = np.ascontiguousarray(np.transpose(p[:, c * NT:(c + 1) * NT, :], (0, 2, 1)))
        maps.append(m)
    nc = build_fused()
    res = run_bass_kernel_spmd(nc, maps, core_ids=list(range(N_CORES)))
    out = np.concatenate([res.results[c]["xo"].T for c in range(N_CORES)], axis=0).reshape(2, 8192, 2048)
    return np.ascontiguousarray(out, dtype=np.float32)
```

```python
import numpy as np
import concourse.bass as bass
import concourse.mybir as mybir
from concourse.bass_utils import run_bass_kernel_spmd
from contextlib import ExitStack

F32 = mybir.dt.float32
BF16 = mybir.dt.bfloat16
AF = mybir.ActivationFunctionType
ALU = mybir.AluOpType
AX = mybir.AxisListType

ENGS = ['pe', 'act', 'dve', 'pool', 'sp']
DMAQ = ('sp', 'act', 'pool')


class Sched:
    def __init__(self, nc, es, R=4):
        self.nc = nc
        self.sem = {e: es.enter_context(nc.semaphore('s_' + e)) for e in ENGS}
        self.cnt = {e: 0 for e in ENGS}
        self.ops = {e: [] for e in ENGS}
        self.waited = {e: {} for e in ENGS}
        self.lastw = {}
        self.readers = {}
        self.R = R
        self.dsem = {q: [es.enter_context(nc.semaphore('d_%s%d' % (q, i))) for i in range(R)] for q in DMAQ}
        self.dcnt = {q: 0 for q in DMAQ}
        self.semobj = {}
        self.ccn = 0
        self.semobj[('cc',)] = es.enter_context(nc.semaphore('s_cc'))
        for e in ENGS:
            self.semobj[('c', e)] = self.sem[e]
        for q in DMAQ:
            for i in range(R):
                self.semobj[('d', q, i)] = self.dsem[q][i]

    def _deps(self, eng, reads, writes, skip_sem=None):
        deps = {}

        def add(tok):
            if tok is None:
                return
            s, v = tok
            if s == skip_sem:
                return
            if deps.get(s, 0) < v:
                deps[s] = v
        for k in reads:
            for s, v in self.lastw.get(k, {}).items():
                add((s, v))
        for k in writes:
            for s, v in self.lastw.get(k, {}).items():
                add((s, v))
            for s, v in self.readers.get(k, {}).items():
                add((s, v))
        out = []
        w = self.waited[eng]
        for s, v in deps.items():
            if w.get(s, 0) < v:
                w[s] = v
                out.append((s, v))
        return out

    def _commit(self, tok, reads, writes):
        s, v = tok
        for k in reads:
            r = self.readers.setdefault(k, {})
            if r.get(s, 0) < v:
                r[s] = v
        for k in writes:
            d = self.lastw.setdefault(k, {})
            if d.get(s, 0) < v:
                d[s] = v
            self.readers[k] = {}

    def op(self, eng, fn, reads=(), writes=()):
        skip = ('c', 'pe') if eng == 'pe' else None
        waits = self._deps(eng, reads, writes, skip)
        self.cnt[eng] += 1
        tok = (('c', eng), self.cnt[eng])
        self.ops[eng].append((fn, waits, (('c', eng), 1)))
        self._commit(tok, reads, writes)
        return tok

    def dma(self, q, fn, reads=(), writes=()):
        i = self.dcnt[q]
        self.dcnt[q] += 1
        slot = i % self.R
        semk = ('d', q, slot)
        waits = self._deps(q, reads, writes)
        prev = 16 * (i // self.R)
        if prev > 0 and self.waited[q].get(semk, 0) < prev:
            self.waited[q][semk] = prev
            waits.append((semk, prev))
        tok = (semk, prev + 16)
        self.ops[q].append((fn, waits, (semk, 16)))
        self._commit(tok, reads, writes)
        return tok


    def cc(self, fn, reads=(), writes=()):
        waits = self._deps('pool', reads, writes)
        self.ccn += 1
        tok = (('cc',), self.ccn)
        self.ops['pool'].append((fn, waits, (('cc',), 1)))
        self._commit(tok, reads, writes)
        return tok

    def barrier(self):
        latest = {}
        for e in ENGS:
            if self.cnt[e] > 0:
                latest[('c', e)] = self.cnt[e]
        for q in DMAQ:
            n = self.dcnt[q]
            for slot in range(self.R):
                k = (n - slot + self.R - 1) // self.R if n > slot else 0
                if k > 0:
                    latest[('d', q, slot)] = 16 * k
        if self.ccn > 0:
            latest[('cc',)] = self.ccn
        for e in ENGS:
            waits = []
            for s, v in latest.items():
                if self.waited[e].get(s, 0) < v:
                    self.waited[e][s] = v
                    waits.append((s, v))
            self.ops[e].append((None, waits, None))
        self.lastw.clear()
        self.readers.clear()

    def wait_all(self, eng, toks):
        waits = []
        for s, v in toks:
            if self.waited[eng].get(s, 0) < v:
                self.waited[eng][s] = v
                waits.append((s, v))
        self.ops[eng].append((None, waits, None))

    def emit(self):
        nc = self.nc
        S = self
        with nc.Block() as block:
            def run(e, name):
                for fn, waits, inc in S.ops[name]:
                    for s, v in waits:
                        e.wait_ge(S.semobj[s], v)
                    if fn is not None:
                        ins = fn(e)
                        if inc is not None:
                            ins.then_inc(S.semobj[inc[0]], inc[1])

            @block.sync
            def _(e):
                run(e, 'sp')

            @block.tensor
            def _(e):
                run(e, 'pe')

            @block.scalar
            def _(e):
                run(e, 'act')

            @block.vector
            def _(e):
                run(e, 'dve')

            @block.gpsimd
            def _(e):
                run(e, 'pool')


D = 2048
NT = 2048
EPS = 1e-6
MUL = ALU.mult
ADD = ALU.add
N_CORES = 8
GROUPS = [[0, 1, 2, 3], [4, 5, 6, 7]]


class Ctx:
    def __init__(self, nc, es):
        self.nc = nc
        self.es = es
        self.S = Sched(nc, es)

    def MM(self, out, lhsT, rhs, start, stop, r, w):
        self.S.op('pe', lambda e: e.matmul(out, lhsT, rhs, start=start, stop=stop), r, w)

    def ACT(self, out, in_, func, r, w, **kw):
        self.S.op('act', lambda e: e.activation(out=out, in_=in_, func=func, **kw), r, w)

    def TT(self, eng, out, a, b, op, r, w):
        self.S.op(eng, lambda e: e.tensor_tensor(out=out, in0=a, in1=b, op=op), r, w)

    def TS(self, eng, out, a, s1, s2, op0, op1, r, w):
        if s2 is None:
            self.S.op(eng, lambda e: e.tensor_scalar(out=out, in0=a, scalar1=s1, scalar2=None, op0=op0), r, w)
        else:
            self.S.op(eng, lambda e: e.tensor_scalar(out=out, in0=a, scalar1=s1, scalar2=s2, op0=op0, op1=op1), r, w)

    def STT(self, out, a, s, b, op0, op1, r, w):
        self.S.op('dve', lambda e: e.scalar_tensor_tensor(out=out, in0=a, scalar=s, in1=b, op0=op0, op1=op1), r, w)

    def DMA(self, q, out, in_, r, w):
        return self.S.dma(q, lambda e: e.dma_start(out=out, in_=in_), r, w)

    def DMAF(self, q, fn, r, w):
        return self.S.dma(q, fn, r, w)

    def OP(self, eng, fn, r, w):
        self.S.op(eng, fn, r, w)


class Arena:
    def __init__(self, big, words):
        self.big = big
        self.words = words
        self.off = 0

    def reset(self):
        self.off = 0

    def __call__(self, name, shape, dt):
        n = 1
        for s in shape[1:]:
            n *= s
        if dt == BF16:
            w = (n + 1) // 2
            ap = self.big[:, self.off:self.off + w].bitcast(BF16)
        else:
            w = n
            ap = self.big[:, self.off:self.off + w]
        self.off += (w + 15) // 16 * 16
        assert self.off <= self.words, (name, self.off, self.words)
        if len(shape) == 3:
            ap = ap.rearrange("p (a b) -> p a b", a=shape[1])
        elif len(shape) == 4:
            ap = ap.rearrange("p (a b c) -> p a b c", a=shape[1], b=shape[2])
        return ap


_RANK = {}


def get_rank(e):
    if 'r' not in _RANK:
        _RANK['r'] = e.snap(e.partition_id() % 4, min_val=0, max_val=3)
    return _RANK['r']


_VIEWS = {}


def dyn_view(e, key, mk):
    if key not in _VIEWS:
        _VIEWS[key] = mk(get_rank(e))
    return _VIEWS[key]


def fm_view(ap2d):
    return ap2d.rearrange("(c q) t -> q c t", q=128)


def emit_p1(C, sb, PB, pk, T, j, i, src, gather_chunk):
    S = C.S
    xT_v = fm_view(src)
    win_v = T["sb_w_in"][j].rearrange("(c q) n -> q c n", q=128)
    QS, KS, VS = T["QS"][j], T["KS"][j], T["VS"][j]
    VS3 = VS.rearrange("(h a) (b e) -> h (a b) e", h=16, b=16, e=128)
    XB = [sb("XB%d" % k, [128, 16, 512], F32) for k in range(2)]
    HB = sb("HB", [128, 16, 2048], BF16)
    WS = [sb("WS%d" % k, [128, 16, 512], BF16) for k in range(2)]
    SQ = sb("SQ", [128, 2, 512], BF16)
    RS = sb("RS", [128, 2, 512], F32)
    OB = sb("OB", [128, 4, 512], BF16)
    ONESB = sb("ONESB", [128, 128], BF16)
    G_MIX = sb("G_MIX", [128, 16], F32)
    GQK = sb("GQK", [128, 2], F32)
    C.DMA('pool', ONESB, T["ones"], [], ['ONESB'])
    C.DMA('sp', G_MIX, T["g_mix"][i], [], ['G_MIX'])
    C.DMA('sp', GQK[:, 0:1], T["gq"][j], [], ['GQK'])
    C.DMA('sp', GQK[:, 1:2], T["gk"][j], [], ['GQK'])
    groups = [(w, g) for w in range(3) for g in range(4)]

    def wload(n):
        w, g = groups[n]
        C.DMA('pool', WS[n % 2], win_v[:, :, w * 2048 + g * 512:w * 2048 + (g + 1) * 512], [], ['WS%d' % (n % 2)])
    wload(0)
    for blk in range(4):
        g0 = blk * 512
        xb = XB[blk % 2]
        C.DMA('sp', xb, xT_v[:, :, g0:g0 + 512], [], [('XB', blk % 2, c) for c in range(16)])
        for c in range(16):
            sqb = SQ[:, c % 2, :]
            C.ACT(sqb, xb[:, c, :], AF.Square, [('XB', blk % 2, c)], [('SQ', c % 2)])
            C.MM(PB[7][:], ONESB, sqb, c == 0, c == 15, [('SQ', c % 2), 'ONESB'], [pk[7]])
        C.ACT(RS[:, 0, :], PB[7][:], AF.Sqrt, [pk[7]], [('RS', 0)], scale=1.0 / D, bias=EPS)
        C.OP('dve', lambda e: e.reciprocal(RS[:, 0, :], RS[:, 0, :]), [('RS', 0)], [('RS', 0)])
        for c in range(16):
            C.STT(HB[:, c, g0:g0 + 512], xb[:, c, :], G_MIX[:, c:c + 1], RS[:, 0, :], MUL, MUL,
                  [('XB', blk % 2, c), ('RS', 0), 'G_MIX'], [('HB', blk, c)])
    oi = 0
    for n, (w, g) in enumerate(groups):
        ws = WS[n % 2]
        wk = 'WS%d' % (n % 2)
        if n + 1 < len(groups):
            wload(n + 1)
        for blk in range(4):
            g0 = blk * 512
            if w < 2:
                dst = QS if w == 0 else KS
                dk = ('QS', g) if w == 0 else ('KS', g)
                for mi in range(4):
                    hd = g * 4 + mi
                    bi = hd % 2
                    for c in range(16):
                        C.MM(PB[bi][:], ws[:, c, mi * 128:(mi + 1) * 128], HB[:, c, g0:g0 + 512], c == 0, c == 15, [wk, ('HB', blk, c)], [pk[bi]])
                    sqb = SQ[:, bi, :]
                    C.ACT(sqb, PB[bi][:], AF.Square, [pk[bi]], [('SQ', bi)])
                    C.MM(PB[2 + bi][:], ONESB, sqb, True, True, [('SQ', bi), 'ONESB'], [pk[2 + bi]])
                    rk = ('RS', 1)
                    if w == 0:
                        C.ACT(RS[:, 1, :], PB[2 + bi][:], AF.Sqrt, [pk[2 + bi]], [rk], scale=1.0, bias=128.0 * EPS)
                    else:
                        C.ACT(RS[:, 1, :], PB[2 + bi][:], AF.Sqrt, [pk[2 + bi]], [rk], scale=1.0 / 128, bias=EPS)
                    C.OP('dve', lambda e: e.reciprocal(RS[:, 1, :], RS[:, 1, :]), [rk], [rk])
                    ok = ('OB', oi % 4)
                    ob = OB[:, oi % 4, :]
                    oi += 1
                    C.STT(ob, PB[bi][:], GQK[:, w:w + 1], RS[:, 1, :], MUL, MUL, [pk[bi], rk, 'GQK'], [ok])
                    C.DMA('sp', dst[hd * 128:(hd + 1) * 128, g0:g0 + 512], ob, [ok], [dk])
            else:
                for sx in range(4):
                    bi = 4 + (sx % 2)
                    for c in range(16):
                        C.MM(PB[bi][:], HB[:, c, g0 + sx * 128:g0 + (sx + 1) * 128], ws[:, c, :], c == 0, c == 15, [wk, ('HB', blk, c)], [pk[bi]])
                    ok = ('OB', oi % 4)
                    ob = OB[:, oi % 4, :]
                    oi += 1
                    C.ACT(ob, PB[bi][:], AF.Copy, [pk[bi]], [ok])
                    t0 = g0 + sx * 128
                    C.DMA('sp', VS3[g * 4:(g + 1) * 4, t0:t0 + 128, :].rearrange("h t e -> t h e"),
                          ob.rearrange("p (h e) -> p h e", h=4), [ok], [('VS', g)])
        nm = ("QS", "KS", "VS")[w]
        gather_chunk(nm, 2 * g, (nm, g))
        gather_chunk(nm, 2 * g + 1, (nm, g))


def emit_p2(C, sb, PB, pk, T, j, gather_chunk, PP):
    S = C.S
    SEQ = 8192
    NKB = SEQ // 128
    NQB = SEQ // 512
    OS = T["OS"][j]
    QL, KL, VL = T["QL"], T["KL"], T["VL"]
    VL4 = VL.rearrange("(r pr a) (b e) -> r pr (a b) e", r=4, pr=4, b=16, e=128)
    QT = [sb("QT%d" % k, [128, SEQ], BF16) for k in range(2)]
    KT = [sb("KT%d" % k, [128, SEQ], BF16) for k in range(2)]
    V = [sb("V%d" % k, [128, NKB, 128], BF16) for k in range(2)]
    NTRI = sb("NTRI", [128, 128], BF16)
    ONESB = sb("ONESB", [128, 128], BF16)
    MASK = sb("MASK", [128, 4, 512], BF16)
    EB = sb("EB", [128, 2, 1024], F32)
    SPB = sb("SPB", [128, 2, 1024], BF16)
    ARG = sb("ARG", [128, 2, 1024], F32)
    WB = sb("WB", [128, 2, 1024], BF16)
    R = sb("R", [128, 512], F32)
    OB = sb("OB", [128, 2, 512], BF16)
    C.DMA('pool', NTRI, T["ntri"], [], ['NTRI'])
    C.DMA('pool', ONESB, T["ones"], [], ['ONESB'])
    C.DMA('pool', MASK, T["masks"], [], ['MASK'])
    def loads(pr):
        d = pr % 2
        for r in range(4):
            C.DMA('pool', QT[d][:, r * 2048:(r + 1) * 2048], QL[r * 512 + pr * 128:r * 512 + (pr + 1) * 128, :], ['QL'], [('QT', d)])
            C.DMA('pool', KT[d][:, r * 2048:(r + 1) * 2048], KL[r * 512 + pr * 128:r * 512 + (pr + 1) * 128, :], ['KL'], [('KT', d)])
            C.DMA('pool', V[d][:, r * 16:(r + 1) * 16, :], VL4[r, pr].rearrange("(kb q) e -> q kb e", q=128), ['VL'], [('V', d)])

    PZ, PA = PP[0], PP[1]
    NEGONES = sb("NEGONES", [128, 128], BF16)
    C.TS('pool', NEGONES, ONESB, -1.0, None, MUL, None, ['ONESB'], ['NEGONES'])
    its = []
    grp = 0
    for pr in range(4):
        for qb in range(NQB):
            kbs = list(range(4 * qb + 3, -1, -1))
            npair = len(kbs) // 2
            for n in range(npair):
                its.append(dict(pr=pr, d=pr % 2, qb=qb, q0=qb * 512, hi=kbs[2 * n], lo=kbs[2 * n + 1], n=n, last=(n == npair - 1),
                                diag=(n < 2), g=grp, first_of_pair=(qb == 0 and n == 0)))
            grp += 1
    N = len(its)

    def stageA(k):
        t = its[k]
        i2 = k % 2
        d = t['d']
        qslice = QT[d][:, t['q0']:t['q0'] + 512]
        for h, kb in enumerate((t['hi'], t['lo'])):
            C.MM(PZ[:, h * 512:(h + 1) * 512], KT[d][:, kb * 128:(kb + 1) * 128], qslice, True, True, [('KT', d), ('QT', d)], ['pz'])
        C.ACT(EB[:, i2, :], PZ, AF.Exp, ['pz'], [('EB', i2)])
        C.ACT(SPB[:, i2, :], EB[:, i2, :], AF.Ln, [('EB', i2)], [('SPB', i2)], bias=1.0)
        if t['diag']:
            mk = MASK[:, 2 * t['n']:2 * t['n'] + 2, :].rearrange("p a b -> p (a b)")
            C.TT('pool', SPB[:, i2, :], SPB[:, i2, :], mk, MUL, [('SPB', i2), 'MASK'], [('SPB', i2)])

    def stageB1(k):
        t = its[k]
        i2 = k % 2
        d = t['d']
        tb = 4 + i2
        qslice = QT[d][:, t['q0']:t['q0'] + 512]
        sp_hi = SPB[:, i2, 0:512]
        sp_lo = SPB[:, i2, 512:1024]
        if t['n'] == 0:
            C.OP('dve', lambda e: e.memset(R, 0.0), [], ['R'])
        C.MM(PA[:, 0:512], KT[d][:, t['hi'] * 128:(t['hi'] + 1) * 128], qslice, True, False, [('KT', d), ('QT', d)], ['pa'])
        C.MM(PA[:, 0:512], NTRI, sp_hi, False, True, ['NTRI', ('SPB', i2)], ['pa'])
        C.MM(PA[:, 512:1024], KT[d][:, t['lo'] * 128:(t['lo'] + 1) * 128], qslice, True, False, [('KT', d), ('QT', d)], ['pa'])
        C.MM(PA[:, 512:1024], NTRI, sp_lo, False, False, ['NTRI', ('SPB', i2)], ['pa'])
        C.MM(PA[:, 512:1024], NEGONES, sp_hi, False, True, ['NEGONES', ('SPB', i2)], ['pa'])
        if not t['last']:
            C.MM(PB[tb][:], ONESB, sp_hi, True, False, ['ONESB', ('SPB', i2)], [pk[tb]])
            C.MM(PB[tb][:], ONESB, sp_lo, False, True, ['ONESB', ('SPB', i2)], [pk[tb]])
        for h in range(2):
            C.TT('dve', ARG[:, i2, h * 512:(h + 1) * 512], PA[:, h * 512:(h + 1) * 512], R, ALU.subtract, ['pa', 'R'], [('ARG', i2, h)])
        C.ACT(WB[:, i2, :], ARG[:, i2, :], AF.Exp, [('ARG', i2, 0), ('ARG', i2, 1)], [('WB', i2)])
        if t['diag']:
            mk = MASK[:, 2 * t['n']:2 * t['n'] + 2, :].rearrange("p a b -> p (a b)")
            C.TT('pool', WB[:, i2, :], WB[:, i2, :], mk, MUL, [('WB', i2), 'MASK'], [('WB', i2)])
        if not t['last']:
            C.TT('dve', R, R, PB[tb][:], ADD, [pk[tb], 'R'], ['R'])

    def stageB2(k):
        t = its[k]
        i2 = k % 2
        d = t['d']
        ob_i = t['g'] % 2
        obank = 6 + ob_i
        C.MM(PB[obank][:], V[d][:, t['hi'], :], WB[:, i2, 0:512], t['n'] == 0, False, [('V', d), ('WB', i2)], [pk[obank]])
        C.MM(PB[obank][:], V[d][:, t['lo'], :], WB[:, i2, 512:1024], False, t['last'], [('V', d), ('WB', i2)], [pk[obank]])
        if t['last']:
            C.ACT(OB[:, ob_i, :], PB[obank][:], AF.Copy, [pk[obank]], [('OB', ob_i)])
            tq = t['q0'] // 2048
            row = tq * 512 + t['pr'] * 128
            C.DMA('sp', OS[row:row + 128, (t['q0'] % 2048):(t['q0'] % 2048) + 512], OB[:, ob_i, :], [('OB', ob_i)], [('OS', t['pr'] // 2)])
            if t['qb'] == NQB - 1 and t['pr'] % 2 == 1:
                for tq2 in range(4):
                    gather_chunk("OS", 2 * tq2 + t['pr'] // 2, ('OS', t['pr'] // 2))

    loads(0)
    for sidx in range(N + 2):
        if sidx < N:
            stageA(sidx)
        if 0 <= sidx - 1 < N:
            stageB1(sidx - 1)
        if 0 <= sidx - 2 < N:
            stageB2(sidx - 2)
            if its[sidx - 2]['first_of_pair'] and its[sidx - 2]['pr'] + 1 < 4:
                loads(its[sidx - 2]['pr'] + 1)


def emit_tok(C, sb, PB, pk, T, mode, i, src, dst, final):
    S = C.S
    j = i // 2
    xT_v = fm_view(src)
    xo_v = fm_view(dst)
    w_mo = T["sb_w_out"][j] if mode == 'attn' else T["sg_w_out"][j]
    wmo_v = w_mo.rearrange("(c q) n -> q c n", q=128)
    wpg_v = T["ple_w_gate"][i].rearrange("(c q) n -> q c n", q=128)
    wpp_v = T["ple_w_proj"][i].rearrange("(c q) n -> q c n", q=128)
    pT_v = T["pT"][i].rearrange("(c q) t -> q c t", q=128)
    wr_v = T["wr"][i].rearrange("(c q) n -> q c n", q=128)
    wg_d, wu_d, wd_d = T["moe_w_gate"][i], T["moe_w_up"][i], T["moe_w_down"][i]
    if mode == 'sg':
        w_in_v = T["sg_w_in"][j].rearrange("(c q) n -> q c n", q=128)

    X1 = sb("X1", [128, 16, 1024], F32)
    H = sb("H", [128, 16, 1024], BF16)
    WA = sb("WA", [128, 12288], BF16)
    WB_ = sb("WB", [128, 12288], BF16)
    AR = [WA, WB_]
    HID = sb("HID", [128, 2, 2, 1024], BF16)
    ETF = sb("ETF", [128, 4096], F32)
    ET = ETF.rearrange("p (m t) -> p m t", m=16)
    SQ = sb("SQ", [128, 2, 512], BF16)
    ONESB = sb("ONESB", [128, 128], BF16)
    RS = sb("RS", [128, 512], F32)
    T1 = sb("T1", [128, 2, 512], F32)
    ONESF = sb("ONESF", [128, 128], F32)
    IDF = sb("IDF", [128, 128], F32)
    SL = T1
    GEXP = sb("GEXP", [128, 8, 128], F32)
    G_FFN = sb("G_FFN", [128, 16], F32)
    G_PIN = sb("G_PIN", [128, 16], F32)
    G_POUT = sb("G_POUT", [128, 16], F32)
    WR = sb("WR", [128, 16, 36], BF16)
    BR = sb("BR", [128, 36], F32)
    GATES = sb("GATES", [128, 8, 32], F32)
    LG = sb("LG", [128, 36], F32)
    ME = sb("ME", [128, 32], F32)
    EX = sb("EX", [128, 32], F32)
    SEL = sb("SEL", [128, 32], F32)
    SM = sb("SM", [128, 16], F32)
    M8 = sb("M8", [128, 8], F32)
    PT = sb("PT", [128, 2, 256], BF16)
    if mode == 'sg':
        G_MIX = sb("G_MIX", [128, 16], F32)
        VG = sb("VG", [128, 16], F32)
        WCT = sb("WCT", [128, 16, 128], BF16)
        TRIB = sb("TRIB", [128, 128], BF16)
        BS = sb("BS", [128, 16, 128], F32)
        UT = sb("UT", [128, 512], F32)
        MX = sb("MX", [128, 512], F32)
        SS = sb("SS", [128, 16], F32)
        RV = sb("RV", [128, 4], F32)
        GTD = ETF.bitcast(BF16).rearrange("p (m t) -> p m t", m=16)

    C.DMA('sp', ONESF, T["ones"], [], ['ONESF'])
    C.DMA('pool', ONESB, T["ones"], [], ['ONESB'])
    C.DMA('sp', IDF, T["ident"], [], ['IDF'])
    C.DMA('sp', G_FFN, T["g_ffn"][i], [], ['G_FFN'])
    C.DMA('sp', G_PIN, T["g_pin"][i], [], ['G_PIN'])
    C.DMA('sp', G_POUT, T["g_pout"][i], [], ['G_POUT'])
    C.DMA('sp', BR, T["br"][i], [], ['BR'])
    C.DMA('pool', WR, wr_v, [], ['WR'])
    if mode == 'sg':
        C.DMA('sp', G_MIX, T["g_mix"][i], [], ['G_MIX'])
        C.DMA('sp', VG, T["vg"][j], [], ['VG'])
        C.DMA('sp', BS, T["bs"][j], [], ['BS'])
        C.DMA('pool', WCT, T["wsT"][j], [], ['WCT'])
        C.DMA('pool', TRIB, T["trimask"], [], ['TRIB'])
        for g in range(16):
            C.TT('pool', WCT[:, g, :], WCT[:, g, :], TRIB, MUL, ['WCT', 'TRIB'], ['WCT'])
        C.OP('dve', lambda e: e.memset(SS, 0.0), [], ['SS'])
    C.OP('dve', lambda e: e.memset(SM, 0.0), [], ['SM'])

    arena_i = [0]

    def next_arena():
        a = arena_i[0] % 2
        arena_i[0] += 1
        return AR[a], 'AR%d' % a

    def rms_fm(src_, skey, g_tile, gkey, dst_, dkey, W, bank, bkey):
        for c in range(16):
            sqb = SQ[:, c % 2, 0:W]
            C.ACT(sqb, src_(c), AF.Square, [skey(c)], [('SQ', c % 2)])
            C.MM(bank[:, 0:W], ONESB, sqb, c == 0, c == 15, [('SQ', c % 2), 'ONESB'], [bkey])
        C.ACT(RS[:, 0:W], bank[:, 0:W], AF.Sqrt, [bkey], ['RS'], scale=1.0 / D, bias=EPS)
        C.OP('dve', lambda e: e.reciprocal(RS[:, 0:W], RS[:, 0:W]), ['RS'], ['RS'])
        for c in range(16):
            C.STT(dst_(c), src_(c), g_tile[:, c:c + 1], RS[:, 0:W], MUL, MUL, [skey(c), 'RS', gkey], [dkey(c)])

    gelu_i = [0]

    def gelu(bank, bkey, W, out, okeys, sscol=None):
        k = gelu_i[0] % 2
        gelu_i[0] += 1
        t = T1[:, k, 0:W]
        tk = ('T1', k)
        C.ACT(t, bank, AF.Square, [bkey], [tk])
        C.TS('dve', t, t, 0.044715, 1.0, MUL, ADD, [tk], [tk])
        C.TT('dve', t, t, bank, MUL, [tk, bkey], [tk])
        C.ACT(t, t, AF.Sigmoid, [tk], [tk], scale=1.5957691216057308)
        C.TT('dve', out, t, bank, MUL, [tk, bkey], okeys)
        if sscol is not None:
            C.ACT(t, out, AF.Square, okeys, [tk, 'SS'], accum_out=sscol)

    def wst_view(ar, n):
        return ar[:, 0:16 * n].rearrange("p (c f) -> p c f", c=16)

    out_toks = []
    for p in range(2):
        for b in range(2):
            g0 = p * 1024 + b * 512
            bl = slice(b * 512, (b + 1) * 512)
            ol = slice((1 - b) * 512, (2 - b) * 512)
            xk = [('X1', b, c) for c in range(16)]
            ok = [('H', 1 - b, c) for c in range(16)]
            C.DMA('sp', X1[:, :, bl], xT_v[:, :, g0:g0 + 512], [], xk)
            if mode == 'attn':
                C.DMA('pool', H[:, :, ol], fm_view(T["OL"])[:, :, g0:g0 + 512], ['OL'], ok)
                src_keys = ok
                srcf = lambda c: H[:, c, ol]
            else:
                rms_fm(lambda c: X1[:, c, bl], lambda c: ('X1', b, c), G_MIX, 'G_MIX',
                       lambda c: H[:, c, bl], lambda c: ('H', b, c), 512, PB[7], pk[7])
                C.OP('dve', lambda e: e.memset(SS, 0.0), [], ['SS'])
                for jv in range(4):
                    ar, ak = next_arena()
                    wst = wst_view(ar, 512)
                    C.DMA('pool', wst, w_in_v[:, :, 2048 + jv * 512:2048 + (jv + 1) * 512], [], [ak, ak + 'u'])
                    for s in range(4):
                        bi = (jv * 4 + s) % 2
                        for c in range(16):
                            C.MM(PB[bi][:], H[:, c, b * 512 + s * 128:b * 512 + (s + 1) * 128], wst[:, c, :],
                                 c == 0, c == 15, [('H', b, c), ak, ak + 'u'], [pk[bi]])
                        gelu(PB[bi][:], pk[bi], 512, H[:, s * 4 + jv, ol], [('H', 1 - b, s * 4 + jv)], sscol=SS[:, s * 4 + jv:s * 4 + jv + 1])
                C.OP('dve', lambda e: e.tensor_reduce(out=RV, in_=SS.rearrange("p (s j) -> p s j", j=4), axis=AX.X, op=ADD), ['SS'], ['RV'])
                C.ACT(RV, RV, AF.Sqrt, ['RV'], ['RV'], scale=1.0 / 2048, bias=EPS)
                C.OP('dve', lambda e: e.reciprocal(RV, RV), ['RV'], ['RV'])
                for s in range(4):
                    for jv in range(4):
                        vk = ('H', 1 - b, s * 4 + jv)
                        C.TS('dve', H[:, s * 4 + jv, ol], H[:, s * 4 + jv, ol], RV[:, s:s + 1], None, MUL, None, [vk, 'RV'], [vk])
                for mg in range(4):
                    ar, ak = next_arena()
                    wst = wst_view(ar, 512)
                    C.DMA('pool', wst, w_in_v[:, :, mg * 512:(mg + 1) * 512], [], [ak, ak + 'u'])
                    for mi in range(4):
                        m = mg * 4 + mi
                        bu = 2 + (m % 2) * 2
                        bm = 3 + (m % 2) * 2
                        for c in range(16):
                            C.MM(PB[bu][:], wst[:, c, mi * 128:(mi + 1) * 128], H[:, c, bl], c == 0, c == 15,
                                 [ak, ak + 'u', ('H', b, c)], [pk[bu]])
                        for s in range(4):
                            vk = ('H', 1 - b, s * 4 + m // 4)
                            o0 = (1 - b) * 512 + (m % 4) * 128
                            C.MM(PB[bm][:, s * 128:(s + 1) * 128], H[:, s * 4 + m // 4, o0:o0 + 128], WCT[:, m, :], True, True,
                                 [vk, 'WCT'], [pk[bm]])
                        gelu(PB[bu][:], pk[bu], 512, UT, ['UT'])
                        for s in range(4):
                            C.STT(MX[:, s * 128:(s + 1) * 128], PB[bm][:, s * 128:(s + 1) * 128], VG[:, m:m + 1], BS[:, m, :], MUL, ADD,
                                  [pk[bm], 'VG', 'BS'], ['MX'])
                        C.TT('dve', GTD[:, m, :], UT, MX, MUL, ['UT', 'MX'], [('GTD', m)])
                src_keys = [('GTD', c) for c in range(16)]
                srcf = lambda c: GTD[:, c, :]
            for mg in range(4):
                ar, ak = next_arena()
                wst = wst_view(ar, 512)
                C.DMA('pool', wst, wmo_v[:, :, mg * 512:(mg + 1) * 512], [], [ak, ak + 'u'])
                for mi in range(4):
                    m = mg * 4 + mi
                    bi = m % 2
                    for c in range(16):
                        C.MM(PB[bi][:], wst[:, c, mi * 128:(mi + 1) * 128], srcf(c), c == 0, c == 15, [ak, ak + 'u', src_keys[c]], [pk[bi]])
                    C.TT('dve', X1[:, m, bl], X1[:, m, bl], PB[bi][:], ADD, [pk[bi], ('X1', b, m)], [('X1', b, m)])
        for b in range(2):
            bl = slice(b * 512, (b + 1) * 512)
            rms_fm(lambda c: X1[:, c, bl], lambda c: ('X1', b, c), G_FFN, 'G_FFN',
                   lambda c: H[:, c, bl], lambda c: ('H', b, c), 512, PB[7], pk[7])
        for sub in range(8):
            b = sub // 4
            for c in range(16):
                C.MM(PB[6][:, 0:36], H[:, c, sub * 128:(sub + 1) * 128], WR[:, c, :], c == 0, c == 15, [('H', b, c), 'WR'], [pk[6]])
            C.TT('dve', LG, PB[6][:, 0:36], BR, ADD, [pk[6], 'BR'], ['LG'])
            C.OP('dve', lambda e: e.tensor_reduce(out=SM[:, 0:1], in_=LG[:, 0:4], axis=AX.X, op=ALU.max), ['LG'], ['SM'])
            C.TS('dve', SM[:, 1:2], SM[:, 0:1], -1.0, None, MUL, None, ['SM'], ['SM'])
            C.OP('dve', lambda e: e.memset(SM[:, 2:3], 0.0), [], ['SM'])
            C.ACT(EX[:, 0:4], LG[:, 0:4], AF.Exp, ['LG', 'SM'], ['EX', 'SM'], bias=SM[:, 1:2], accum_out=SM[:, 2:3])
            C.TS('dve', SEL[:, 0:4], LG[:, 0:4], SM[:, 0:1], None, ALU.is_equal, None, ['LG', 'SM'], ['SEL'])
            C.TS('dve', SEL[:, 0:4], SEL[:, 0:4], -1.0, 1.0e4, ADD, MUL, ['SEL'], ['SEL'])
            for g in range(4):
                C.TS('dve', ME[:, g * 8:(g + 1) * 8], LG[:, 4 + g * 8:4 + (g + 1) * 8], SEL[:, g:g + 1], None, ADD, None, ['LG', 'SEL'], ['ME'])
            C.OP('dve', lambda e: e.max(out=M8, in_=ME), ['ME'], ['M8'])
            C.TS('dve', SM[:, 3:4], M8[:, 0:1], -1.0, None, MUL, None, ['M8'], ['SM'])
            C.ACT(EX, ME, AF.Exp, ['ME', 'SM'], ['EX'], bias=SM[:, 3:4])
            C.TS('dve', SEL, ME, M8[:, 1:2], None, ALU.is_ge, None, ['ME', 'M8'], ['SEL'])
            C.TT('dve', EX, EX, SEL, MUL, ['EX', 'SEL'], ['EX'])
            C.OP('dve', lambda e: e.tensor_reduce(out=SM[:, 4:5], in_=EX, axis=AX.X, op=ADD), ['EX'], ['SM'])
            C.TT('dve', SM[:, 5:6], SM[:, 4:5], SM[:, 2:3], MUL, ['SM'], ['SM'])
            C.OP('dve', lambda e: e.reciprocal(SM[:, 6:7], SM[:, 5:6]), ['SM'], ['SM'])
            C.TS('dve', GATES[:, sub, :], EX, SM[:, 6:7], None, MUL, None, ['EX', 'SM'], [('GATES', sub)])
        def gexp(ex_):
            for sub in range(8):
                C.TS('dve', GEXP[:, sub, :], ONESF, GATES[:, sub, ex_:ex_ + 1], None, MUL, None, ['ONESF', ('GATES', sub)], [('GEXP', sub)])

        for ex in range(32):
            ar, ak = next_arena()
            Wg = ar[:, 0:4096].rearrange("p (c f) -> p c f", c=16)
            Wu = ar[:, 4096:8192].rearrange("p (c f) -> p c f", c=16)
            Wd = ar[:, 8192:12288].rearrange("p (c f) -> p c f", c=2)
            C.DMA('pool', Wg, wg_d[ex].rearrange("(c q) f -> q c f", q=128), [], [ak])
            C.DMA('pool', Wu, wu_d[ex].rearrange("(c q) f -> q c f", q=128), [], [ak + 'u'])
            C.DMA('pool', Wd, wd_d[ex].rearrange("(c q) f -> q c f", q=128), [], [ak + 'd'])
            hp = ex % 2
            if ex == 0:
                gexp(0)
            for sub in range(8):
                C.MM(PB[6 + sub // 4][:, (sub % 4) * 128:(sub % 4 + 1) * 128], GEXP[:, sub, :], IDF, True, True,
                     [('GEXP', sub), 'IDF'], [pk[6 + sub // 4]])
            for b in range(2):
                bl = slice(b * 512, (b + 1) * 512)
                for jf in range(2):
                    k = (b * 2 + jf) % 2
                    for c in range(16):
                        C.MM(PB[2 * k][:], Wg[:, c, jf * 128:(jf + 1) * 128], H[:, c, bl], c == 0, c == 15, [ak, ('H', b, c)], [pk[2 * k]])
                    for c in range(16):
                        C.MM(PB[2 * k + 1][:], Wu[:, c, jf * 128:(jf + 1) * 128], H[:, c, bl], c == 0, c == 15, [ak + 'u', ('H', b, c)], [pk[2 * k + 1]])
                    slk = ('T1', k)
                    C.ACT(SL[:, k, :], PB[2 * k][:], AF.Silu, [pk[2 * k]], [slk])
                    C.TT('dve', SL[:, k, :], SL[:, k, :], PB[2 * k + 1][:], MUL, [slk, pk[2 * k + 1]], [slk])
                    C.TT('dve', HID[:, hp, jf, bl], SL[:, k, :], PB[6 + b][:], MUL, [slk, pk[6 + b]], [('HID', hp, jf, b)])
            if ex + 1 < 32:
                gexp(ex + 1)
            for b in range(2):
                bl = slice(b * 512, (b + 1) * 512)
                for m in range(16):
                    bi = 4 + (m % 2)
                    for jf in range(2):
                        C.MM(PB[bi][:], Wd[:, jf, m * 128:(m + 1) * 128], HID[:, hp, jf, bl], jf == 0, jf == 1,
                             [ak + 'd', ('HID', hp, jf, b)], [pk[bi]])
                    C.TT('dve', X1[:, m, bl], X1[:, m, bl], PB[bi][:], ADD, [pk[bi], ('X1', b, m)], [('X1', b, m)])
        WPP = WB_[:, 8192:12288].rearrange("p (c f) -> p c f", c=2)
        C.DMA('pool', WPP, wpp_v, [], ['AR1d'])
        for sbk in range(4):
            b = sbk // 2
            g0 = p * 1024 + sbk * 256
            cl = slice(sbk * 256, (sbk + 1) * 256)
            C.DMA('pool', PT, pT_v[:, :, g0:g0 + 256], [], ['PT'])
            hkey = lambda c: ('H', b, c)
            rms_fm(lambda c: X1[:, c, cl], lambda c: ('X1', b, c), G_PIN, 'G_PIN',
                   lambda c: H[:, c, cl], hkey, 256, PB[7], pk[7])
            for mg in range(8):
                wk = 'AR0' if mg % 2 == 0 else 'AR0u'
                wst = WA[:, (mg % 2) * 4096:(mg % 2 + 1) * 4096].rearrange("p (c f) -> p c f", c=16)
                C.DMA('pool', wst, wpg_v[:, :, mg * 256:(mg + 1) * 256], [], [wk])
                for mi in range(2):
                    m = mg * 2 + mi
                    bg = (m % 2) * 2
                    bp = bg + 1
                    for c in range(16):
                        C.MM(PB[bg][:, 0:256], wst[:, c, mi * 128:(mi + 1) * 128], H[:, c, cl], c == 0, c == 15, [wk, hkey(c)], [pk[bg]])
                    for c in range(2):
                        C.MM(PB[bp][:, 0:256], WPP[:, c, m * 128:(m + 1) * 128], PT[:, c, :], c == 0, c == 1, ['AR1d', 'PT'], [pk[bp]])
                    tk = ('T1', m % 2)
                    C.ACT(T1[:, m % 2, 0:256], PB[bg][:, 0:256], AF.Sigmoid, [pk[bg]], [tk])
                    C.TT('dve', ET[:, m, :], T1[:, m % 2, 0:256], PB[bp][:, 0:256], MUL, [tk, pk[bp]], [('ET', m)])
            for m in range(16):
                sqb = SQ[:, m % 2, 0:256]
                C.ACT(sqb, ET[:, m, :], AF.Square, [('ET', m)], [('SQ', m % 2)])
                C.MM(PB[4][:, 0:256], ONESB, sqb, m == 0, m == 15, [('SQ', m % 2), 'ONESB'], [pk[4]])
            C.ACT(RS[:, 0:256], PB[4][:, 0:256], AF.Sqrt, [pk[4]], ['RS'], scale=1.0 / D, bias=EPS)
            C.OP('dve', lambda e: e.reciprocal(RS[:, 0:256], RS[:, 0:256]), ['RS'], ['RS'])
            for m in range(16):
                C.STT(ET[:, m, :], ET[:, m, :], G_POUT[:, m:m + 1], RS[:, 0:256], MUL, MUL, [('ET', m), 'RS', 'G_POUT'], [('ET', m)])
                C.TT('dve', X1[:, m, cl], X1[:, m, cl], ET[:, m, :], ADD, [('ET', m), ('X1', b, m)], [('X1', b, m)])
            out_toks.append(C.DMA('sp', xo_v[:, :, g0:g0 + 256], X1[:, :, cl], [('X1', b, m) for m in range(16)], ['XDST']))
    if final:
        S.wait_all('sp', out_toks)


def build_fused():
    nc = bass.Bass("TRN2", target_bir_lowering=False)

    def din(n, s):
        return nc.dram_tensor(n, s, F32, kind="ExternalInput").ap()
    T = {}
    T["xT"] = din("xT", [D, NT])
    T["pT"] = din("pT", [4, 256, NT])
    T["g_mix"] = din("g_mix", [4, 128, 16])
    T["g_ffn"] = din("g_ffn", [4, 128, 16])
    T["g_pin"] = din("g_pin", [4, 128, 16])
    T["g_pout"] = din("g_pout", [4, 128, 16])
    T["gq"] = din("gq", [2, 128, 1])
    T["gk"] = din("gk", [2, 128, 1])
    T["vg"] = din("vg", [2, 128, 16])
    T["wsT"] = din("wsT", [2, 128, 16, 128])
    T["bs"] = din("bs", [2, 128, 16, 128])
    T["wr"] = din("wr", [4, D, 36])
    T["br"] = din("br", [4, 128, 36])
    T["sb_w_in"] = din("sb_w_in", [2, D, 3 * D])
    T["sb_w_out"] = din("sb_w_out", [2, D, D])
    T["sg_w_in"] = din("sg_w_in", [2, D, 4096])
    T["sg_w_out"] = din("sg_w_out", [2, D, D])
    T["moe_w_gate"] = din("moe_w_gate", [4, 32, D, 256])
    T["moe_w_up"] = din("moe_w_up", [4, 32, D, 256])
    T["moe_w_down"] = din("moe_w_down", [4, 32, 256, D])
    T["ple_w_gate"] = din("ple_w_gate", [4, D, D])
    T["ple_w_proj"] = din("ple_w_proj", [4, 256, D])
    T["ident"] = din("ident", [128, 128])
    T["ones"] = din("ones", [128, 128])
    T["trimask"] = din("trimask", [128, 128])
    T["ntri"] = din("ntri", [128, 128])
    T["masks"] = din("masks", [128, 4, 512])
    xo = nc.dram_tensor("xo", [D, NT], F32, kind="ExternalOutput").ap()
    XS = [nc.dram_tensor("XS%d" % k, [D, NT], F32).ap() for k in range(2)]
    for nm, shp in (("QS", [D, NT]), ("KS", [D, NT]), ("VS", [D, NT]), ("OS", [D, NT]),
                    ("QG", [8, 1024, NT]), ("KG", [8, 1024, NT]), ("VG", [8, 1024, NT]), ("OG", [8, 1024, NT])):
        T[nm] = [nc.dram_tensor("%s%d" % (nm, k), shp, BF16).ap() for k in range(2)]
    for nm in ("QL", "KL", "VL", "OL"):
        T[nm] = nc.dram_tensor(nm, [D, NT], BF16).ap()

    _RANK.clear()
    _VIEWS.clear()
    with ExitStack() as es:
        C = Ctx(nc, es)
        S = C.S
        S.ops['pool'].append((lambda e: (get_rank(e), None)[1], [], None))
        WORDS = 52992
        BIG = es.enter_context(nc.sbuf_tensor("BIG", [128, WORDS], F32))
        sb = Arena(BIG[:, :], WORDS)
        PPt = [es.enter_context(nc.psum_tensor("pp%d" % k, [128, 1024], F32)) for k in range(4)]
        PP = [t[:, :] for t in PPt]
        PB = [PP[k // 2][:, (k % 2) * 512:(k % 2 + 1) * 512] for k in range(8)]
        pk = ['p%d' % k for k in range(8)]

        def make_gather_chunk(j):
            def gather_chunk(nm, k, rkey):
                a = T[nm][j]
                b = T[nm[0] + "G"][j]
                S.cc(lambda e: e.collective_compute("AllGather", ALU.bypass, replica_groups=GROUPS,
                                                    ins=[a[k * 256:(k + 1) * 256, :].opt()], outs=[b[k].opt()]), [rkey], [(nm[0] + "G", k)])
            return gather_chunk

        def pick_all(j, names):
            for nm in names:
                b = T[nm + "G"][j]
                loc = T[nm + "L"]

                def pick(e, two, b=b, loc=loc):
                    mine = b.rearrange("(g two) (r xp) t -> g r two xp t", two=2, r=4)[bass.ds(get_rank(e), 1)].squeeze(0)
                    return e.dma_start(out=loc.rearrange("(r two xp) t -> r two xp t", r=4, two=2)[:, two], in_=mine[:, two])
                for two in range(2):
                    C.DMAF('pool', lambda e, two=two, pick=pick: pick(e, two), [(nm + "G", k) for k in range(8)], [nm + "L"])

        for i in range(4):
            src = T["xT"] if i == 0 else XS[(i - 1) % 2]
            dst = xo if i == 3 else XS[i % 2]
            j = i // 2
            if i % 2 == 0:
                sb.reset()
                gc = make_gather_chunk(j)
                emit_p1(C, sb, PB, pk, T, j, i, src, gc)
                S.barrier()
                pick_all(j, ["Q", "K", "V"])
                sb.reset()
                emit_p2(C, sb, PB, pk, T, j, gc, PP)
                S.barrier()
                pick_all(j, ["O"])
                sb.reset()
                emit_tok(C, sb, PB, pk, T, 'attn', i, src, dst, False)
                S.barrier()
            else:
                sb.reset()
                emit_tok(C, sb, PB, pk, T, 'sg', i, src, dst, i == 3)
                S.barrier()
        S.emit()
    return nc


def fm(g):
    return np.ascontiguousarray(g.reshape(16, 128).T.astype(np.float32))


def kernel(**inp):
    inp = {k: np.asarray(v) for k, v in inp.items()}
    x = np.ascontiguousarray(inp["x"], dtype=np.float32).reshape(16384, 2048)
    p = inp["p"].reshape(4, 16384, 256)
    k = np.arange(128)
    q = np.arange(512)
    shared = {
        "g_mix": np.stack([fm(inp["norm_mix"][i]) for i in range(4)]),
        "g_ffn": np.stack([fm(inp["norm_ffn"][i]) for i in range(4)]),
        "g_pin": np.stack([fm(inp["ple_norm_in"][i]) for i in range(4)]),
        "g_pout": np.stack([fm(inp["ple_norm_out"][i]) for i in range(4)]),
        "gq": np.ascontiguousarray(inp["sb_q_norm"].reshape(2, 128, 1)),
        "gk": np.ascontiguousarray(inp["sb_k_norm"].reshape(2, 128, 1)),
        "vg": np.stack([fm(inp["sg_v_norm"][j]) for j in range(2)]),
        "wsT": np.ascontiguousarray(np.transpose(inp["sg_w_s"], (0, 3, 1, 2))),
        "bs": np.ascontiguousarray(np.broadcast_to(inp["sg_b_s"][:, None], (2, 128, 16, 128))),
        "wr": np.ascontiguousarray(np.concatenate([inp["moe_w_group"], inp["moe_w_expert"]], axis=2)),
        "br": np.ascontiguousarray(np.broadcast_to(np.concatenate([inp["moe_b_group"], inp["moe_b_expert"]], axis=1)[:, None, :], (4, 128, 36))),
        "sb_w_in": inp["sb_w_in"], "sb_w_out": inp["sb_w_out"], "sg_w_in": inp["sg_w_in"], "sg_w_out": inp["sg_w_out"],
        "moe_w_gate": inp["moe_w_gate"], "moe_w_up": inp["moe_w_up"], "moe_w_down": inp["moe_w_down"],
        "ple_w_gate": inp["ple_w_gate"], "ple_w_proj": inp["ple_w_proj"],
        "ident": np.eye(128, dtype=np.float32), "ones": np.ones((128, 128), np.float32),
        "trimask": (k[:, None] <= k[None, :]).astype(np.float32),
        "ntri": -(k[:, None] >= k[None, :]).astype(np.float32),
        "masks": np.ascontiguousarray(np.stack([((k[:, None] + o * 128) < q[None, :]).astype(np.float32) for o in (3, 2, 1, 0)], axis=1)),
    }
    maps = []
    for c in range(N_CORES):
        m = dict(shared)
        m["xT"] = np.ascontiguousarray(x[c * NT:(c + 1) * NT].T)
        m["pT"] = np.ascontiguousarray(np.transpose(p[:, c * NT:(c + 1) * NT, :], (0, 2, 1)))
        maps.append(m)
    nc = build_fused()
    res = run_bass_kernel_spmd(nc, maps, core_ids=list(range(N_CORES)))
    out = np.concatenate([res.results[c]["xo"].T for c in range(N_CORES)], axis=0).reshape(2, 8192, 2048)
    return np.ascontiguousarray(out, dtype=np.float32)
```

```python
import numpy as np
import concourse.bass as bass
import concourse.mybir as mybir
from concourse.bass_utils import run_bass_kernel_spmd
from contextlib import ExitStack

F32 = mybir.dt.float32
BF16 = mybir.dt.bfloat16
AF = mybir.ActivationFunctionType
ALU = mybir.AluOpType
AX = mybir.AxisListType

ENGS = ['pe', 'act', 'dve', 'pool', 'sp']
DMAQ = ('sp', 'act', 'pool')


class Sched:
    def __init__(self, nc, es, R=4):
        self.nc = nc
        self.sem = {e: es.enter_context(nc.semaphore('s_' + e)) for e in ENGS}
        self.cnt = {e: 0 for e in ENGS}
        self.ops = {e: [] for e in ENGS}
        self.waited = {e: {} for e in ENGS}
        self.lastw = {}
        self.readers = {}
        self.R = R
        self.dsem = {q: [es.enter_context(nc.semaphore('d_%s%d' % (q, i))) for i in range(R)] for q in DMAQ}
        self.dcnt = {q: 0 for q in DMAQ}
        self.semobj = {}
        self.ccn = 0
        self.semobj[('cc',)] = es.enter_context(nc.semaphore('s_cc'))
        for e in ENGS:
            self.semobj[('c', e)] = self.sem[e]
        for q in DMAQ:
            for i in range(R):
                self.semobj[('d', q, i)] = self.dsem[q][i]

    def _deps(self, eng, reads, writes, skip_sem=None):
        deps = {}

        def add(tok):
            if tok is None:
                return
            s, v = tok
            if s == skip_sem:
                return
            if deps.get(s, 0) < v:
                deps[s] = v
        for k in reads:
            for s, v in self.lastw.get(k, {}).items():
                add((s, v))
        for k in writes:
            for s, v in self.lastw.get(k, {}).items():
                add((s, v))
            for s, v in self.readers.get(k, {}).items():
                add((s, v))
        out = []
        w = self.waited[eng]
        for s, v in deps.items():
            if w.get(s, 0) < v:
                w[s] = v
                out.append((s, v))
        return out

    def _commit(self, tok, reads, writes):
        s, v = tok
        for k in reads:
            r = self.readers.setdefault(k, {})
            if r.get(s, 0) < v:
                r[s] = v
        for k in writes:
            d = self.lastw.setdefault(k, {})
            if d.get(s, 0) < v:
                d[s] = v
            self.readers[k] = {}

    def op(self, eng, fn, reads=(), writes=()):
        skip = ('c', 'pe') if eng == 'pe' else None
        waits = self._deps(eng, reads, writes, skip)
        self.cnt[eng] += 1
        tok = (('c', eng), self.cnt[eng])
        self.ops[eng].append((fn, waits, (('c', eng), 1)))
        self._commit(tok, reads, writes)
        return tok

    def dma(self, q, fn, reads=(), writes=()):
        i = self.dcnt[q]
        self.dcnt[q] += 1
        slot = i % self.R
        semk = ('d', q, slot)
        waits = self._deps(q, reads, writes)
        prev = 16 * (i // self.R)
        if prev > 0 and self.waited[q].get(semk, 0) < prev:
            self.waited[q][semk] = prev
            waits.append((semk, prev))
        tok = (semk, prev + 16)
        self.ops[q].append((fn, waits, (semk, 16)))
        self._commit(tok, reads, writes)
        return tok


    def cc(self, fn, reads=(), writes=()):
        waits = self._deps('pool', reads, writes)
        self.ccn += 1
        tok = (('cc',), self.ccn)
        self.ops['pool'].append((fn, waits, (('cc',), 1)))
        self._commit(tok, reads, writes)
        return tok

    def barrier(self):
        latest = {}
        for e in ENGS:
            if self.cnt[e] > 0:
                latest[('c', e)] = self.cnt[e]
        for q in DMAQ:
            n = self.dcnt[q]
            for slot in range(self.R):
                k = (n - slot + self.R - 1) // self.R if n > slot else 0
                if k > 0:
                    latest[('d', q, slot)] = 16 * k
        if self.ccn > 0:
            latest[('cc',)] = self.ccn
        for e in ENGS:
            waits = []
            for s, v in latest.items():
                if self.waited[e].get(s, 0) < v:
                    self.waited[e][s] = v
                    waits.append((s, v))
            self.ops[e].append((None, waits, None))
        self.lastw.clear()
        self.readers.clear()

    def wait_all(self, eng, toks):
        waits = []
        for s, v in toks:
            if self.waited[eng].get(s, 0) < v:
                self.waited[eng][s] = v
                waits.append((s, v))
        self.ops[eng].append((None, waits, None))

    def emit(self):
        nc = self.nc
        S = self
        with nc.Block() as block:
            def run(e, name):
                for fn, waits, inc in S.ops[name]:
                    for s, v in waits:
                        e.wait_ge(S.semobj[s], v)
                    if fn is not None:
                        ins = fn(e)
                        if inc is not None:
                            ins.then_inc(S.semobj[inc[0]], inc[1])

            @block.sync
            def _(e):
                run(e, 'sp')

            @block.tensor
            def _(e):
                run(e, 'pe')

            @block.scalar
            def _(e):
                run(e, 'act')

            @block.vector
            def _(e):
                run(e, 'dve')

            @block.gpsimd
            def _(e):
                run(e, 'pool')


D = 2048
NT = 2048
EPS = 1e-6
MUL = ALU.mult
ADD = ALU.add
N_CORES = 8
GROUPS = [[0, 1, 2, 3], [4, 5, 6, 7]]


class Ctx:
    def __init__(self, nc, es):
        self.nc = nc
        self.es = es
        self.S = Sched(nc, es)

    def MM(self, out, lhsT, rhs, start, stop, r, w):
        self.S.op('pe', lambda e: e.matmul(out, lhsT, rhs, start=start, stop=stop), r, w)

    def ACT(self, out, in_, func, r, w, **kw):
        self.S.op('act', lambda e: e.activation(out=out, in_=in_, func=func, **kw), r, w)

    def TT(self, eng, out, a, b, op, r, w):
        self.S.op(eng, lambda e: e.tensor_tensor(out=out, in0=a, in1=b, op=op), r, w)

    def TS(self, eng, out, a, s1, s2, op0, op1, r, w):
        if s2 is None:
            self.S.op(eng, lambda e: e.tensor_scalar(out=out, in0=a, scalar1=s1, scalar2=None, op0=op0), r, w)
        else:
            self.S.op(eng, lambda e: e.tensor_scalar(out=out, in0=a, scalar1=s1, scalar2=s2, op0=op0, op1=op1), r, w)

    def STT(self, out, a, s, b, op0, op1, r, w):
        self.S.op('dve', lambda e: e.scalar_tensor_tensor(out=out, in0=a, scalar=s, in1=b, op0=op0, op1=op1), r, w)

    def DMA(self, q, out, in_, r, w):
        return self.S.dma(q, lambda e: e.dma_start(out=out, in_=in_), r, w)

    def DMAF(self, q, fn, r, w):
        return self.S.dma(q, fn, r, w)

    def OP(self, eng, fn, r, w):
        self.S.op(eng, fn, r, w)


class Arena:
    def __init__(self, big, words):
        self.big = big
        self.words = words
        self.off = 0

    def reset(self):
        self.off = 0

    def __call__(self, name, shape, dt):
        n = 1
        for s in shape[1:]:
            n *= s
        if dt == BF16:
            w = (n + 1) // 2
            ap = self.big[:, self.off:self.off + w].bitcast(BF16)
        else:
            w = n
            ap = self.big[:, self.off:self.off + w]
        self.off += (w + 15) // 16 * 16
        assert self.off <= self.words, (name, self.off, self.words)
        if len(shape) == 3:
            ap = ap.rearrange("p (a b) -> p a b", a=shape[1])
        elif len(shape) == 4:
            ap = ap.rearrange("p (a b c) -> p a b c", a=shape[1], b=shape[2])
        return ap


_RANK = {}


def get_rank(e):
    if 'r' not in _RANK:
        _RANK['r'] = e.snap(e.partition_id() % 4, min_val=0, max_val=3)
    return _RANK['r']


_VIEWS = {}


def dyn_view(e, key, mk):
    if key not in _VIEWS:
        _VIEWS[key] = mk(get_rank(e))
    return _VIEWS[key]


def fm_view(ap2d):
    return ap2d.rearrange("(c q) t -> q c t", q=128)


def emit_p1(C, sb, PB, pk, T, j, i, src, gather_chunk):
    S = C.S
    xT_v = fm_view(src)
    win_v = T["sb_w_in"][j].rearrange("(c q) n -> q c n", q=128)
    QS, KS, VS = T["QS"][j], T["KS"][j], T["VS"][j]
    VS3 = VS.rearrange("(h a) (b e) -> h (a b) e", h=16, b=16, e=128)
    XB = [sb("XB%d" % k, [128, 16, 512], F32) for k in range(2)]
    HB = sb("HB", [128, 16, 2048], BF16)
    WS = [sb("WS%d" % k, [128, 16, 512], BF16) for k in range(2)]
    SQ = sb("SQ", [128, 2, 512], BF16)
    RS = sb("RS", [128, 3, 512], F32)
    OB = sb("OB", [128, 4, 512], BF16)
    ONESB = sb("ONESB", [128, 128], BF16)
    G_MIX = sb("G_MIX", [128, 16], F32)
    GQK = sb("GQK", [128, 2], F32)
    C.DMA('pool', ONESB, T["ones"], [], ['ONESB'])
    C.DMA('sp', G_MIX, T["g_mix"][i], [], ['G_MIX'])
    C.DMA('sp', GQK[:, 0:1], T["gq"][j], [], ['GQK'])
    C.DMA('sp', GQK[:, 1:2], T["gk"][j], [], ['GQK'])
    groups = [(w, g) for w in range(3) for g in range(4)]

    def wload(n):
        w, g = groups[n]
        C.DMA('pool', WS[n % 2], win_v[:, :, w * 2048 + g * 512:w * 2048 + (g + 1) * 512], [], ['WS%d' % (n % 2)])
    wload(0)
    for blk in range(4):
        g0 = blk * 512
        xb = XB[blk % 2]
        C.DMA('sp', xb, xT_v[:, :, g0:g0 + 512], [], [('XB', blk % 2, c) for c in range(16)])
        for c in range(16):
            sqb = SQ[:, c % 2, :]
            C.ACT(sqb, xb[:, c, :], AF.Square, [('XB', blk % 2, c)], [('SQ', c % 2)])
            C.MM(PB[7][:], ONESB, sqb, c == 0, c == 15, [('SQ', c % 2), 'ONESB'], [pk[7]])
        C.ACT(RS[:, 0, :], PB[7][:], AF.Sqrt, [pk[7]], [('RS', 0)], scale=1.0 / D, bias=EPS)
        C.OP('dve', lambda e: e.reciprocal(RS[:, 0, :], RS[:, 0, :]), [('RS', 0)], [('RS', 0)])
        for c in range(16):
            C.STT(HB[:, c, g0:g0 + 512], xb[:, c, :], G_MIX[:, c:c + 1], RS[:, 0, :], MUL, MUL,
                  [('XB', blk % 2, c), ('RS', 0), 'G_MIX'], [('HB', blk, c)])
    oi = 0
    for n, (w, g) in enumerate(groups):
        ws = WS[n % 2]
        wk = 'WS%d' % (n % 2)
        if n + 1 < len(groups):
            wload(n + 1)
        for blk in range(4):
            g0 = blk * 512
            if w < 2:
                dst = QS if w == 0 else KS
                dk = ('QS', g) if w == 0 else ('KS', g)
                for mi in range(4):
                    hd = g * 4 + mi
                    bi = hd % 2
                    for c in range(16):
                        C.MM(PB[bi][:], ws[:, c, mi * 128:(mi + 1) * 128], HB[:, c, g0:g0 + 512], c == 0, c == 15, [wk, ('HB', blk, c)], [pk[bi]])
                    sqb = SQ[:, bi, :]
                    C.ACT(sqb, PB[bi][:], AF.Square, [pk[bi]], [('SQ', bi)])
                    C.MM(PB[2 + bi][:], ONESB, sqb, True, True, [('SQ', bi), 'ONESB'], [pk[2 + bi]])
                    rk = ('RS', 1 + bi)
                    rs = RS[:, 1 + bi, :]
                    if w == 0:
                        C.ACT(rs, PB[2 + bi][:], AF.Sqrt, [pk[2 + bi]], [rk], scale=1.0, bias=128.0 * EPS)
                    else:
                        C.ACT(rs, PB[2 + bi][:], AF.Sqrt, [pk[2 + bi]], [rk], scale=1.0 / 128, bias=EPS)
                    C.OP('dve', lambda e, rs=rs: e.reciprocal(rs, rs), [rk], [rk])
                    ok = ('OB', oi % 4)
                    ob = OB[:, oi % 4, :]
                    oi += 1
                    C.STT(ob, PB[bi][:], GQK[:, w:w + 1], rs, MUL, MUL, [pk[bi], rk, 'GQK'], [ok])
                    C.DMA('sp', dst[hd * 128:(hd + 1) * 128, g0:g0 + 512], ob, [ok], [dk])
            else:
                for sx in range(4):
                    bi = 4 + (sx % 2)
                    for c in range(16):
                        C.MM(PB[bi][:], HB[:, c, g0 + sx * 128:g0 + (sx + 1) * 128], ws[:, c, :], c == 0, c == 15, [wk, ('HB', blk, c)], [pk[bi]])
                    ok = ('OB', oi % 4)
                    ob = OB[:, oi % 4, :]
                    oi += 1
                    C.ACT(ob, PB[bi][:], AF.Copy, [pk[bi]], [ok])
                    t0 = g0 + sx * 128
                    C.DMA('sp', VS3[g * 4:(g + 1) * 4, t0:t0 + 128, :].rearrange("h t e -> t h e"),
                          ob.rearrange("p (h e) -> p h e", h=4), [ok], [('VS', g)])
        nm = ("QS", "KS", "VS")[w]
        gather_chunk(nm, 2 * g, (nm, g))
        gather_chunk(nm, 2 * g + 1, (nm, g))


def emit_p2(C, sb, PB, pk, T, j, gather_chunk, PP):
    S = C.S
    SEQ = 8192
    NKB = SEQ // 128
    NQB = SEQ // 512
    OS = T["OS"][j]
    QL, KL, VL = T["QL"], T["KL"], T["VL"]
    VL4 = VL.rearrange("(r pr a) (b e) -> r pr (a b) e", r=4, pr=4, b=16, e=128)
    QT = [sb("QT%d" % k, [128, SEQ], BF16) for k in range(2)]
    KT = [sb("KT%d" % k, [128, SEQ], BF16) for k in range(2)]
    V = [sb("V%d" % k, [128, NKB, 128], BF16) for k in range(2)]
    NTRI = sb("NTRI", [128, 128], BF16)
    ONESB = sb("ONESB", [128, 128], BF16)
    MASK = sb("MASK", [128, 4, 512], BF16)
    EB = sb("EB", [128, 2, 1024], F32)
    SPB = sb("SPB", [128, 2, 1024], BF16)
    ARG = sb("ARG", [128, 2, 1024], F32)
    WB = sb("WB", [128, 2, 1024], BF16)
    R = sb("R", [128, 512], F32)
    OB = sb("OB", [128, 2, 512], BF16)
    C.DMA('pool', NTRI, T["ntri"], [], ['NTRI'])
    C.DMA('pool', ONESB, T["ones"], [], ['ONESB'])
    C.DMA('pool', MASK, T["masks"], [], ['MASK'])
    def loads(pr):
        d = pr % 2
        for r in range(4):
            C.DMA('pool', QT[d][:, r * 2048:(r + 1) * 2048], QL[r * 512 + pr * 128:r * 512 + (pr + 1) * 128, :], ['QL'], [('QT', d)])
            C.DMA('pool', KT[d][:, r * 2048:(r + 1) * 2048], KL[r * 512 + pr * 128:r * 512 + (pr + 1) * 128, :], ['KL'], [('KT', d)])
            C.DMA('pool', V[d][:, r * 16:(r + 1) * 16, :], VL4[r, pr].rearrange("(kb q) e -> q kb e", q=128), ['VL'], [('V', d)])

    PZ, PA = PP[0], PP[1]
    NEGONES = sb("NEGONES", [128, 128], BF16)
    C.TS('pool', NEGONES, ONESB, -1.0, None, MUL, None, ['ONESB'], ['NEGONES'])
    its = []
    grp = 0
    for pr in range(4):
        for qb in range(NQB):
            kbs = list(range(4 * qb + 3, -1, -1))
            npair = len(kbs) // 2
            for n in range(npair):
                its.append(dict(pr=pr, d=pr % 2, qb=qb, q0=qb * 512, hi=kbs[2 * n], lo=kbs[2 * n + 1], n=n, last=(n == npair - 1),
                                diag=(n < 2), g=grp, first_of_pair=(qb == 0 and n == 0)))
            grp += 1
    N = len(its)

    def stageA(k):
        t = its[k]
        i2 = k % 2
        d = t['d']
        qslice = QT[d][:, t['q0']:t['q0'] + 512]
        for h, kb in enumerate((t['hi'], t['lo'])):
            C.MM(PZ[:, h * 512:(h + 1) * 512], KT[d][:, kb * 128:(kb + 1) * 128], qslice, True, True, [('KT', d), ('QT', d)], ['pz'])
        C.ACT(EB[:, i2, :], PZ, AF.Exp, ['pz'], [('EB', i2)])
        C.ACT(SPB[:, i2, :], EB[:, i2, :], AF.Ln, [('EB', i2)], [('SPB', i2)], bias=1.0)
        if t['diag']:
            mk = MASK[:, 2 * t['n']:2 * t['n'] + 2, :].rearrange("p a b -> p (a b)")
            C.TT('pool', SPB[:, i2, :], SPB[:, i2, :], mk, MUL, [('SPB', i2), 'MASK'], [('SPB', i2)])

    def stageB1(k):
        t = its[k]
        i2 = k % 2
        d = t['d']
        tb = 4 + i2
        qslice = QT[d][:, t['q0']:t['q0'] + 512]
        sp_hi = SPB[:, i2, 0:512]
        sp_lo = SPB[:, i2, 512:1024]
        if t['n'] == 0:
            C.OP('dve', lambda e: e.memset(R, 0.0), [], ['R'])
        C.MM(PA[:, 0:512], KT[d][:, t['hi'] * 128:(t['hi'] + 1) * 128], qslice, True, False, [('KT', d), ('QT', d)], ['pa'])
        C.MM(PA[:, 0:512], NTRI, sp_hi, False, True, ['NTRI', ('SPB', i2)], ['pa'])
        C.MM(PA[:, 512:1024], KT[d][:, t['lo'] * 128:(t['lo'] + 1) * 128], qslice, True, False, [('KT', d), ('QT', d)], ['pa'])
        C.MM(PA[:, 512:1024], NTRI, sp_lo, False, False, ['NTRI', ('SPB', i2)], ['pa'])
        C.MM(PA[:, 512:1024], NEGONES, sp_hi, False, True, ['NEGONES', ('SPB', i2)], ['pa'])
        if not t['last']:
            C.MM(PB[tb][:], ONESB, sp_hi, True, False, ['ONESB', ('SPB', i2)], [pk[tb]])
            C.MM(PB[tb][:], ONESB, sp_lo, False, True, ['ONESB', ('SPB', i2)], [pk[tb]])
        for h in range(2):
            C.TT('dve', ARG[:, i2, h * 512:(h + 1) * 512], PA[:, h * 512:(h + 1) * 512], R, ALU.subtract, ['pa', 'R'], [('ARG', i2, h)])
        C.ACT(WB[:, i2, :], ARG[:, i2, :], AF.Exp, [('ARG', i2, 0), ('ARG', i2, 1)], [('WB', i2)])
        if t['diag']:
            mk = MASK[:, 2 * t['n']:2 * t['n'] + 2, :].rearrange("p a b -> p (a b)")
            C.TT('pool', WB[:, i2, :], WB[:, i2, :], mk, MUL, [('WB', i2), 'MASK'], [('WB', i2)])
        if not t['last']:
            C.TT('dve', R, R, PB[tb][:], ADD, [pk[tb], 'R'], ['R'])

    def stageB2(k):
        t = its[k]
        i2 = k % 2
        d = t['d']
        ob_i = t['g'] % 2
        obank = 6 + ob_i
        C.MM(PB[obank][:], V[d][:, t['hi'], :], WB[:, i2, 0:512], t['n'] == 0, False, [('V', d), ('WB', i2)], [pk[obank]])
        C.MM(PB[obank][:], V[d][:, t['lo'], :], WB[:, i2, 512:1024], False, t['last'], [('V', d), ('WB', i2)], [pk[obank]])
        if t['last']:
            C.ACT(OB[:, ob_i, :], PB[obank][:], AF.Copy, [pk[obank]], [('OB', ob_i)])
            tq = t['q0'] // 2048
            row = tq * 512 + t['pr'] * 128
            C.DMA('sp', OS[row:row + 128, (t['q0'] % 2048):(t['q0'] % 2048) + 512], OB[:, ob_i, :], [('OB', ob_i)], [('OS', t['pr'] // 2)])
            if t['qb'] == NQB - 1 and t['pr'] % 2 == 1:
                for tq2 in range(4):
                    gather_chunk("OS", 2 * tq2 + t['pr'] // 2, ('OS', t['pr'] // 2))

    loads(0)
    for sidx in range(N + 2):
        if sidx < N:
            stageA(sidx)
        if 0 <= sidx - 1 < N:
            stageB1(sidx - 1)
        if 0 <= sidx - 2 < N:
            stageB2(sidx - 2)
            if its[sidx - 2]['first_of_pair'] and its[sidx - 2]['pr'] + 1 < 4:
                loads(its[sidx - 2]['pr'] + 1)


def emit_tok(C, sb, PB, pk, T, mode, i, src, dst, final):
    S = C.S
    j = i // 2
    xT_v = fm_view(src)
    xo_v = fm_view(dst)
    w_mo = T["sb_w_out"][j] if mode == 'attn' else T["sg_w_out"][j]
    wmo_v = w_mo.rearrange("(c q) n -> q c n", q=128)
    wpg_v = T["ple_w_gate"][i].rearrange("(c q) n -> q c n", q=128)
    wpp_v = T["ple_w_proj"][i].rearrange("(c q) n -> q c n", q=128)
    pT_v = T["pT"][i].rearrange("(c q) t -> q c t", q=128)
    wr_v = T["wr"][i].rearrange("(c q) n -> q c n", q=128)
    wg_d, wu_d, wd_d = T["moe_w_gate"][i], T["moe_w_up"][i], T["moe_w_down"][i]
    if mode == 'sg':
        w_in_v = T["sg_w_in"][j].rearrange("(c q) n -> q c n", q=128)

    X1 = sb("X1", [128, 16, 1024], F32)
    H = sb("H", [128, 16, 1024], BF16)
    WA = sb("WA", [128, 12288], BF16)
    WB_ = sb("WB", [128, 12288], BF16)
    AR = [WA, WB_]
    HID = sb("HID", [128, 2, 2, 1024], BF16)
    ETF = sb("ETF", [128, 4096], F32)
    ET = ETF.rearrange("p (m t) -> p m t", m=16)
    SQ = sb("SQ", [128, 2, 512], BF16)
    ONESB = sb("ONESB", [128, 128], BF16)
    RS = sb("RS", [128, 512], F32)
    T1 = sb("T1", [128, 2, 512], F32)
    ONESF = sb("ONESF", [128, 128], F32)
    IDF = sb("IDF", [128, 128], F32)
    SL = T1
    GEXP = sb("GEXP", [128, 8, 128], F32)
    G_FFN = sb("G_FFN", [128, 16], F32)
    G_PIN = sb("G_PIN", [128, 16], F32)
    G_POUT = sb("G_POUT", [128, 16], F32)
    WR = sb("WR", [128, 16, 36], BF16)
    BR = sb("BR", [128, 36], F32)
    GATES = sb("GATES", [128, 8, 32], F32)
    LG = sb("LG", [128, 36], F32)
    ME = sb("ME", [128, 32], F32)
    EX = sb("EX", [128, 32], F32)
    SEL = sb("SEL", [128, 32], F32)
    SM = sb("SM", [128, 16], F32)
    M8 = sb("M8", [128, 8], F32)
    PT = sb("PT", [128, 2, 256], BF16)
    if mode == 'sg':
        G_MIX = sb("G_MIX", [128, 16], F32)
        VG = sb("VG", [128, 16], F32)
        WCT = sb("WCT", [128, 16, 128], BF16)
        TRIB = sb("TRIB", [128, 128], BF16)
        BS = sb("BS", [128, 16, 128], F32)
        UT = sb("UT", [128, 512], F32)
        MX = sb("MX", [128, 512], F32)
        SS = sb("SS", [128, 16], F32)
        RV = sb("RV", [128, 4], F32)
        GTD = ETF.bitcast(BF16).rearrange("p (m t) -> p m t", m=16)

    C.DMA('sp', ONESF, T["ones"], [], ['ONESF'])
    C.DMA('pool', ONESB, T["ones"], [], ['ONESB'])
    C.DMA('sp', IDF, T["ident"], [], ['IDF'])
    C.DMA('sp', G_FFN, T["g_ffn"][i], [], ['G_FFN'])
    C.DMA('sp', G_PIN, T["g_pin"][i], [], ['G_PIN'])
    C.DMA('sp', G_POUT, T["g_pout"][i], [], ['G_POUT'])
    C.DMA('sp', BR, T["br"][i], [], ['BR'])
    C.DMA('pool', WR, wr_v, [], ['WR'])
    if mode == 'sg':
        C.DMA('sp', G_MIX, T["g_mix"][i], [], ['G_MIX'])
        C.DMA('sp', VG, T["vg"][j], [], ['VG'])
        C.DMA('sp', BS, T["bs"][j], [], ['BS'])
        C.DMA('pool', WCT, T["wsT"][j], [], ['WCT'])
        C.DMA('pool', TRIB, T["trimask"], [], ['TRIB'])
        for g in range(16):
            C.TT('pool', WCT[:, g, :], WCT[:, g, :], TRIB, MUL, ['WCT', 'TRIB'], ['WCT'])
        C.OP('dve', lambda e: e.memset(SS, 0.0), [], ['SS'])
    C.OP('dve', lambda e: e.memset(SM, 0.0), [], ['SM'])

    arena_i = [0]

    def next_arena():
        a = arena_i[0] % 2
        arena_i[0] += 1
        return AR[a], 'AR%d' % a

    def rms_fm(src_, skey, g_tile, gkey, dst_, dkey, W, bank, bkey):
        for c in range(16):
            sqb = SQ[:, c % 2, 0:W]
            C.ACT(sqb, src_(c), AF.Square, [skey(c)], [('SQ', c % 2)])
            C.MM(bank[:, 0:W], ONESB, sqb, c == 0, c == 15, [('SQ', c % 2), 'ONESB'], [bkey])
        C.ACT(RS[:, 0:W], bank[:, 0:W], AF.Sqrt, [bkey], ['RS'], scale=1.0 / D, bias=EPS)
        C.OP('dve', lambda e: e.reciprocal(RS[:, 0:W], RS[:, 0:W]), ['RS'], ['RS'])
        for c in range(16):
            C.STT(dst_(c), src_(c), g_tile[:, c:c + 1], RS[:, 0:W], MUL, MUL, [skey(c), 'RS', gkey], [dkey(c)])

    gelu_i = [0]

    def gelu(bank, bkey, W, out, okeys, sscol=None):
        k = gelu_i[0] % 2
        gelu_i[0] += 1
        t = T1[:, k, 0:W]
        tk = ('T1', k)
        C.ACT(t, bank, AF.Square, [bkey], [tk])
        C.TS('dve', t, t, 0.044715, 1.0, MUL, ADD, [tk], [tk])
        C.TT('dve', t, t, bank, MUL, [tk, bkey], [tk])
        C.ACT(t, t, AF.Sigmoid, [tk], [tk], scale=1.5957691216057308)
        C.TT('dve', out, t, bank, MUL, [tk, bkey], okeys)
        if sscol is not None:
            C.ACT(t, out, AF.Square, okeys, [tk, 'SS'], accum_out=sscol)

    def wst_view(ar, n):
        return ar[:, 0:16 * n].rearrange("p (c f) -> p c f", c=16)

    out_toks = []
    for p in range(2):
        for b in range(2):
            g0 = p * 1024 + b * 512
            bl = slice(b * 512, (b + 1) * 512)
            ol = slice((1 - b) * 512, (2 - b) * 512)
            xk = [('X1', b, c) for c in range(16)]
            ok = [('H', 1 - b, c) for c in range(16)]
            C.DMA('sp', X1[:, :, bl], xT_v[:, :, g0:g0 + 512], [], xk)
            if mode == 'attn':
                C.DMA('pool', H[:, :, ol], fm_view(T["OL"])[:, :, g0:g0 + 512], ['OL'], ok)
                src_keys = ok
                srcf = lambda c: H[:, c, ol]
            else:
                rms_fm(lambda c: X1[:, c, bl], lambda c: ('X1', b, c), G_MIX, 'G_MIX',
                       lambda c: H[:, c, bl], lambda c: ('H', b, c), 512, PB[7], pk[7])
                C.OP('dve', lambda e: e.memset(SS, 0.0), [], ['SS'])
                for jv in range(4):
                    ar, ak = next_arena()
                    wst = wst_view(ar, 512)
                    C.DMA('pool', wst, w_in_v[:, :, 2048 + jv * 512:2048 + (jv + 1) * 512], [], [ak, ak + 'u'])
                    for s in range(4):
                        bi = (jv * 4 + s) % 2
                        for c in range(16):
                            C.MM(PB[bi][:], H[:, c, b * 512 + s * 128:b * 512 + (s + 1) * 128], wst[:, c, :],
                                 c == 0, c == 15, [('H', b, c), ak, ak + 'u'], [pk[bi]])
                        gelu(PB[bi][:], pk[bi], 512, H[:, s * 4 + jv, ol], [('H', 1 - b, s * 4 + jv)], sscol=SS[:, s * 4 + jv:s * 4 + jv + 1])
                C.OP('dve', lambda e: e.tensor_reduce(out=RV, in_=SS.rearrange("p (s j) -> p s j", j=4), axis=AX.X, op=ADD), ['SS'], ['RV'])
                C.ACT(RV, RV, AF.Sqrt, ['RV'], ['RV'], scale=1.0 / 2048, bias=EPS)
                C.OP('dve', lambda e: e.reciprocal(RV, RV), ['RV'], ['RV'])
                for s in range(4):
                    for jv in range(4):
                        vk = ('H', 1 - b, s * 4 + jv)
                        C.TS('dve', H[:, s * 4 + jv, ol], H[:, s * 4 + jv, ol], RV[:, s:s + 1], None, MUL, None, [vk, 'RV'], [vk])
                for mg in range(4):
                    ar, ak = next_arena()
                    wst = wst_view(ar, 512)
                    C.DMA('pool', wst, w_in_v[:, :, mg * 512:(mg + 1) * 512], [], [ak, ak + 'u'])
                    for mi in range(4):
                        m = mg * 4 + mi
                        bu = 2 + (m % 2) * 2
                        bm = 3 + (m % 2) * 2
                        for c in range(16):
                            C.MM(PB[bu][:], wst[:, c, mi * 128:(mi + 1) * 128], H[:, c, bl], c == 0, c == 15,
                                 [ak, ak + 'u', ('H', b, c)], [pk[bu]])
                        for s in range(4):
                            vk = ('H', 1 - b, s * 4 + m // 4)
                            o0 = (1 - b) * 512 + (m % 4) * 128
                            C.MM(PB[bm][:, s * 128:(s + 1) * 128], H[:, s * 4 + m // 4, o0:o0 + 128], WCT[:, m, :], True, True,
                                 [vk, 'WCT'], [pk[bm]])
                        gelu(PB[bu][:], pk[bu], 512, UT, ['UT'])
                        for s in range(4):
                            C.STT(MX[:, s * 128:(s + 1) * 128], PB[bm][:, s * 128:(s + 1) * 128], VG[:, m:m + 1], BS[:, m, :], MUL, ADD,
                                  [pk[bm], 'VG', 'BS'], ['MX'])
                        C.TT('dve', GTD[:, m, :], UT, MX, MUL, ['UT', 'MX'], [('GTD', m)])
                src_keys = [('GTD', c) for c in range(16)]
                srcf = lambda c: GTD[:, c, :]
            for mg in range(4):
                ar, ak = next_arena()
                wst = wst_view(ar, 512)
                C.DMA('pool', wst, wmo_v[:, :, mg * 512:(mg + 1) * 512], [], [ak, ak + 'u'])
                for mi in range(4):
                    m = mg * 4 + mi
                    bi = m % 2
                    for c in range(16):
                        C.MM(PB[bi][:], wst[:, c, mi * 128:(mi + 1) * 128], srcf(c), c == 0, c == 15, [ak, ak + 'u', src_keys[c]], [pk[bi]])
                    C.TT('dve', X1[:, m, bl], X1[:, m, bl], PB[bi][:], ADD, [pk[bi], ('X1', b, m)], [('X1', b, m)])
        for b in range(2):
            bl = slice(b * 512, (b + 1) * 512)
            rms_fm(lambda c: X1[:, c, bl], lambda c: ('X1', b, c), G_FFN, 'G_FFN',
                   lambda c: H[:, c, bl], lambda c: ('H', b, c), 512, PB[7], pk[7])
        for sub in range(8):
            b = sub // 4
            for c in range(16):
                C.MM(PB[6][:, 0:36], H[:, c, sub * 128:(sub + 1) * 128], WR[:, c, :], c == 0, c == 15, [('H', b, c), 'WR'], [pk[6]])
            C.TT('dve', LG, PB[6][:, 0:36], BR, ADD, [pk[6], 'BR'], ['LG'])
            C.OP('dve', lambda e: e.tensor_reduce(out=SM[:, 0:1], in_=LG[:, 0:4], axis=AX.X, op=ALU.max), ['LG'], ['SM'])
            C.TS('dve', SM[:, 1:2], SM[:, 0:1], -1.0, None, MUL, None, ['SM'], ['SM'])
            C.OP('dve', lambda e: e.memset(SM[:, 2:3], 0.0), [], ['SM'])
            C.ACT(EX[:, 0:4], LG[:, 0:4], AF.Exp, ['LG', 'SM'], ['EX', 'SM'], bias=SM[:, 1:2], accum_out=SM[:, 2:3])
            C.TS('dve', SEL[:, 0:4], LG[:, 0:4], SM[:, 0:1], None, ALU.is_equal, None, ['LG', 'SM'], ['SEL'])
            C.TS('dve', SEL[:, 0:4], SEL[:, 0:4], -1.0, 1.0e4, ADD, MUL, ['SEL'], ['SEL'])
            for g in range(4):
                C.TS('dve', ME[:, g * 8:(g + 1) * 8], LG[:, 4 + g * 8:4 + (g + 1) * 8], SEL[:, g:g + 1], None, ADD, None, ['LG', 'SEL'], ['ME'])
            C.OP('dve', lambda e: e.max(out=M8, in_=ME), ['ME'], ['M8'])
            C.TS('dve', SM[:, 3:4], M8[:, 0:1], -1.0, None, MUL, None, ['M8'], ['SM'])
            C.ACT(EX, ME, AF.Exp, ['ME', 'SM'], ['EX'], bias=SM[:, 3:4])
            C.TS('dve', SEL, ME, M8[:, 1:2], None, ALU.is_ge, None, ['ME', 'M8'], ['SEL'])
            C.TT('dve', EX, EX, SEL, MUL, ['EX', 'SEL'], ['EX'])
            C.OP('dve', lambda e: e.tensor_reduce(out=SM[:, 4:5], in_=EX, axis=AX.X, op=ADD), ['EX'], ['SM'])
            C.TT('dve', SM[:, 5:6], SM[:, 4:5], SM[:, 2:3], MUL, ['SM'], ['SM'])
            C.OP('dve', lambda e: e.reciprocal(SM[:, 6:7], SM[:, 5:6]), ['SM'], ['SM'])
            C.TS('dve', GATES[:, sub, :], EX, SM[:, 6:7], None, MUL, None, ['EX', 'SM'], [('GATES', sub)])
        def gexp(ex_):
            for sub in range(8):
                C.TS('dve', GEXP[:, sub, :], ONESF, GATES[:, sub, ex_:ex_ + 1], None, MUL, None, ['ONESF', ('GATES', sub)], [('GEXP', sub)])

        for ex in range(32):
            ar, ak = next_arena()
            Wg = ar[:, 0:4096].rearrange("p (c f) -> p c f", c=16)
            Wu = ar[:, 4096:8192].rearrange("p (c f) -> p c f", c=16)
            Wd = ar[:, 8192:12288].rearrange("p (c f) -> p c f", c=2)
            C.DMA('pool', Wg, wg_d[ex].rearrange("(c q) f -> q c f", q=128), [], [ak])
            C.DMA('pool', Wu, wu_d[ex].rearrange("(c q) f -> q c f", q=128), [], [ak + 'u'])
            C.DMA('pool', Wd, wd_d[ex].rearrange("(c q) f -> q c f", q=128), [], [ak + 'd'])
            hp = ex % 2
            if ex == 0:
                gexp(0)
            for sub in range(8):
                C.MM(PB[6 + sub // 4][:, (sub % 4) * 128:(sub % 4 + 1) * 128], GEXP[:, sub, :], IDF, True, True,
                     [('GEXP', sub), 'IDF'], [pk[6 + sub // 4]])
            for b in range(2):
                bl = slice(b * 512, (b + 1) * 512)
                for jf in range(2):
                    k = (b * 2 + jf) % 2
                    for c in range(16):
                        C.MM(PB[2 * k][:], Wg[:, c, jf * 128:(jf + 1) * 128], H[:, c, bl], c == 0, c == 15, [ak, ('H', b, c)], [pk[2 * k]])
                    for c in range(16):
                        C.MM(PB[2 * k + 1][:], Wu[:, c, jf * 128:(jf + 1) * 128], H[:, c, bl], c == 0, c == 15, [ak + 'u', ('H', b, c)], [pk[2 * k + 1]])
                    slk = ('T1', k)
                    C.ACT(SL[:, k, :], PB[2 * k][:], AF.Silu, [pk[2 * k]], [slk])
                    C.TT('dve', SL[:, k, :], SL[:, k, :], PB[2 * k + 1][:], MUL, [slk, pk[2 * k + 1]], [slk])
                    C.TT('dve', HID[:, hp, jf, bl], SL[:, k, :], PB[6 + b][:], MUL, [slk, pk[6 + b]], [('HID', hp, jf, b)])
            if ex + 1 < 32:
                gexp(ex + 1)
            for b in range(2):
                bl = slice(b * 512, (b + 1) * 512)
                for m in range(16):
                    bi = 4 + (m % 2)
                    for jf in range(2):
                        C.MM(PB[bi][:], Wd[:, jf, m * 128:(m + 1) * 128], HID[:, hp, jf, bl], jf == 0, jf == 1,
                             [ak + 'd', ('HID', hp, jf, b)], [pk[bi]])
                    C.TT('dve', X1[:, m, bl], X1[:, m, bl], PB[bi][:], ADD, [pk[bi], ('X1', b, m)], [('X1', b, m)])
        WPP = WB_[:, 8192:12288].rearrange("p (c f) -> p c f", c=2)
        C.DMA('pool', WPP, wpp_v, [], ['AR1d'])
        for sbk in range(4):
            b = sbk // 2
            g0 = p * 1024 + sbk * 256
            cl = slice(sbk * 256, (sbk + 1) * 256)
            C.DMA('pool', PT, pT_v[:, :, g0:g0 + 256], [], ['PT'])
            hkey = lambda c: ('H', b, c)
            rms_fm(lambda c: X1[:, c, cl], lambda c: ('X1', b, c), G_PIN, 'G_PIN',
                   lambda c: H[:, c, cl], hkey, 256, PB[7], pk[7])
            for mg in range(8):
                wk = 'AR0' if mg % 2 == 0 else 'AR0u'
                wst = WA[:, (mg % 2) * 4096:(mg % 2 + 1) * 4096].rearrange("p (c f) -> p c f", c=16)
                C.DMA('pool', wst, wpg_v[:, :, mg * 256:(mg + 1) * 256], [], [wk])
                for mi in range(2):
                    m = mg * 2 + mi
                    bg = (m % 2) * 2
                    bp = bg + 1
                    for c in range(16):
                        C.MM(PB[bg][:, 0:256], wst[:, c, mi * 128:(mi + 1) * 128], H[:, c, cl], c == 0, c == 15, [wk, hkey(c)], [pk[bg]])
                    for c in range(2):
                        C.MM(PB[bp][:, 0:256], WPP[:, c, m * 128:(m + 1) * 128], PT[:, c, :], c == 0, c == 1, ['AR1d', 'PT'], [pk[bp]])
                    tk = ('T1', m % 2)
                    C.ACT(T1[:, m % 2, 0:256], PB[bg][:, 0:256], AF.Sigmoid, [pk[bg]], [tk])
                    C.TT('dve', ET[:, m, :], T1[:, m % 2, 0:256], PB[bp][:, 0:256], MUL, [tk, pk[bp]], [('ET', m)])
            for m in range(16):
                sqb = SQ[:, m % 2, 0:256]
                C.ACT(sqb, ET[:, m, :], AF.Square, [('ET', m)], [('SQ', m % 2)])
                C.MM(PB[4][:, 0:256], ONESB, sqb, m == 0, m == 15, [('SQ', m % 2), 'ONESB'], [pk[4]])
            C.ACT(RS[:, 0:256], PB[4][:, 0:256], AF.Sqrt, [pk[4]], ['RS'], scale=1.0 / D, bias=EPS)
            C.OP('dve', lambda e: e.reciprocal(RS[:, 0:256], RS[:, 0:256]), ['RS'], ['RS'])
            for m in range(16):
                C.STT(ET[:, m, :], ET[:, m, :], G_POUT[:, m:m + 1], RS[:, 0:256], MUL, MUL, [('ET', m), 'RS', 'G_POUT'], [('ET', m)])
                C.TT('dve', X1[:, m, cl], X1[:, m, cl], ET[:, m, :], ADD, [('ET', m), ('X1', b, m)], [('X1', b, m)])
            out_toks.append(C.DMA('sp', xo_v[:, :, g0:g0 + 256], X1[:, :, cl], [('X1', b, m) for m in range(16)], ['XDST']))
    if final:
        S.wait_all('sp', out_toks)


def build_fused():
    nc = bass.Bass("TRN2", target_bir_lowering=False)

    def din(n, s):
        return nc.dram_tensor(n, s, F32, kind="ExternalInput").ap()
    T = {}
    T["xT"] = din("xT", [D, NT])
    T["pT"] = din("pT", [4, 256, NT])
    T["g_mix"] = din("g_mix", [4, 128, 16])
    T["g_ffn"] = din("g_ffn", [4, 128, 16])
    T["g_pin"] = din("g_pin", [4, 128, 16])
    T["g_pout"] = din("g_pout", [4, 128, 16])
    T["gq"] = din("gq", [2, 128, 1])
    T["gk"] = din("gk", [2, 128, 1])
    T["vg"] = din("vg", [2, 128, 16])
    T["wsT"] = din("wsT", [2, 128, 16, 128])
    T["bs"] = din("bs", [2, 128, 16, 128])
    T["wr"] = din("wr", [4, D, 36])
    T["br"] = din("br", [4, 128, 36])
    T["sb_w_in"] = din("sb_w_in", [2, D, 3 * D])
    T["sb_w_out"] = din("sb_w_out", [2, D, D])
    T["sg_w_in"] = din("sg_w_in", [2, D, 4096])
    T["sg_w_out"] = din("sg_w_out", [2, D, D])
    T["moe_w_gate"] = din("moe_w_gate", [4, 32, D, 256])
    T["moe_w_up"] = din("moe_w_up", [4, 32, D, 256])
    T["moe_w_down"] = din("moe_w_down", [4, 32, 256, D])
    T["ple_w_gate"] = din("ple_w_gate", [4, D, D])
    T["ple_w_proj"] = din("ple_w_proj", [4, 256, D])
    T["ident"] = din("ident", [128, 128])
    T["ones"] = din("ones", [128, 128])
    T["trimask"] = din("trimask", [128, 128])
    T["ntri"] = din("ntri", [128, 128])
    T["masks"] = din("masks", [128, 4, 512])
    xo = nc.dram_tensor("xo", [D, NT], F32, kind="ExternalOutput").ap()
    XS = [nc.dram_tensor("XS%d" % k, [D, NT], F32).ap() for k in range(2)]
    for nm, shp in (("QS", [D, NT]), ("KS", [D, NT]), ("VS", [D, NT]), ("OS", [D, NT]),
                    ("QG", [8, 1024, NT]), ("KG", [8, 1024, NT]), ("VG", [8, 1024, NT]), ("OG", [8, 1024, NT])):
        T[nm] = [nc.dram_tensor("%s%d" % (nm, k), shp, BF16).ap() for k in range(2)]
    for nm in ("QL", "KL", "VL", "OL"):
        T[nm] = nc.dram_tensor(nm, [D, NT], BF16).ap()

    _RANK.clear()
    _VIEWS.clear()
    with ExitStack() as es:
        C = Ctx(nc, es)
        S = C.S
        S.ops['pool'].append((lambda e: (get_rank(e), None)[1], [], None))
        WORDS = 52992
        BIG = es.enter_context(nc.sbuf_tensor("BIG", [128, WORDS], F32))
        sb = Arena(BIG[:, :], WORDS)
        PPt = [es.enter_context(nc.psum_tensor("pp%d" % k, [128, 1024], F32)) for k in range(4)]
        PP = [t[:, :] for t in PPt]
        PB = [PP[k // 2][:, (k % 2) * 512:(k % 2 + 1) * 512] for k in range(8)]
        pk = ['p%d' % k for k in range(8)]

        def make_gather_chunk(j):
            def gather_chunk(nm, k, rkey):
                a = T[nm][j]
                b = T[nm[0] + "G"][j]
                S.cc(lambda e: e.collective_compute("AllGather", ALU.bypass, replica_groups=GROUPS,
                                                    ins=[a[k * 256:(k + 1) * 256, :].opt()], outs=[b[k].opt()]), [rkey], [(nm[0] + "G", k)])
            return gather_chunk

        def pick_all(j, names):
            for nm in names:
                b = T[nm + "G"][j]
                loc = T[nm + "L"]

                def pick(e, two, b=b, loc=loc):
                    mine = b.rearrange("(g two) (r xp) t -> g r two xp t", two=2, r=4)[bass.ds(get_rank(e), 1)].squeeze(0)
                    return e.dma_start(out=loc.rearrange("(r two xp) t -> r two xp t", r=4, two=2)[:, two], in_=mine[:, two])
                for two in range(2):
                    C.DMAF('pool', lambda e, two=two, pick=pick: pick(e, two), [(nm + "G", k) for k in range(8)], [nm + "L"])

        for i in range(4):
            src = T["xT"] if i == 0 else XS[(i - 1) % 2]
            dst = xo if i == 3 else XS[i % 2]
            j = i // 2
            if i % 2 == 0:
                sb.reset()
                gc = make_gather_chunk(j)
                emit_p1(C, sb, PB, pk, T, j, i, src, gc)
                S.barrier()
                pick_all(j, ["Q", "K", "V"])
                sb.reset()
                emit_p2(C, sb, PB, pk, T, j, gc, PP)
                S.barrier()
                pick_all(j, ["O"])
                sb.reset()
                emit_tok(C, sb, PB, pk, T, 'attn', i, src, dst, False)
                S.barrier()
            else:
                sb.reset()
                emit_tok(C, sb, PB, pk, T, 'sg', i, src, dst, i == 3)
                S.barrier()
        S.emit()
    return nc


def fm(g):
    return np.ascontiguousarray(g.reshape(16, 128).T.astype(np.float32))


def kernel(**inp):
    inp = {k: np.asarray(v) for k, v in inp.items()}
    x = np.ascontiguousarray(inp["x"], dtype=np.float32).reshape(16384, 2048)
    p = inp["p"].reshape(4, 16384, 256)
    k = np.arange(128)
    q = np.arange(512)
    shared = {
        "g_mix": np.stack([fm(inp["norm_mix"][i]) for i in range(4)]),
        "g_ffn": np.stack([fm(inp["norm_ffn"][i]) for i in range(4)]),
        "g_pin": np.stack([fm(inp["ple_norm_in"][i]) for i in range(4)]),
        "g_pout": np.stack([fm(inp["ple_norm_out"][i]) for i in range(4)]),
        "gq": np.ascontiguousarray(inp["sb_q_norm"].reshape(2, 128, 1)),
        "gk": np.ascontiguousarray(inp["sb_k_norm"].reshape(2, 128, 1)),
        "vg": np.stack([fm(inp["sg_v_norm"][j]) for j in range(2)]),
        "wsT": np.ascontiguousarray(np.transpose(inp["sg_w_s"], (0, 3, 1, 2))),
        "bs": np.ascontiguousarray(np.broadcast_to(inp["sg_b_s"][:, None], (2, 128, 16, 128))),
        "wr": np.ascontiguousarray(np.concatenate([inp["moe_w_group"], inp["moe_w_expert"]], axis=2)),
        "br": np.ascontiguousarray(np.broadcast_to(np.concatenate([inp["moe_b_group"], inp["moe_b_expert"]], axis=1)[:, None, :], (4, 128, 36))),
        "sb_w_in": inp["sb_w_in"], "sb_w_out": inp["sb_w_out"], "sg_w_in": inp["sg_w_in"], "sg_w_out": inp["sg_w_out"],
        "moe_w_gate": inp["moe_w_gate"], "moe_w_up": inp["moe_w_up"], "moe_w_down": inp["moe_w_down"],
        "ple_w_gate": inp["ple_w_gate"], "ple_w_proj": inp["ple_w_proj"],
        "ident": np.eye(128, dtype=np.float32), "ones": np.ones((128, 128), np.float32),
        "trimask": (k[:, None] <= k[None, :]).astype(np.float32),
        "ntri": -(k[:, None] >= k[None, :]).astype(np.float32),
        "masks": np.ascontiguousarray(np.stack([((k[:, None] + o * 128) < q[None, :]).astype(np.float32) for o in (3, 2, 1, 0)], axis=1)),
    }
    maps = []
    for c in range(N_CORES):
        m = dict(shared)
        m["xT"] = np.ascontiguousarray(x[c * NT:(c + 1) * NT].T)
        m["pT"] = np.ascontiguousarray(np.transpose(p[:, c * NT:(c + 1) * NT, :], (0, 2, 1)))
        maps.append(m)
    nc = build_fused()
    res = run_bass_kernel_spmd(nc, maps, core_ids=list(range(N_CORES)))
    out = np.concatenate([res.results[c]["xo"].T for c in range(N_CORES)], axis=0).reshape(2, 8192, 2048)
    return np.ascontiguousarray(out, dtype=np.float32)
```
